# Optimizing a Trainium2 kernel written in Bass

```python
import jax, jax.numpy as jnp
from jax import lax
import numpy as np

D_MODEL = 1024
BATCH = 8
SEQ = 4096
DEPTH = 1

N_HEADS = 8
HEAD_DIM = 64
ATTN_WIDTH = N_HEADS * HEAD_DIM
KV_RANK = 128
IDX_HEADS = 8
IDX_DIM = 64
TOPK_MAX = 256
Q_BLOCK = 128
CHUNK = 128
GMLP_GROUPS = 4
GMLP_GROUP_DIM = 128
GMLP_WIDTH = GMLP_GROUPS * GMLP_GROUP_DIM
NUM_BUCKETS = 32
MAX_DISTANCE = 128
N_GROUPS = 4
EXPERTS_PER_GROUP = 8
N_EXPERTS = N_GROUPS * EXPERTS_PER_GROUP
TOP_K_INNER = 2
D_FF_EXPERT = 256
EPS = 1e-6
IN_SIZES = (ATTN_WIDTH, KV_RANK, IDX_HEADS * IDX_DIM, IDX_DIM, IDX_HEADS, GMLP_WIDTH, GMLP_WIDTH, D_MODEL, D_MODEL)
IN_COLS = ATTN_WIDTH + KV_RANK + IDX_HEADS * IDX_DIM + IDX_DIM + IDX_HEADS + 2 * GMLP_WIDTH + 2 * D_MODEL

kernel_name = "hybrid_dsa_gmlp_hmoe"


def rms_norm(x, g):
    xf = x.astype(jnp.float32)
    y = xf * lax.rsqrt(jnp.mean(xf * xf, axis=-1, keepdims=True) + EPS)
    return y.astype(x.dtype) * g


def layer_norm(x, g, b):
    xf = x.astype(jnp.float32)
    mu = jnp.mean(xf, axis=-1, keepdims=True)
    var = jnp.mean(jnp.square(xf - mu), axis=-1, keepdims=True)
    return ((xf - mu) * lax.rsqrt(var + EPS)).astype(x.dtype) * g + b


def t5_bucket(n):
    max_exact = NUM_BUCKETS // 2
    nf = jnp.maximum(n, 1).astype(jnp.float32)
    large = max_exact + (jnp.log(nf / max_exact) / np.float32(np.log(MAX_DISTANCE / max_exact)) * (NUM_BUCKETS - max_exact)).astype(jnp.int32)
    large = jnp.minimum(large, NUM_BUCKETS - 1)
    return jnp.where(n < max_exact, n, large)


def sparse_attention(q_lat, q_idx, w_idx, c_kv, k_idx, rel_bias):
    B, T = c_kv.shape[:2]
    nb = T // Q_BLOCK
    topk = min(TOPK_MAX, T // 4)
    scale = HEAD_DIM ** -0.5
    s_pos = jnp.arange(T, dtype=jnp.int32)

    def to_blocks(a):
        return jnp.moveaxis(a.reshape((B, nb, Q_BLOCK) + a.shape[2:]), 1, 0)

    def block(args):
        blk, ql, qi, wi = args
        t_pos = blk * Q_BLOCK + jnp.arange(Q_BLOCK, dtype=jnp.int32)
        rel = jax.nn.relu(jnp.einsum('bqhd,bsd->bqhs', qi, k_idx))
        index = jnp.einsum('bqhs,bqh->bqs', rel, wi)
        causal = s_pos[None, :] <= t_pos[:, None]
        index = jnp.where(causal[None], index, -jnp.inf)
        _, sel = lax.top_k(index, topk)
        c_sel = jax.vmap(lambda c, i: c[i])(c_kv, sel)
        dist = t_pos[None, :, None] - sel
        valid = dist >= 0
        bias = rel_bias[t5_bucket(jnp.maximum(dist, 0))]
        scores = jnp.einsum('bqhr,bqkr->bqhk', ql, c_sel).astype(jnp.float32) * scale
        scores = scores + jnp.transpose(bias, (0, 1, 3, 2)).astype(jnp.float32)
        scores = jnp.where(valid[:, :, None, :], scores, -jnp.inf)
        p = jax.nn.softmax(scores, axis=-1).astype(c_sel.dtype)
        return jnp.einsum('bqhk,bqkr->bqhr', p, c_sel)

    out = lax.map(block, (jnp.arange(nb, dtype=jnp.int32), to_blocks(q_lat), to_blocks(q_idx), to_blocks(w_idx)))
    return jnp.moveaxis(out, 0, 1).reshape(B, T, N_HEADS, KV_RANK)


def chunked_gmlp(u, v, ln_g, ln_b, w_s, b_s):
    B, T = u.shape[:2]
    u = jax.nn.gelu(u)
    v = layer_norm(jax.nn.gelu(v), ln_g, ln_b)
    vc = v.reshape(B, T // CHUNK, CHUNK, GMLP_GROUPS, GMLP_GROUP_DIM)
    mask = jnp.tril(jnp.ones((CHUNK, CHUNK), dtype=bool))
    ws = jnp.where(mask[None], w_s, 0.0)
    s = jnp.einsum('gij,bcjge->bcige', ws, vc) + jnp.transpose(b_s)[None, None, :, :, None]
    return u * s.reshape(B, T, GMLP_WIDTH)


def hier_moe(h, rg_w, rg_b, re_w, re_b, w_gate, w_up, w_down):
    N = h.shape[0]
    gp = jax.nn.softmax((h @ rg_w + rg_b).astype(jnp.float32), axis=-1)
    g_w, g_idx = lax.top_k(gp, 1)
    el = (h @ re_w + re_b).reshape(N, N_GROUPS, EXPERTS_PER_GROUP)
    el = jnp.take_along_axis(el, g_idx[:, :, None], axis=1)[:, 0]
    ep = jax.nn.softmax(el.astype(jnp.float32), axis=-1)
    e_w, e_idx = lax.top_k(ep, TOP_K_INNER)
    e_w = g_w * e_w / jnp.sum(e_w, axis=-1, keepdims=True)
    eid = g_idx * EXPERTS_PER_GROUP + e_idx
    comb = jnp.sum(jax.nn.one_hot(eid, N_EXPERTS, dtype=jnp.float32) * e_w[..., None], axis=1).astype(h.dtype)
    y = jnp.zeros_like(h)
    for e in range(N_EXPERTS):
        he = jax.nn.silu(h @ w_gate[e]) * (h @ w_up[e])
        y = y + comb[:, e:e + 1] * (he @ w_down[e])
    return y


def setup_inputs(seed: int = 0) -> dict:
    key = jax.random.key(seed)
    ks = jax.random.split(key, 24)

    def nrm(k, shape, s):
        return jax.random.normal(k, shape, jnp.float32) * s

    return {
        "x": nrm(ks[0], (BATCH, SEQ, D_MODEL), 1.0),
        "w_in": nrm(ks[1], (DEPTH, D_MODEL, IN_COLS), D_MODEL ** -0.5),
        "kv_norm_g": 1.0 + nrm(ks[2], (DEPTH, KV_RANK), 0.01),
        "w_uk": nrm(ks[3], (DEPTH, KV_RANK, N_HEADS, HEAD_DIM), KV_RANK ** -0.5),
        "w_uv": nrm(ks[4], (DEPTH, KV_RANK, N_HEADS, HEAD_DIM), KV_RANK ** -0.5),
        "rel_bias": nrm(ks[5], (NUM_BUCKETS, N_HEADS), 0.5),
        "ln_v_g": 1.0 + nrm(ks[6], (DEPTH, GMLP_WIDTH), 0.01),
        "ln_v_b": nrm(ks[7], (DEPTH, GMLP_WIDTH), 0.01),
        "w_spatial": nrm(ks[8], (DEPTH, GMLP_GROUPS, CHUNK, CHUNK), CHUNK ** -0.5),
        "b_spatial": 1.0 + nrm(ks[9], (DEPTH, GMLP_GROUPS, CHUNK), 0.01),
        "w_proj_a": nrm(ks[10], (DEPTH, ATTN_WIDTH, D_MODEL), ATTN_WIDTH ** -0.5),
        "w_proj_b": nrm(ks[11], (DEPTH, GMLP_WIDTH, D_MODEL), GMLP_WIDTH ** -0.5),
        "w_out": nrm(ks[12], (DEPTH, D_MODEL, D_MODEL), D_MODEL ** -0.5),
        "norm1_g": 1.0 + nrm(ks[13], (DEPTH, D_MODEL), 0.01),
        "norm2_g": 1.0 + nrm(ks[14], (DEPTH, D_MODEL), 0.01),
        "router_group_w": nrm(ks[15], (DEPTH, D_MODEL, N_GROUPS), D_MODEL ** -0.5),
        "router_group_b": nrm(ks[16], (DEPTH, N_GROUPS), 0.01),
        "router_expert_w": nrm(ks[17], (DEPTH, D_MODEL, N_EXPERTS), D_MODEL ** -0.5),
        "router_expert_b": nrm(ks[18], (DEPTH, N_EXPERTS), 0.01),
        "w_gate": nrm(ks[19], (DEPTH, N_EXPERTS, D_MODEL, D_FF_EXPERT), D_MODEL ** -0.5),
        "w_up": nrm(ks[20], (DEPTH, N_EXPERTS, D_MODEL, D_FF_EXPERT), D_MODEL ** -0.5),
        "w_down": nrm(ks[21], (DEPTH, N_EXPERTS, D_FF_EXPERT, D_MODEL), D_FF_EXPERT ** -0.5),
        "final_norm_g": 1.0 + nrm(ks[22], (D_MODEL,), 0.01),
    }


def reference(x, w_in, kv_norm_g, w_uk, w_uv, rel_bias, ln_v_g, ln_v_b, w_spatial, b_spatial, w_proj_a, w_proj_b, w_out, norm1_g, norm2_g, router_group_w, router_group_b, router_expert_w, router_expert_b, w_gate, w_up, w_down, final_norm_g):
    B, T, D = x.shape
    splits = [int(c) for c in np.cumsum(IN_SIZES)[:-1]]
    idx_scale = (IDX_HEADS ** -0.5) * (IDX_DIM ** -0.5)
    for l in range(DEPTH):
        h = rms_norm(x, norm1_g[l])
        proj = h @ w_in[l]
        q, ckv, qi, ki, wi, u, v, ga, gb = jnp.split(proj, splits, axis=-1)
        q = q.reshape(B, T, N_HEADS, HEAD_DIM)
        c_kv = rms_norm(ckv, kv_norm_g[l])
        q_lat = jnp.einsum('bthd,rhd->bthr', q, w_uk[l])
        q_idx = qi.reshape(B, T, IDX_HEADS, IDX_DIM)
        w_idx = wi * idx_scale
        o_lat = sparse_attention(q_lat, q_idx, w_idx, c_kv, ki, rel_bias)
        o_a = jnp.einsum('bthr,rhd->bthd', o_lat, w_uv[l]).reshape(B, T, ATTN_WIDTH)
        y_a = o_a @ w_proj_a[l]
        y_b = chunked_gmlp(u, v, ln_v_g[l], ln_v_b[l], w_spatial[l], b_spatial[l]) @ w_proj_b[l]
        merged = jax.nn.sigmoid(ga) * y_a + jax.nn.sigmoid(gb) * y_b
        x = x + merged @ w_out[l]
        h2 = rms_norm(x, norm2_g[l]).reshape(B * T, D)
        y = hier_moe(h2, router_group_w[l], router_group_b[l], router_expert_w[l], router_expert_b[l], w_gate[l], w_up[l], w_down[l])
        x = x + y.reshape(B, T, D)
    return rms_norm(x, final_norm_g)
```

```python
import os
from contextlib import ExitStack
import numpy as np
import concourse.bass as bass
import concourse.mybir as mybir
from concourse.bass_utils import run_bass_kernel_spmd

F32 = mybir.dt.float32
BF16 = mybir.dt.bfloat16
I32 = mybir.dt.int32
AF = mybir.ActivationFunctionType
ALU = mybir.AluOpType
AX = mybir.AxisListType

ENGS = ("tensor", "vector", "scalar", "gpsimd", "sync")

T = 4096
D = 1024
NT = 32
P = 128
NCOLS = 4296
C_Q, C_CKV, C_QI, C_KI, C_WI, C_U, C_V, C_GA, C_GB = 0, 512, 640, 1152, 1216, 1224, 1736, 2248, 3272
NSLOT_T = 96
NSLOT = NSLOT_T * 128
EPS = 1e-6
NIT = int(os.environ.get("K_NIT", "14"))
BIG = 1.0e4


class DSem:
    def __init__(self, prog, name):
        self.sem = prog.ctx.enter_context(prog.nc.semaphore(name))
        self.count = 0
        self.id = name


class Prog:
    def __init__(self, nc, ctx):
        self.nc = nc
        self.ctx = ctx
        self.ops = {e: [] for e in ENGS}
        self.cnt = {e: 0 for e in ENGS}
        self.esem = {e: ctx.enter_context(nc.semaphore("es_" + e)) for e in ENGS}
        self.seen = {e: {} for e in ENGS}
        self.lastw = {}
        self.readers = {}
        self.dsems = []
        self.free = []
        self.inuse = []
        self.pending_noinc = {e: False for e in ENGS}

    def dsem(self, name=None):
        if self.free:
            d = self.free.pop()
        else:
            d = DSem(self, f"ds{len(self.dsems)}")
            self.dsems.append(d)
        self.inuse.append(d)
        return d

    def recycle(self):
        self.free.extend(self.inuse)
        self.inuse = []

    def _need(self, eng, ev, waits):
        sem, val, sid, src = ev
        if self.seen[eng].get(sid, 0) >= val:
            return
        if sid in waits:
            val = max(val, waits[sid][1])
        waits[sid] = (sem, val)

    def _collect(self, eng, reads, writes):
        waits = {}
        for r in reads:
            ev = self.lastw.get(r)
            if ev is not None:
                if ev[3] == eng and eng == "tensor":
                    continue
                self._need(eng, ev, waits)
        for w in writes:
            ev = self.lastw.get(w)
            if ev is not None and ev[3] != eng:
                self._need(eng, ev, waits)
            for ev in self.readers.get(w, ()):
                if ev[3] == eng:
                    continue
                self._need(eng, ev, waits)
        for sid, (sem, val) in waits.items():
            self.seen[eng][sid] = val
        return list(waits.values())

    def _record(self, ev, reads, writes):
        for r in reads:
            self.readers.setdefault(r, []).append(ev)
        for w in writes:
            self.lastw[w] = ev
            self.readers[w] = []

    def op(self, eng, fn, reads=(), writes=(), inc=True):
        waits = self._collect(eng, reads, writes)
        if inc:
            self.cnt[eng] += 1
            val = self.cnt[eng]
            self.pending_noinc[eng] = False
        else:
            val = self.cnt[eng] + 1
            self.pending_noinc[eng] = True
        ev = (self.esem[eng], val, "es_" + eng, eng)
        self._record(ev, reads, writes)
        self.ops[eng].append((waits, fn, (self.esem[eng], 1) if inc else None))
        return ev

    def dma(self, queue, ds, fn, reads=(), writes=()):
        waits = self._collect(queue, reads, writes)
        ds.count += 16
        ev = (ds.sem, ds.count, ds.id, "dma")
        self._record(ev, reads, writes)
        self.ops[queue].append((waits, fn, (ds.sem, 16)))
        return ev

    def barrier(self):
        for e in ENGS:
            waits = {}
            for e2 in ENGS:
                if e2 != e and self.cnt[e2] > 0:
                    self._need(e, (self.esem[e2], self.cnt[e2], "es_" + e2, e2), waits)
            for d in self.dsems:
                if d.count > 0:
                    self._need(e, (d.sem, d.count, d.id, "dma"), waits)
            for sid, (sem, val) in waits.items():
                self.seen[e][sid] = val
            if waits:
                self.ops[e].append((list(waits.values()), None, None))

    def emit(self):
        nc = self.nc
        for e in ENGS:
            assert not self.pending_noinc[e], f"engine {e} ends with non-inc op"
            assert self.cnt[e] < 60000, (e, self.cnt[e])
        with nc.Block() as block:
            for e in ENGS:
                ops = self.ops[e]

                def body(h, ops=ops):
                    for waits, fn, inc in ops:
                        for sem, val in waits:
                            h.wait_ge(sem, val)
                        if fn is None:
                            continue
                        ins = fn(h)
                        if inc is not None:
                            ins.then_inc(inc[0], inc[1])

                getattr(block, e)(body)
        self.ops = {e: [] for e in ENGS}


class Ring:
    def __init__(self, tiles, name, prog=None):
        self.tiles = tiles
        self.name = name
        self.i = 0
        self.ds = [prog.dsem(f"{name}_d{k}") for k in range(len(tiles))] if prog else None

    def next(self):
        k = self.i % len(self.tiles)
        self.i += 1
        if self.ds:
            return self.tiles[k], f"{self.name}{k}", self.ds[k]
        return self.tiles[k], f"{self.name}{k}"


def bc_mid(ap, n):
    a = [list(x) for x in ap.ap]
    return bass.AP(ap.tensor, ap.offset, [a[0], [0, n]] + a[1:])


def bc_last(ap, n):
    a = [list(x) for x in ap.ap]
    return bass.AP(ap.tensor, ap.offset, a + [[0, n]])


def t5_bucket_np(n):
    n = np.asarray(n, dtype=np.int32)
    nf = np.maximum(n, 1).astype(np.float32)
    large = 16 + (np.log(nf / np.float32(16)) / np.float32(np.log(128 / 16)) * np.float32(16)).astype(np.int32)
    large = np.minimum(large, 31)
    return np.where(n < 16, n, large)


def host_consts():
    c = {}
    c["c_ident"] = np.eye(128, dtype=np.float32)
    k = np.arange(128)
    c["c_tri"] = (k[:, None] <= k[None, :]).astype(np.float32)
    c["c_cneg"] = np.where(k[None, :] <= k[:, None], 0.0, -1.0e30).astype(np.float32)
    oh = np.zeros((33, 383), np.float32)
    for npr in range(383):
        n = npr - 127
        if n < 0:
            oh[32, npr] = 1.0
        else:
            oh[int(t5_bucket_np(n)), npr] = 1.0
    c["c_oh33"] = oh
    mc = np.zeros((32, 33), np.float32)
    for m in range(32):
        mc[m, m] += 1.0
        mc[31, m] -= 1.0
    c["c_mc"] = mc
    addc = np.zeros((33, 1), np.float32)
    addc[32, 0] = -30000.0
    c["c_addc"] = addc
    c["c_tokid"] = (np.arange(32)[None, :, None] * 128 + np.arange(128)[:, None, None] + np.zeros((1, 1, 16))).astype(np.int32)
    c["c_pcol"] = np.arange(128, dtype=np.float32)[:, None].copy()
    c["c_j128"] = (np.zeros((128, 1)) + np.arange(NSLOT_T)[None, :] * 128.0).astype(np.float32)
    c["c_pw"] = (np.zeros((128, 1)) + (0.5 ** np.arange(32))[None, :]).astype(np.float32)
    return c


CONST_SHAPES = {"c_ident": ([128, 128], F32), "c_tri": ([128, 128], F32), "c_cneg": ([128, 128], F32),
                "c_oh33": ([33, 383], F32), "c_mc": ([32, 33], F32), "c_addc": ([33, 1], F32),
                "c_tokid": ([128, 32, 16], I32), "c_pcol": ([128, 1], F32), "c_j128": ([128, NSLOT_T], F32), "c_pw": ([128, 32], F32)}

IN_SHAPES = {
    "x": [T, D], "w_in": [D, NCOLS], "kv_norm_g": [1, 128], "w_uk": [128, 512], "w_uv": [128, 512],
    "rel_bias": [32, 8], "ln_v_g": [1, 512], "ln_v_b": [1, 512], "w_spatial": [4, 128, 128], "b_spatial": [1, 512],
    "w_proj_a": [512, D], "w_proj_b": [512, D], "w_out": [D, D], "norm1_g": [128, 8], "norm2_g": [1, D],
    "router_group_w": [D, 4], "router_group_b": [1, 4], "router_expert_w": [D, 32], "router_expert_b": [1, 32],
    "w_gate": [32 * 128, 2048], "w_up": [32 * 128, 2048], "w_down": [32 * 128, 2048], "final_norm_g": [1, D],
}


def build_program(stage="E", debug=False):
    nc = bass.Bass("TRN2", target_bir_lowering=False)
    I = {k: nc.dram_tensor(k, s, F32, kind="ExternalInput") for k, s in IN_SHAPES.items()}
    for k, (s, dt) in CONST_SHAPES.items():
        I[k] = nc.dram_tensor(k, s, dt, kind="ExternalInput")
    out_d = nc.dram_tensor("out", [T, D], F32, kind="ExternalOutput")
    skind = "ExternalOutput" if debug else "Internal"
    QL = nc.dram_tensor("s_ql", [NT, 128, 1024], BF16, kind=skind)
    QI = nc.dram_tensor("s_qi", [NT, 128, 512], BF16, kind=skind)
    GM = nc.dram_tensor("s_gm", [NT, 128, 512], BF16, kind=skind)
    SG = nc.dram_tensor("s_sg", [NT, 128, 2048], BF16, kind=skind)
    X1 = nc.dram_tensor("s_x1", [T, D], F32, kind=skind)
    H2 = nc.dram_tensor("s_h2", [T + 128, D], BF16, kind=skind)
    SLOT = nc.dram_tensor("s_slot", [NSLOT, 16], I32, kind=skind)
    YS = nc.dram_tensor("s_ys", [NSLOT, D], F32, kind=skind)
    EBS = nc.dram_tensor("s_ebs", [8, 128, 383], F32, kind=skind)
    WEB = nc.dram_tensor("s_b_w", [32 * 128, 6144], BF16, kind="Internal")
    DBG = nc.dram_tensor("s_dbg", [NT, 128, 1024], F32, kind=skind) if debug else None

    with ExitStack() as ctx:
        ctx.enter_context(nc.allow_low_precision(reason="bf16 matmul operands / bf16 intermediates by design"))
        p = Prog(nc, ctx)

        def sbt(c, name, shape, dt):
            return c.enter_context(nc.sbuf_tensor(name, shape, dt))

        def pst(c, name, shape, dt=F32):
            return c.enter_context(nc.psum_tensor(name, shape, dt))

        def MM(out, lhsT, rhs, start, stop, reads, writes, inc=True, skip=False):
            if skip:
                return p.op("tensor", lambda e: e.matmul(out, lhsT, rhs, start=start, stop=stop, skip_group_check=True), reads, writes, inc)
            return p.op("tensor", lambda e: e.matmul(out, lhsT, rhs, start=start, stop=stop), reads, writes, inc)

        def TR(out, in_, ident, reads, writes, inc=True):
            return p.op("tensor", lambda e: e.transpose(out, in_, ident), reads, writes, inc)

        def ACT(out, in_, func, reads, writes, **kw):
            return p.op("scalar", lambda e: e.activation(out=out, in_=in_, func=func, **kw), reads, writes)

        def TS(eng, out, in0, s1, s2, op0, op1, reads, writes, accum_out=None):
            if op1 is None:
                return p.op(eng, lambda e: e.tensor_scalar(out=out, in0=in0, scalar1=s1, scalar2=None, op0=op0), reads, writes)
            if accum_out is not None:
                return p.op(eng, lambda e: e.tensor_scalar(out=out, in0=in0, scalar1=s1, scalar2=s2, op0=op0, op1=op1, accum_out=accum_out), reads, writes)
            return p.op(eng, lambda e: e.tensor_scalar(out=out, in0=in0, scalar1=s1, scalar2=s2, op0=op0, op1=op1), reads, writes)

        def TT(eng, out, in0, in1, op, reads, writes):
            return p.op(eng, lambda e: e.tensor_tensor(out=out, in0=in0, in1=in1, op=op), reads, writes)

        def STT(out, in0, scalar, in1, op0, op1, reads, writes):
            return p.op("vector", lambda e: e.scalar_tensor_tensor(out=out, in0=in0, scalar=scalar, in1=in1, op0=op0, op1=op1), reads, writes)

        def COPY(eng, out, in_, reads, writes):
            if eng == "scalar":
                return ACT(out, in_, AF.Copy, reads, writes)
            return p.op(eng, lambda e: e.tensor_copy(out=out, in_=in_), reads, writes)

        def RED(out, in_, op, reads, writes, negate=False):
            return p.op("vector", lambda e: e.tensor_reduce(out=out, in_=in_, axis=AX.X, op=op, negate=negate), reads, writes)

        def RECIP(out, in_, reads, writes):
            return p.op("vector", lambda e: e.reciprocal(out=out, in_=in_), reads, writes)

        def MEMSET(eng, ap, val, writes):
            return p.op(eng, lambda e: e.memset(ap, val), (), writes)

        def DMA(queue, ds, out, in_, reads, writes):
            return p.dma(queue, ds, lambda e: e.dma_start(out=out, in_=in_), reads, writes)

        def LOAD(queue, out, in_, key, reads=()):
            return DMA(queue, p.dsem(), out, in_, list(reads), [key])

        def GATHER(ds, out, in_, idx, reads, writes, bound=None):
            if bound is not None:
                return p.dma("gpsimd", ds, lambda e: e.indirect_dma_start(out=out, out_offset=None, in_=in_, in_offset=bass.IndirectOffsetOnAxis(ap=idx, axis=0), bounds_check=bound, oob_is_err=False), reads, writes)
            return p.dma("gpsimd", ds, lambda e: e.indirect_dma_start(out=out, out_offset=None, in_=in_, in_offset=bass.IndirectOffsetOnAxis(ap=idx, axis=0)), reads, writes)

        def SCATTER(ds, out, idx, in_, reads, writes):
            return p.dma("gpsimd", ds, lambda e: e.indirect_dma_start(out=out, out_offset=bass.IndirectOffsetOnAxis(ap=idx, axis=0), in_=in_, in_offset=None), reads, writes)

        def rstd_from_ss(rstd, ss, scale, tmp, rk, sk, tk, eps=EPS):
            TS("gpsimd", tmp, ss, scale, eps, ALU.mult, ALU.add, [sk], [tk])
            TT("gpsimd", rstd, tmp, mhalf[:, 0:1], ALU.pow, [tk, "mhalf"], [rk])

        ident_f = sbt(ctx, "ident_f", [128, 128], F32)
        ident_b = sbt(ctx, "ident_b", [128, 128], BF16)
        tri_b = sbt(ctx, "tri_b", [128, 128], BF16)
        tri_f = sbt(ctx, "tri_f", [128, 128], F32)
        ones_b = sbt(ctx, "ones_b", [128, 128], BF16)
        zeros_b = sbt(ctx, "zeros_b", [128, 128], BF16)
        mhalf = sbt(ctx, "mhalf", [128, 1], F32)
        cneg = sbt(ctx, "cneg", [128, 128], F32)
        kiT2 = sbt(ctx, "kiT2", [128, T], BF16)
        ckvT = sbt(ctx, "ckvT", [128, T], BF16)
        Vaug = sbt(ctx, "Vaug", [128, NT, 130], BF16)
        WIall = sbt(ctx, "WIall", [128, NT, 8], F32)
        LG = sbt(ctx, "LG", [128, NT, 36], F32)
        MASKall = sbt(ctx, "MASKall", [128, NT, 32], BF16)
        OH1all = sbt(ctx, "OH1all", [128, NT, 32], BF16)
        OH2all = sbt(ctx, "OH2all", [128, NT, 32], BF16)
        W1all = sbt(ctx, "W1all", [128, NT], F32)
        W2all = sbt(ctx, "W2all", [128, NT], F32)
        banks = [pst(ctx, f"bank{k}", [128, 512]) for k in range(8)]
        dconst = p.dsem("dconst")

        with ExitStack() as c0:
            oh33 = sbt(c0, "oh33", [33, 383], F32)
            mc = sbt(c0, "mc", [32, 33], F32)
            addc = sbt(c0, "addc", [33, 1], F32)
            rb = sbt(c0, "rb", [32, 8], F32)
            rbrel = sbt(c0, "rbrel", [33, 8], F32)
            rbB = sbt(c0, "rbB", [33, 8, 128], F32)
            ebrow = sbt(c0, "ebrow", [128, 8, 383], F32)
            LOAD("sync", ident_f[:], I["c_ident"].ap(), "ident_f")
            LOAD("sync", tri_f[:], I["c_tri"].ap(), "tri_f")
            LOAD("sync", cneg[:], I["c_cneg"].ap(), "cneg")
            LOAD("sync", oh33[:], I["c_oh33"].ap(), "oh33")
            LOAD("sync", mc[:], I["c_mc"].ap(), "mc")
            LOAD("sync", addc[:], I["c_addc"].ap(), "addc")
            LOAD("sync", rb[:], I["rel_bias"].ap(), "rb")
            COPY("vector", ident_b[:], ident_f[:], ["ident_f"], ["ident_b"])
            COPY("vector", tri_b[:], tri_f[:], ["tri_f"], ["tri_b"])
            MEMSET("vector", ones_b[:], 1.0, ["ones_b"])
            MEMSET("vector", zeros_b[:], 0.0, ["zeros_b"])
            MEMSET("gpsimd", mhalf[:], -0.5, ["mhalf"])
            MEMSET("vector", Vaug[:, :, 128:129], 1.0, ["Vaug_ones"])
            MM(banks[0][0:33, 0:8], mc[:], rb[:], True, True, ["mc", "rb"], ["bank0"])
            TS("vector", rbrel[:], banks[0][0:33, 0:8], addc[:, 0:1], None, ALU.add, None, ["bank0", "addc"], ["rbrel"])
            COPY("vector", rbB[:], bc_last(rbrel[:], 128), ["rbrel"], ["rbB"])
            for h in range(8):
                bk = banks[1 + (h % 4)]
                bkey = f"bank{1 + (h % 4)}"
                MM(bk[:, 0:383], rbB[:, h, :], oh33[:], True, True, ["rbB", "oh33"], [bkey])
                ACT(ebrow[:, h, :], bk[:, 0:383], AF.Exp, [bkey], [f"ebrow{h}"])
            LOAD("sync", EBS.ap().rearrange("h p n -> p h n"), ebrow[:], "EBS", reads=[f"ebrow{h}" for h in range(8)])
            p.barrier()
            p.recycle()
            p.emit()

        with ExitStack() as ca:
            Win = sbt(ca, "Win", [128, 8, NCOLS], BF16)
            Wki2 = sbt(ca, "Wki2", [128, 8, 128], BF16)
            g1 = sbt(ca, "g1", [128, 8], F32)
            wuk_n = sbt(ca, "wuk_n", [128, 512], BF16)
            WukT = sbt(ca, "WukT", [128, 8, 128], BF16)
            ws_n = sbt(ca, "ws_n", [128, 4, 128], BF16)
            WsT = sbt(ca, "WsT", [128, 4, 128], BF16)
            bsB = sbt(ca, "bsB", [128, 512], F32)
            lngB = sbt(ca, "lngB", [128, 512], F32)
            lnbB = sbt(ca, "lnbB", [128, 512], F32)
            kvgB = sbt(ca, "kvgB", [128, 128], F32)
            dwa = p.dsem("dwa")
            LOAD("sync", g1[:], I["norm1_g"].ap(), "g1")
            for c in range(8):
                LOAD("gpsimd", Win[:, c, :], I["w_in"].ap()[c * 128:(c + 1) * 128, :], f"Win{c}")
            for c in range(8):
                TS("vector", Win[:, c, :], Win[:, c, :], g1[:, c:c + 1], None, ALU.mult, None, [f"Win{c}", "g1"], [f"Win{c}"])
            for c in range(8):
                COPY("vector", Wki2[:, c, 0:64], Win[:, c, C_KI:C_KI + 64], [f"Win{c}"], ["Wki2"])
                COPY("vector", Wki2[:, c, 64:128], Win[:, c, C_KI:C_KI + 64], [f"Win{c}"], ["Wki2"])
            LOAD("gpsimd", wuk_n[:], I["w_uk"].ap(), "wuk_n")
            LOAD("gpsimd", ws_n[:], I["w_spatial"].ap().rearrange("g i j -> i g j"), "ws_n")
            LOAD("sync", bsB[:], I["b_spatial"].ap()[0:1, :].partition_broadcast(128) if False else bass.AP(I["b_spatial"], 0, [[0, 128], [1, 512]]), "bsB")
            LOAD("sync", lngB[:], bass.AP(I["ln_v_g"], 0, [[0, 128], [1, 512]]), "lngB")
            LOAD("sync", lnbB[:], bass.AP(I["ln_v_b"], 0, [[0, 128], [1, 512]]), "lnbB")
            LOAD("sync", kvgB[:], bass.AP(I["kv_norm_g"], 0, [[0, 128], [1, 128]]), "kvgB")
            MEMSET("vector", WukT[:], 0.0, ["WukT"])
            for k in range(4):
                bk = banks[k][:].bitcast(BF16)
                TR(bk[:, 0:128], wuk_n[:, k * 128:(k + 1) * 128], ident_b[:], ["wuk_n", "ident_b"], [f"bank{k}"])
                TS("vector", WukT[0:64, 2 * k, :], bk[0:64, 0:128], 0.125, None, ALU.mult, None, [f"bank{k}"], ["WukT"])
                TS("vector", WukT[64:128, 2 * k + 1, :], bk[64:128, 0:128], 0.125, None, ALU.mult, None, [f"bank{k}"], ["WukT"])
            for g in range(4):
                bk = banks[4 + g][:].bitcast(BF16)
                TR(bk[:, 0:128], ws_n[:, g, :], ident_b[:], ["ws_n", "ident_b"], [f"bank{4 + g}"])
                TT("vector", WsT[:, g, :], bk[:, 0:128], tri_b[:], ALU.mult, [f"bank{4 + g}", "tri_b"], ["WsT"])

            xin_r = Ring([sbt(ca, f"xin{k}", [128, D], F32) for k in range(4)], "xin", p)
            junk_a = sbt(ca, "junk_a", [128, D], BF16)
            ss_r = Ring([sbt(ca, f"ss{k}", [128, 4], F32) for k in range(2)], "ss")
            xs_r = Ring([sbt(ca, f"xs{k}", [128, D], BF16) for k in range(2)], "xs")
            hT_r = Ring([sbt(ca, f"hT{k}", [128, 8, 256], BF16) for k in range(2)], "hT")
            qT_r = Ring([sbt(ca, f"qT{k}", [128, 4, 128], BF16) for k in range(2)], "qT")
            ql_r = Ring([sbt(ca, f"qlt{k}", [128, 8, 128], BF16) for k in range(2)], "qlt", p)
            qi_r = Ring([sbt(ca, f"qit{k}", [128, 4, 128], BF16) for k in range(2)], "qit", p)
            gup_r = Ring([sbt(ca, f"gup{k}", [128, 4, 2, 128], BF16) for k in range(2)], "gup")
            t1_r = Ring([sbt(ca, f"ta{k}", [128, 512], F32) for k in range(3)], "ta")
            t2_r = Ring([sbt(ca, f"tb{k}", [128, 512], F32) for k in range(3)], "tb")
            gv_r = Ring([sbt(ca, f"gv{k}", [128, 512], F32) for k in range(2)], "gv")
            vn_r = Ring([sbt(ca, f"vn{k}", [128, 512], BF16) for k in range(2)], "vn")
            st_r = Ring([sbt(ca, f"st{k}", [128, 16], F32) for k in range(2)], "st")
            gm_r = Ring([sbt(ca, f"gmt{k}", [128, 512], BF16) for k in range(2)], "gmt", p)
            sg_r = Ring([sbt(ca, f"sgt{k}", [128, 2, 16, 128], BF16) for k in range(2)], "sgt", p)
            bank_i = [0]

            def nbank():
                k = bank_i[0] % 8
                bank_i[0] += 1
                return banks[k], f"bank{k}"

            def gelu_chain(ps, pk, outap, outk):
                ta, tak = t1_r.next()
                tb, tbk = t2_r.next()
                ACT(ta[:], ps, AF.Square, [pk], [tak], scale=float(np.sqrt(0.044715)))
                STT(tb[:], ta[:], 1.0, ps, ALU.add, ALU.mult, [tak, pk], [tbk])
                ACT(ta[:], tb[:], AF.Tanh, [tbk], [tak], scale=0.7978845608028654)
                STT(outap, ta[:], 1.0, ps, ALU.add, ALU.mult, [tak, pk], [outk])

            ntiles_a = NT if stage != "A1" else 2
            xin_q = {}

            def load_x(i):
                xin, xk, xd = xin_r.next()
                DMA("sync", xd, xin[:], I["x"].ap()[i * 128:(i + 1) * 128, :], [], [xk])
                xin_q[i] = (xin, xk)

            HT = {}
            XS = {}
            hstate = {}

            def fe(i):
                xin, xk = xin_q.pop(i)
                ss, ssk = ss_r.next()
                ACT(junk_a[:], xin[:], AF.Square, [xk], ["junk_a", ssk + "a"], accum_out=ss[:, 0:1])
                rstd_from_ss(ss[:, 2:3], ss[:, 0:1], 1.0 / D, ss[:, 1:2], ssk + "c", ssk + "a", ssk + "b")
                xs, xsk = xs_r.next()
                TS("vector", xs[:], xin[:], ss[:, 2:3], None, ALU.mult, None, [xk, ssk + "c"], [xsk])
                XS[i] = (xs, xsk)

            def fe_b(i):
                xs, xsk = XS.pop(i)
                bk, bkk = nbank()
                bkb = bk[:].bitcast(BF16)
                for c in range(8):
                    TR(bkb[:, c * 128:(c + 1) * 128], xs[:, c * 128:(c + 1) * 128], ident_b[:], [xsk, "ident_b"], [bkk], inc=(c == 7))
                par = i % 2
                if par == 0:
                    hstate["p"] = hT_r.next()
                hTp, hpk = hstate["p"]
                hT = hTp[:, :, par * 128:(par + 1) * 128]
                hk = hpk + f"_{par}"
                COPY("scalar", hT, bkb[:, 0:1024].rearrange("p (c t) -> p c t", t=128), [bkk], [hk])
                HT[i] = (hTp, hpk, hT, hk, par)

            def pview(bk, par):
                return bk[:, :].rearrange("p (g q t) -> p g q t", g=2, q=2)[:, :, par, :]

            def fm_pair(col0, ngroups, hTp, hkeys, wt=None):
                outb = []
                for bb in range((ngroups + 1) // 2):
                    bk, bkk = nbank()
                    ng = min(2, ngroups - 2 * bb)
                    for g in range(ng):
                        gg = 2 * bb + g
                        for c in range(8):
                            lhsT = Win[:, c, col0 + gg * 128: col0 + (gg + 1) * 128] if wt is None else wt[:, c, :]
                            MM(bk[:, g * 256:(g + 1) * 256], lhsT, hTp[:, c, :], c == 0, c == 7,
                               [f"Win{c}"] + hkeys + (["Wki2"] if wt is not None else []), [bkk], inc=(c == 7 and g == ng - 1))
                    outb.append((bk, bkk))
                return outb

            assert ntiles_a % 2 == 0
            for ii in range(min(4, ntiles_a)):
                load_x(ii)
            fe(0)
            fe(1)
            fe_b(0)
            fe_b(1)
            for m in range(ntiles_a // 2):
                i0 = 2 * m
                for ii in (i0 + 4, i0 + 5):
                    if ii < ntiles_a:
                        load_x(ii)
                hTp, hpk = HT[i0][0], HT[i0][1]
                hkeys = [hpk + "_0", hpk + "_1"]
                qb = fm_pair(C_Q, 4, hTp, hkeys)
                qTs = []
                for par in range(2):
                    qT, qk = qT_r.next()
                    for bb, (bk, bkk) in enumerate(qb):
                        COPY("scalar", qT[:, 2 * bb:2 * bb + 2, :], pview(bk, par), [bkk], [qk])
                    qTs.append((qT, qk))
                for par in range(2):
                    i = i0 + par
                    qT, qk = qTs[par]
                    qlt, qlk, qld = ql_r.next()
                    for half in range(2):
                        bk, bkk = nbank()
                        for hh in range(4):
                            h = half * 4 + hh
                            MM(bk[:, hh * 128:(hh + 1) * 128], WukT[:, h, :], qT[:, h // 2, :], True, True, ["WukT", qk], [bkk], inc=(hh == 3))
                        COPY("scalar", qlt[:, half * 4:(half + 1) * 4, :].rearrange("p c t -> p (c t)"), bk[:, :], [bkk], [qlk])
                    DMA("sync", qld, QL.ap()[i], qlt[:].rearrange("p c t -> p (c t)"), [qlk], [])
                for ii in (i0 + 2, i0 + 3):
                    if ii < ntiles_a:
                        fe(ii)
                qib = fm_pair(C_QI, 4, hTp, hkeys)
                for par in range(2):
                    i = i0 + par
                    qit, qik, qid = qi_r.next()
                    for bb, (bk, bkk) in enumerate(qib):
                        COPY("vector", qit[:, 2 * bb:2 * bb + 2, :], pview(bk, par), [bkk], [qik])
                    DMA("sync", qid, QI.ap()[i], qit[:].rearrange("p c t -> p (c t)"), [qik], [])
                (bk, bkk), = fm_pair(0, 1, hTp, hkeys, wt=Wki2)
                COPY("scalar", kiT2[:, i0 * 128:(i0 + 2) * 128], bk[:, 0:256], [bkk], [f"ki{i0}", f"ki{i0 + 1}"])
                ub = fm_pair(C_U, 4, hTp, hkeys)
                gup, gupk = gup_r.next()
                for bb, (bk, bkk) in enumerate(ub):
                    gelu_chain(bk[:, :], bkk, gup[:, 2 * bb:2 * bb + 2, :, :].rearrange("p g q t -> p (g q t)"), gupk)
                if i0 + 2 < ntiles_a:
                    fe_b(i0 + 2)
                TP = {}

                def tile_a(par):
                    i = i0 + par
                    _, _, hT, hk, _ = HT.pop(i)
                    bk, bkk = nbank()
                    for c in range(8):
                        MM(bk[:, :], hT[:, c, :], Win[:, c, C_V:C_V + 512], c == 0, c == 7, [hk, f"Win{c}"], [bkk], inc=(c == 7))
                    gv, gvk = gv_r.next()
                    gelu_chain(bk[:, :], bkk, gv[:], gvk)
                    st, stk = st_r.next()
                    p.op("vector", lambda e, o=st[:, 0:6], a=gv[:]: e.bn_stats(out=o, in_=a), [gvk], [stk + "a"])
                    p.op("vector", lambda e, o=st[:, 6:8], a=st[:, 0:6]: e.bn_aggr(out=o, in_=a), [stk + "a"], [stk + "b"])
                    rstd_from_ss(st[:, 9:10], st[:, 7:8], 1.0, st[:, 8:9], stk + "d", stk + "b", stk + "c", eps=4.0 * EPS)
                    TS("vector", gv[:], gv[:], st[:, 6:7], st[:, 9:10], ALU.subtract, ALU.mult, [gvk, stk + "b", stk + "d"], [gvk])
                    TT("vector", gv[:], gv[:], lngB[:], ALU.mult, [gvk, "lngB"], [gvk])
                    vn, vnk = vn_r.next()
                    TT("vector", vn[:], gv[:], lnbB[:], ALU.add, [gvk, "lnbB"], [vnk])
                    bk, bkk = nbank()
                    for c in range(8):
                        MM(bk[:, 0:128], hT[:, c, :], Win[:, c, C_CKV:C_CKV + 128], c == 0, c == 7, [hk, f"Win{c}"], [bkk], inc=False)
                    for c in range(8):
                        MM(bk[:, 128:136], hT[:, c, :], Win[:, c, C_WI:C_WI + 8], c == 0, c == 7, [hk, f"Win{c}"], [bkk], inc=(c == 7))
                    ACT(junk_a[:, 0:128], bk[:, 0:128], AF.Square, [bkk], ["junk_a", stk + "e"], accum_out=st[:, 10:11])
                    rstd_from_ss(st[:, 12:13], st[:, 10:11], 1.0 / 128, st[:, 11:12], stk + "g", stk + "e", stk + "f")
                    STT(Vaug[:, i, 0:128], bk[:, 0:128], st[:, 12:13], kvgB[:], ALU.mult, ALU.mult, [bkk, stk + "g", "kvgB"], [f"Vaug{i}"])
                    COPY("vector", WIall[:, i, :], bk[:, 128:136], [bkk], ["WIall"])
                    TP[par] = (vn, vnk)

                def tile_b(par):
                    i = i0 + par
                    vn, vnk = TP[par]
                    bk2, bk2k = nbank()
                    bk2b = bk2[:].bitcast(BF16)
                    TR(bk2b[:, 0:128], Vaug[:, i, 0:128], ident_b[:], [f"Vaug{i}", "ident_b"], [bk2k])
                    COPY("scalar", ckvT[:, i * 128:(i + 1) * 128], bk2b[:, 0:128], [bk2k], [f"ckvT{i}"])
                    bk, bkk = nbank()
                    for g in range(4):
                        MM(bk[:, g * 128:(g + 1) * 128], vn[:, g * 128:(g + 1) * 128], WsT[:, g, :], True, True, [vnk, "WsT"], [bkk], inc=(g == 3))
                    ta, tak = t1_r.next()
                    TT("vector", ta[:], bk[:, :], bsB[:], ALU.add, [bkk, "bsB"], [tak])
                    gmt, gmk, gmd = gm_r.next()
                    STT(gmt[:].rearrange("p (g t) -> p g t", t=128), gup[:, :, par, :], 0.5, ta[:].rearrange("p (g t) -> p g t", t=128),
                        ALU.mult, ALU.mult, [tak, gupk], [gmk])
                    DMA("sync", gmd, GM.ap()[i], gmt[:], [gmk], [])

                tile_a(0)
                tile_a(1)
                if i0 + 3 < ntiles_a:
                    fe_b(i0 + 3)
                sgp, sgk, sgd = sg_r.next()
                for b8 in range(8):
                    bk, bkk = nbank()
                    for g in range(2):
                        gg = b8 * 2 + g
                        for c in range(8):
                            MM(bk[:, g * 256:(g + 1) * 256], Win[:, c, C_GA + gg * 128:C_GA + (gg + 1) * 128], hTp[:, c, :], c == 0, c == 7,
                               [f"Win{c}"] + hkeys, [bkk], inc=(c == 7 and g == 1))
                    ta, tak = t1_r.next()
                    ACT(ta[:], bk[:, :], AF.Tanh, [bkk], [tak], scale=0.5)
                    TS("vector", sgp[:, :, b8 * 2:b8 * 2 + 2, :].rearrange("p par g t -> p g par t"),
                       ta[:].rearrange("p (g par t) -> p g par t", g=2, par=2), 0.5, 0.5, ALU.mult, ALU.add, [tak], [sgk])
                    if b8 == 3:
                        tile_b(0)
                    if b8 == 6:
                        tile_b(1)
                for pp in range(2):
                    DMA("sync", sgd, SG.ap()[i0 + pp], sgp[:, pp, :, :].rearrange("p c t -> p (c t)"), [sgk], [])
            if debug:
                ddbg = p.dsem("ddbg")
                p.barrier()
                DMA("gpsimd", ddbg, DBG.ap()[0], kiT2[:, 0:1024], [], [])
                DMA("gpsimd", ddbg, DBG.ap()[1], ckvT[:, 0:1024], [], [])
                DMA("gpsimd", ddbg, DBG.ap()[2][:, 0:260], Vaug[:, 0:2, :].rearrange("p a b -> p (a b)"), [], [])
                DMA("gpsimd", ddbg, DBG.ap()[3][:, 0:16], WIall[:, 0:2, :].rearrange("p a b -> p (a b)"), [], [])
            p.barrier()
            p.recycle()
            p.emit()

        if stage in ("A", "A1"):
            return nc

        with ExitStack() as cb:
            Wuv = sbt(cb, "Wuv", [128, 8, 128], BF16)
            Wpa = sbt(cb, "Wpa", [128, 4, D], BF16)
            Wpb = sbt(cb, "Wpb", [128, 4, D], BF16)
            Wout = sbt(cb, "Wout", [128, 8, D], BF16)
            g2B = sbt(cb, "g2B", [128, D], F32)
            Wr = sbt(cb, "Wr", [128, 8, 36], F32)
            rbias = sbt(cb, "rbias", [128, 36], F32)
            MEMSET("vector", Wuv[:], 0.0, ["Wuv"])
            BD = sbt(cb, "BD", [128, 8, 128], F32)
            BO = sbt(cb, "BO", [128, 8, 128], F32)
            for h in range(8):
                LOAD("sync", BD[:, h, :], bass.AP(EBS, h * 128 * 383 + 127, [[382, 128], [1, 128]]), f"BD{h}", reads=["EBS"])
                LOAD("sync", BO[:, h, :], bass.AP(EBS, h * 128 * 383 + 255, [[382, 128], [1, 128]]), f"BO{h}", reads=["EBS"])
            wuv_v = I["w_uv"].ap().rearrange("r (h d) -> r h d", d=64)
            LOAD("gpsimd", Wuv[:, 0::2, 0:64], wuv_v[:, 0::2, :], "Wuv")
            LOAD("gpsimd", Wuv[:, 1::2, 64:128], wuv_v[:, 1::2, :], "Wuv")
            LOAD("gpsimd", Wpa[:], I["w_proj_a"].ap().rearrange("(k p) n -> p k n", p=128), "Wpa")
            LOAD("gpsimd", Wpb[:], I["w_proj_b"].ap().rearrange("(k p) n -> p k n", p=128), "Wpb")
            LOAD("gpsimd", Wout[:], I["w_out"].ap().rearrange("(c p) n -> p c n", p=128), "Wout")
            LOAD("sync", g2B[:], bass.AP(I["norm2_g"], 0, [[0, 128], [1, D]]), "g2B")
            LOAD("sync", Wr[:, :, 0:4], I["router_group_w"].ap().rearrange("(c p) n -> p c n", p=128), "Wr")
            LOAD("sync", Wr[:, :, 4:36], I["router_expert_w"].ap().rearrange("(c p) n -> p c n", p=128), "Wr")
            LOAD("sync", rbias[:, 0:4], bass.AP(I["router_group_b"], 0, [[0, 128], [1, 4]]), "rbias")
            LOAD("sync", rbias[:, 4:36], bass.AP(I["router_expert_b"], 0, [[0, 128], [1, 32]]), "rbias")

            bql_r = Ring([sbt(cb, f"bql{k}", [128, 8, 128], BF16) for k in range(3)], "bql", p)
            bqi_r = Ring([sbt(cb, f"bqi{k}", [128, 8, 128], BF16) for k in range(2)], "bqi", p)
            for k in range(2):
                MEMSET("gpsimd", bqi_r.tiles[k][:], 0.0, [f"bqi{k}"])
            idx2 = [sbt(cb, f"idx{k}", [128, T], F32) for k in range(2)]
            mask = sbt(cb, "mask", [128, T], BF16)
            maskT = sbt(cb, "maskT", [128, NT, 128], BF16)
            rl_r = Ring([sbt(cb, f"rl{k}", [128, 512], BF16) for k in range(3)], "rl")
            Dg_r = Ring([sbt(cb, f"Dg{k}", [128, 8, 128], BF16) for k in range(2)], "Dg")
            aw_r = Ring([sbt(cb, f"aw{k}", [128, 16], F32) for k in range(2)], "aw")
            sc_r = Ring([sbt(cb, f"sc{k}", [128, 64], F32) for k in range(4)], "sc")
            pw = sbt(cb, "pw", [128, 32], F32)
            LOAD("sync", pw[:], I["c_pw"].ap(), "pw")
            MnD = sbt(cb, "MnD", [128, 8, 128], BF16)
            MnO = sbt(cb, "MnO", [128, 8, 128], BF16)
            P_r = Ring([sbt(cb, f"Pt{k}", [128, 8, 128], BF16) for k in range(4)], "Pt")
            OL = sbt(cb, "OL", [128, 8, 128], BF16)
            rden = sbt(cb, "rden", [128, 8], F32)
            OLT = sbt(cb, "OLT", [128, 8, 128], BF16)
            oaT = sbt(cb, "oaT", [128, 4, 128], BF16)
            bgm_r = Ring([sbt(cb, f"bgm{k}", [128, 512], BF16) for k in range(2)], "bgm", p)
            bsg_r = Ring([sbt(cb, f"bsg{k}", [128, 16, 128], BF16) for k in range(2)], "bsg", p)
            xr_r = Ring([sbt(cb, f"xr{k}", [128, D], F32) for k in range(2)], "xr", p)
            x1_ds = [p.dsem(f"x1d{k}") for k in range(2)]
            t12 = sbt(cb, "t12", [128, 512], F32)
            mT = sbt(cb, "mT", [128, 8, 128], BF16)
            junkb = sbt(cb, "junkb", [128, D], BF16)
            h2f = sbt(cb, "h2f", [128, D], F32)
            h2b_r = Ring([sbt(cb, f"h2b{k}", [128, D], BF16) for k in range(2)], "h2b", p)
            h2T = sbt(cb, "h2T", [128, 8, 128], F32)
            bank5_i = [0]

            def nbank5():
                k = bank5_i[0] % 3
                bank5_i[0] += 1
                return banks[k], f"bank{k}"

            def flat(ap):
                return ap.rearrange("p a b -> p (a b)")

            ntiles_b = NT if stage not in ("B1",) else 3
            TS_ = {}

            def P1_load(i):
                t = TS_[i] = {}
                qlt, qlk, qld = bql_r.next()
                DMA("sync", qld, flat(qlt[:]), QL.ap()[i], [], [qlk])
                qip, qik, qid = bqi_r.next()
                qi_v = QI.ap()[i].rearrange("p (k t) -> p k t", t=128)
                DMA("sync", qid, qip[0:64, 0::2, :], qi_v[0:64, :, :], [], [qik])
                DMA("sync", qid, qip[64:128, 1::2, :], qi_v[64:128, :, :], [], [qik])
                sc, sck = sc_r.next()
                t.update(qlt=qlt, qlk=qlk, qip=qip, qik=qik, sc=sc, sck=sck, idx=idx2[i % 2], idxk=f"idx{i % 2}")

            def out_loads(i):
                t = TS_[i]
                gmt, gmk, gmd = bgm_r.next()
                DMA("sync", gmd, gmt[:], GM.ap()[i], [], [gmk])
                sgt, sgk, sgd = bsg_r.next()
                DMA("sync", sgd, flat(sgt[:]), SG.ap()[i], [], [sgk])
                xres, xk, xd = xr_r.next()
                DMA("sync", xd, xres[:], I["x"].ap()[i * 128:(i + 1) * 128, :], [], [xk])
                t.update(gmt=gmt, gmk=gmk, sgt=sgt, sgk=sgk, xres=xres, xk=xk)

            def idx_units(i):
                S = (i + 1) * 128
                t = TS_[i]
                qip, qik, idx, idxk = t["qip"], t["qik"], t["idx"], t["idxk"]
                Dg, Dgk = Dg_r.next()
                TT("vector", Dg[:], bc_mid(ident_b[:], 8), bc_last(WIall[:, i, :], 128), ALU.mult, ["ident_b", "WIall"], [Dgk])
                units = []
                accb, acck = banks[4], "bank4"
                nch = (S + 511) // 512
                st = {"pend": None}
                for ch in range(nch):
                    w = min(512, S - ch * 512)
                    kkeys = [f"ki{j}" for j in range(ch * 4, ch * 4 + w // 128)]

                    def diag(pend, w=w, last=False):
                        ph, prl, prlk = pend
                        MM(accb[:, 0:w], Dg[:, ph, :], prl[:, 0:w], ph == 0, last, [Dgk, prlk], [acck])

                    for h in range(8):
                        def unit(ch=ch, h=h, w=w, kkeys=kkeys, diag=diag):
                            bk, bkk = nbank5()
                            MM(bk[:, 0:w], qip[:, h, :], kiT2[:, ch * 512: ch * 512 + w], True, True, [qik] + kkeys, [bkk])
                            rl, rlk = rl_r.next()
                            ACT(rl[:, 0:w], bk[:, 0:w], AF.Relu, [bkk], [rlk])
                            if h > 0:
                                diag(st["pend"])
                            st["pend"] = (h, rl, rlk)
                        units.append(unit)

                    def fin(ch=ch, w=w, diag=diag):
                        diag(st["pend"], last=True)
                        COPY("scalar", idx[:, ch * 512: ch * 512 + w], accb[:, 0:w], [acck], [idxk])
                    units.append(fin)
                return units

            def P1_fin(i):
                S = (i + 1) * 128
                t = TS_[i]
                idx, idxk, sc, sck = t["idx"], t["idxk"], t["sc"], t["sck"]
                dg = idx[:, i * 128:(i + 1) * 128]
                TT("vector", dg, dg, cneg[:], ALU.add, [idxk, "cneg"], [idxk])
                lo, hi, wid, cnd, cnt, uu = [sc[:, q:q + 1] for q in range(6)]
                Hx = sc[:, 16:16 + NIT + 1]
                if i >= 2:
                    RED(lo, idx[:, 0:256], ALU.min, [idxk], [sck])
                    RED(hi, idx[:, 0:S], ALU.max, [idxk, sck], [sck])
                    TT("vector", wid, hi, lo, ALU.subtract, [sck], [sck])
                    TS("vector", Hx, pw[:, 0:NIT + 1], wid, None, ALU.mult, None, ["pw", sck], [sck])
                    TT("vector", cnd, lo, Hx[:, 1:2], ALU.add, [sck], [sck])
                else:
                    MEMSET("vector", lo, -1.0e29, [sck])

            def bis(i, k):
                S = (i + 1) * 128
                sc, sck, idx, idxk = TS_[i]["sc"], TS_[i]["sck"], TS_[i]["idx"], TS_[i]["idxk"]
                lo, hi, wid, cnd, cnt, uu = [sc[:, q:q + 1] for q in range(6)]
                Hx = sc[:, 16:16 + NIT + 1]
                TS("vector", mask[:, 0:S], idx[:, 0:S], cnd, None, ALU.is_ge, ALU.add, [idxk, sck], ["mask", sck], accum_out=cnt)
                if k < NIT - 1:
                    TS("vector", uu, cnt, 255.5, Hx[:, k + 1:k + 2], ALU.is_ge, ALU.mult, [sck], [sck])
                    STT(cnd, cnd, Hx[:, k + 2:k + 3], uu, ALU.subtract, ALU.add, [sck], [sck])
                else:
                    TS("vector", uu, cnt, 255.5, Hx[:, NIT:NIT + 1], ALU.is_ge, ALU.mult, [sck], [sck])
                    STT(lo, cnd, Hx[:, NIT:NIT + 1], uu, ALU.subtract, ALU.add, [sck], [sck])

            def P1c(i):
                S = (i + 1) * 128
                sc, sck, idx, idxk = TS_[i]["sc"], TS_[i]["sck"], TS_[i]["idx"], TS_[i]["idxk"]
                TS("vector", mask[:, 0:S], idx[:, 0:S], sc[:, 0:1], None, ALU.is_ge, None, [idxk, sck], ["mask"])

            def P2(i):
                for j0 in range(0, i + 1, 8):
                    nb = min(8, i + 1 - j0)
                    bk, bkk = nbank5()
                    bkb = bk[:].bitcast(BF16)
                    for jj in range(nb):
                        TR(bkb[:, jj * 128:(jj + 1) * 128], mask[:, (j0 + jj) * 128:(j0 + jj + 1) * 128], ident_b[:], ["mask", "ident_b"], [bkk], inc=(jj == nb - 1))
                    COPY("scalar", flat(maskT[:, j0:j0 + nb, :]), bkb[:, 0:nb * 128], [bkk], [f"mT{j}" for j in range(j0, j0 + nb)])
                TT("gpsimd", MnD[:], BD[:], bc_mid(maskT[:, i, :], 8), ALU.mult, [f"BD{h}" for h in range(8)] + [f"mT{i}"], ["MnD"])
                if i >= 1:
                    TT("gpsimd", MnO[:], BO[:], bc_mid(maskT[:, i - 1, :], 8), ALU.mult, [f"BO{h}" for h in range(8)] + [f"mT{i - 1}"], ["MnO"])

            def blk_pro(i):
                for b in range(3):
                    MM(banks[5 + b][:, :], zeros_b[:], ckvT[:, 0:512], True, True, ["zeros_b"] + [f"ckvT{j}" for j in range(4)], [f"bank{5 + b}"])

            PQ = {}

            def blk_j(i, j):
                blk_score(i, j)
                if j >= 3:
                    blk_pv(i, j - 3)

            def blk_score(i, j):
                qlt, qlk = TS_[i]["qlt"], TS_[i]["qlk"]
                Pt, Pk = P_r.next()
                PQ[(i, j)] = (Pt, Pk)
                for g in range(2):
                    bk, bkk = nbank5()
                    MM(bk[:, :], ckvT[:, j * 128:(j + 1) * 128], flat(qlt[:, g * 4:(g + 1) * 4, :]), True, True, [f"ckvT{j}", qlk], [bkk])
                    ACT(flat(Pt[:, g * 4:(g + 1) * 4, :]), bk[:, :], AF.Exp, [bkk], [Pk])
                if j == i:
                    TT("gpsimd", Pt[:], Pt[:], MnD[:], ALU.mult, [Pk, "MnD"], [Pk])
                elif j == i - 1:
                    TT("gpsimd", Pt[:], Pt[:], MnO[:], ALU.mult, [Pk, "MnO"], [Pk])
                else:
                    TT("gpsimd", Pt[:], Pt[:], bc_mid(maskT[:, j, :], 8), ALU.mult, [Pk, f"mT{j}"], [Pk])

            def blk_pv(i, j):
                Pt, Pk = PQ.pop((i, j))
                for h in range(8):
                    b = 5 + h // 3
                    MM(banks[b][:, (h % 3) * 130:(h % 3) * 130 + 129], Pt[:, h, :], Vaug[:, j, 0:129], False, False,
                       [Pk, f"Vaug{j}", "Vaug_ones"], [f"bank{b}"], inc=(h == 7), skip=True)

            def blk_epi(i):
                for jj in range(max(0, i - 2), i + 1):
                    blk_pv(i, jj)
                for b in range(3):
                    nb = 3 if b < 2 else 2
                    RECIP(rden[:, 3 * b:3 * b + nb], banks[5 + b][:, 128:128 + 130 * (nb - 1) + 1:130], [f"bank{5 + b}"], ["rden"])
                for h in range(8):
                    b = 5 + h // 3
                    ACT(OL[:, h, :], banks[b][:, (h % 3) * 130:(h % 3) * 130 + 128], AF.Copy, [f"bank{b}", "rden"], ["OL"], scale=rden[:, h:h + 1])
                bk, bkk = nbank5()
                bkb = bk[:].bitcast(BF16)
                for h in range(8):
                    TR(bkb[:, h * 128:(h + 1) * 128], OL[:, h, :], ident_b[:], ["OL", "ident_b"], [bkk], inc=(h == 7))
                COPY("scalar", flat(OLT[:]), bkb[:, 0:1024], [bkk], ["OLT"])
                bk, bkk = nbank5()
                for k in range(4):
                    for hh in range(2):
                        MM(bk[:, k * 128:(k + 1) * 128], Wuv[:, 2 * k + hh, :], OLT[:, 2 * k + hh, :], hh == 0, hh == 1, ["Wuv", "OLT"], [bkk], inc=(k == 3 and hh == 1))
                COPY("scalar", flat(oaT[:]), bk[:, :], [bkk], ["oaT"])

            def out_stages(i):
                t = TS_[i]
                gmt, gmk, sgt, sgk, xres, xk, sc, sck = t["gmt"], t["gmk"], t["sgt"], t["sgk"], t["xres"], t["xk"], t["sc"], t["sck"]
                hb = {}

                B3, B3k = banks[3], "bank3"

                def yq(q):
                    for mm in range(2):
                        m = 2 * q + mm
                        for kc in range(4):
                            MM(B3[:, mm * 128:(mm + 1) * 128], Wpa[:, kc, m * 128:(m + 1) * 128], oaT[:, kc, :], kc == 0, kc == 3, ["Wpa", "oaT"], [B3k], inc=False)
                    for mm in range(2):
                        m = 2 * q + mm
                        for kc in range(4):
                            MM(B3[:, 256 + mm * 128:256 + (mm + 1) * 128], Wpb[:, kc, m * 128:(m + 1) * 128], gmt[:, kc * 128:(kc + 1) * 128], kc == 0, kc == 3, ["Wpb", gmk], [B3k], inc=(kc == 3 and mm == 1))

                def gq(q):
                    sgv = sgt[:].rearrange("p (a g) t -> p a g t", a=2)[:, :, 2 * q:2 * q + 2, :]
                    TT("vector", t12[:].rearrange("p (a g t) -> p a g t", a=2, g=2), B3[:, :].rearrange("p (a g t) -> p a g t", a=2, g=2), sgv,
                       ALU.mult, [B3k, sgk], ["t12"])
                    TT("gpsimd", flat(mT[:, 2 * q:2 * q + 2, :]), t12[:, 0:256], t12[:, 256:512], ALU.add, ["t12"], ["mT"])

                def wout(half):
                    for c in range(8):
                        MM(B3[:, :], mT[:, c, :], Wout[:, c, half * 512:(half + 1) * 512], c == 0, c == 7, ["mT", "Wout"], [B3k], inc=(c == 7))

                def xadd(half):
                    xs_ = xres[:, half * 512:(half + 1) * 512]
                    TT("vector", xs_, B3[:, :], xs_, ALU.add, [B3k, xk], [xk])

                def norm2():
                    DMA("sync", x1_ds[i % 2], X1.ap()[i * 128:(i + 1) * 128, :], xres[:], [xk], [])
                    ACT(junkb[:], xres[:], AF.Square, [xk], ["junkb", sck + "n"], accum_out=sc[:, 40:41])
                    rstd_from_ss(sc[:, 42:43], sc[:, 40:41], 1.0 / D, sc[:, 41:42], sck + "n3", sck + "n", sck + "n2")
                    STT(h2f[:], xres[:], sc[:, 42:43], g2B[:], ALU.mult, ALU.mult, [xk, sck + "n3", "g2B"], ["h2f"])
                    h2b, h2bk, h2bd = h2b_r.next()
                    COPY("scalar", h2b[:], h2f[:], ["h2f"], [h2bk])
                    DMA("sync", h2bd, H2.ap()[i * 128:(i + 1) * 128, :], h2b[:], [h2bk], [])

                def trh(half):
                    for cc in range(4):
                        c = half * 4 + cc
                        TR(B3[:, cc * 128:(cc + 1) * 128], h2f[:, c * 128:(c + 1) * 128], ident_f[:], ["h2f", "ident_f"], [B3k], inc=(cc == 3))
                    COPY("scalar", flat(h2T[:, half * 4:(half + 1) * 4, :]), B3[:, :], [B3k], ["h2T"])

                def rmm():
                    for c in range(8):
                        MM(B3[:, 0:36], h2T[:, c, :], Wr[:, c, :], c == 0, c == 7, ["h2T", "Wr"], [B3k], inc=(c == 7))
                    hb[20] = (B3, B3k)

                stages = [
                    lambda: yq(0),
                    lambda: (gq(0), yq(1)),
                    lambda: (gq(1), yq(2)),
                    lambda: (gq(2), yq(3)),
                    lambda: gq(3),
                    lambda: wout(0),
                    lambda: (xadd(0), wout(1)),
                    lambda: (xadd(1), norm2()),
                    lambda: trh(0),
                    lambda: trh(1),
                    lambda: rmm(),
                ]

                def s7():
                    bk, bkk = hb[20]
                    TT("vector", LG[:, i, :], bk[:, 0:36], rbias[:], ALU.add, [bkk, "rbias"], ["LG"])

                return stages + [s7]

            def run_units(us):
                for u in us:
                    u()

            for i0 in range(min(2, ntiles_b)):
                P1_load(i0)
                run_units(idx_units(i0))
                P1_fin(i0)
            P1c(0)
            P2(0)
            web_ds = {wn: p.dsem() for wn in ("w_gate", "w_up", "w_down")}
            for it in range(ntiles_b):
                for wq, wn in enumerate(("w_gate", "w_up", "w_down")):
                    DMA("gpsimd", web_ds[wn], WEB.ap()[it * 128:(it + 1) * 128, wq * 2048:(wq + 1) * 2048], I[wn].ap()[it * 128:(it + 1) * 128, :], [], [])
                if it >= 1:
                    out_loads(it - 1)
                U = []
                if it + 2 < ntiles_b:
                    P1_load(it + 2)
                    U = idx_units(it + 2)
                nxt = it + 1 < ntiles_b
                blk_pro(it)
                nb = it + 1
                nk = NIT if (nxt and it + 1 >= 2) else 0
                ost = out_stages(it - 1) if it >= 1 else []
                NS = 12
                Tn = max(nb, nk, NS)
                nu = len(U)
                for tck in range(Tn):
                    for k in range((tck * nk) // Tn, ((tck + 1) * nk) // Tn):
                        bis(it + 1, k)
                    for j in range((tck * nb) // Tn, ((tck + 1) * nb) // Tn):
                        blk_j(it, j)
                    for q in range((tck * nu) // Tn, ((tck + 1) * nu) // Tn):
                        U[q]()
                    for si, st_fn in enumerate(ost):
                        if (si * Tn) // NS == tck:
                            st_fn()
                if nxt:
                    P1c(it + 1)
                    P2(it + 1)
                blk_epi(it)
                if it + 2 < ntiles_b:
                    P1_fin(it + 2)
            out_loads(ntiles_b - 1)
            for st_fn in out_stages(ntiles_b - 1):
                st_fn()
            if debug and stage in ("B", "B1"):
                ddbg = p.dsem("ddbg2")
                p.barrier()
                DMA("gpsimd", ddbg, DBG.ap()[6][:, 0:NT], W1all[:], [], [])
                DMA("gpsimd", ddbg, DBG.ap()[7][:, 0:NT], W2all[:], [], [])
                DMA("gpsimd", ddbg, DBG.ap()[8][:, 0:1024], flat(OH1all[:]), [], [])
                DMA("gpsimd", ddbg, DBG.ap()[9][:, 0:1024], flat(OH2all[:]), [], [])
            p.barrier()
            p.recycle()
            p.emit()
        if stage in ("B", "B1"):
            return nc

        with ExitStack() as cr:
            gl = LG[:, :, 0:4]
            el4 = LG[:, :, 4:36].rearrange("p i (g j) -> p i g j", j=8)
            r_gmax = sbt(cr, "r_gmax", [128, NT], F32)
            r_gsh = sbt(cr, "r_gsh", [128, NT, 4], F32)
            r_ge = sbt(cr, "r_ge", [128, NT, 4], F32)
            r_gsum = sbt(cr, "r_gsum", [128, NT], F32)
            r_gw = sbt(cr, "r_gw", [128, NT], F32)
            r_goh = sbt(cr, "r_goh", [128, NT, 4], F32)
            r_gpen = sbt(cr, "r_gpen", [128, NT, 4], F32)
            r_elm = sbt(cr, "r_elm", [128, NT, 32], F32)
            r_elm2 = sbt(cr, "r_elm2", [128, NT, 32], F32)
            r_m1 = sbt(cr, "r_m1", [128, NT], F32)
            r_m2 = sbt(cr, "r_m2", [128, NT], F32)
            r_dd = sbt(cr, "r_dd", [128, NT], F32)
            r_ee = sbt(cr, "r_ee", [128, NT], F32)
            r_s1 = sbt(cr, "r_s1", [128, NT], F32)
            RED(r_gmax[:], gl, ALU.max, ["LG"], ["r_gmax"])
            TT("vector", r_gsh[:], gl, bc_last(r_gmax[:], 4), ALU.subtract, ["LG", "r_gmax"], ["r_gsh"])
            ACT(r_ge[:], r_gsh[:], AF.Exp, ["r_gsh"], ["r_ge"])
            RED(r_gsum[:], r_ge[:], ALU.add, ["r_ge"], ["r_gsum"])
            RECIP(r_gw[:], r_gsum[:], ["r_gsum"], ["r_gw"])
            TS("vector", r_goh[:], r_gsh[:], 0.0, None, ALU.is_ge, None, ["r_gsh"], ["r_goh"])
            TS("vector", r_gpen[:], r_goh[:], 1.0, BIG, ALU.subtract, ALU.mult, ["r_goh"], ["r_gpen"])
            TT("vector", r_elm[:].rearrange("p i (g j) -> p i g j", j=8), el4, bc_last(r_gpen[:], 8), ALU.add, ["LG", "r_gpen"], ["r_elm"])
            RED(r_m1[:], r_elm[:], ALU.max, ["r_elm"], ["r_m1"])
            TT("vector", OH1all[:], r_elm[:], bc_last(r_m1[:], 32), ALU.is_ge, ["r_elm", "r_m1"], ["OH1all"])
            STT(r_elm2[:], OH1all[:], -BIG, r_elm[:], ALU.mult, ALU.add, ["OH1all", "r_elm"], ["r_elm2"])
            RED(r_m2[:], r_elm2[:], ALU.max, ["r_elm2"], ["r_m2"])
            TT("vector", OH2all[:], r_elm2[:], bc_last(r_m2[:], 32), ALU.is_ge, ["r_elm2", "r_m2"], ["OH2all"])
            TT("vector", r_dd[:], r_m2[:], r_m1[:], ALU.subtract, ["r_m1", "r_m2"], ["r_dd"])
            ACT(r_ee[:], r_dd[:], AF.Exp, ["r_dd"], ["r_ee"])
            TS("vector", r_ee[:], r_ee[:], 1.0, None, ALU.add, None, ["r_ee"], ["r_ee"])
            RECIP(r_s1[:], r_ee[:], ["r_ee"], ["r_s1"])
            TT("vector", W1all[:], r_gw[:], r_s1[:], ALU.mult, ["r_gw", "r_s1"], ["W1all"])
            TT("vector", W2all[:], r_gw[:], W1all[:], ALU.subtract, ["r_gw", "W1all"], ["W2all"])
            TT("vector", MASKall[:], OH1all[:], OH2all[:], ALU.add, ["OH1all", "OH2all"], ["MASKall"])
            p.barrier()
            p.recycle()
            p.emit()

        with ExitStack() as cd:
            tokid = sbt(cd, "tokid", [128, NT, 16], I32)
            pcol = sbt(cd, "pcol", [128, 1], F32)
            j128 = sbt(cd, "j128", [128, NSLOT_T], F32)
            gfB = sbt(cd, "gfB", [128, D], F32)
            LOAD("sync", tokid[:], I["c_tokid"].ap(), "tokid")
            LOAD("sync", pcol[:], I["c_pcol"].ap(), "pcol")
            LOAD("sync", j128[:], I["c_j128"].ap(), "j128")
            LOAD("sync", gfB[:], bass.AP(I["final_norm_g"], 0, [[0, 128], [1, D]]), "gfB")
            for i in range(NT):
                bk = banks[i // 16]
                col = (i % 16) * 32
                MM(bk[:, col:col + 32], tri_b[:], MASKall[:, i, :], True, i == 0, ["tri_b", "MASKall"], [f"bank{i // 16}"], inc=(i == 0), skip=True)
                for i2 in range(i):
                    MM(bk[:, col:col + 32], ones_b[:], MASKall[:, i2, :], False, i2 == i - 1, ["ones_b", "MASKall"], [f"bank{i // 16}"], inc=(i2 == i - 1), skip=True)
            for i in range(NT):
                MM(banks[2][:, 0:32], ones_b[:], MASKall[:, i, :], i == 0, i == NT - 1, ["ones_b", "MASKall"], ["bank2"], inc=(i == NT - 1), skip=True)
            ci = sbt(cd, "ci", [128, 32], I32)
            padf = sbt(cd, "padf", [128, 32], F32)
            pa = sbt(cd, "pa", [128, 32], F32)
            pb = sbt(cd, "pb", [128, 32], F32)
            bm1 = sbt(cd, "bm1", [128, 32], F32)
            TS("vector", ci[:], banks[2][:, 0:32], 127.0, None, ALU.add, None, ["bank2"], ["ci"])
            p.op("vector", lambda e: e.tensor_scalar(out=ci[:], in0=ci[:], scalar1=7, scalar2=7, op0=ALU.logical_shift_right, op1=ALU.logical_shift_left), ["ci"], ["ci"])
            COPY("vector", padf[:], ci[:], ["ci"], ["padf"])
            COPY("vector", pa[:], padf[:], ["padf"], ["pa"])
            src, srck, dst, dstk = pa, "pa", pb, "pb"
            for sft in (1, 2, 4, 8, 16):
                COPY("vector", dst[:, 0:sft], src[:, 0:sft], [srck], [dstk])
                TT("vector", dst[:, sft:32], src[:, sft:32], src[:, 0:32 - sft], ALU.add, [srck], [dstk])
                src, srck, dst, dstk = dst, dstk, src, srck
            incl, inclk = src, srck
            TT("vector", bm1[:], incl[:], padf[:], ALU.subtract, [inclk, "padf"], ["bm1"])
            TS("vector", bm1[:], bm1[:], -1.0, None, ALU.add, None, ["bm1"], ["bm1"])
            tmpAll = sbt(cd, "tmpAll", [128, NT, 32], F32)
            prod = sbt(cd, "prod", [128, NT, 32], F32)
            Sf = sbt(cd, "Sf", [128, 2, NT], F32)
            Si = sbt(cd, "Si", [128, 2, NT], I32)
            for b in range(2):
                TT("vector", tmpAll[:, b * 16:(b + 1) * 16, :], banks[b][:, :].rearrange("p (a b) -> p a b", b=32), bc_mid(bm1[:], 16), ALU.add, [f"bank{b}", "bm1"], ["tmpAll"])
            for k, OH in enumerate((OH1all, OH2all)):
                TT("vector", prod[:], tmpAll[:], OH[:], ALU.mult, ["tmpAll", "OH1all", "OH2all"], ["prod"])
                RED(Sf[:, k, :], prod[:], ALU.add, ["prod"], ["Sf"])
            COPY("vector", Si[:], Sf[:], ["Sf"], ["Si"])
            cmp = sbt(cd, "cmp", [128, NSLOT_T, 32], F32)
            texp = sbt(cd, "texp", [128, NSLOT_T], F32)
            IDXW = sbt(cd, "IDXW", [128, NSLOT_T], I32)
            TT("vector", cmp[:], bc_mid(incl[:], NSLOT_T), bc_last(j128[:], 32), ALU.is_le, [inclk, "j128"], ["cmp"])
            RED(texp[:], cmp[:], ALU.add, ["cmp"], ["texp"])
            TS("vector", texp[:], texp[:], 31.0, 128.0, ALU.min, ALU.mult, ["texp"], ["texp"])
            TS("vector", texp[:], texp[:], pcol[:, 0:1], None, ALU.add, None, ["texp", "pcol"], ["texp"])
            COPY("vector", IDXW[:], texp[:], ["texp"], ["IDXW"])
            si = sbt(cd, "si", [128, 16], I32)
            zrow = sbt(cd, "zrow", [128, D], BF16)
            MEMSET("gpsimd", si[:], T, ["si"])
            MEMSET("gpsimd", zrow[:], 0.0, ["zrow"])
            LOAD("sync", SLOT.ap().rearrange("(j p) c -> p j c", p=128), bc_mid(si[:], NSLOT_T), "SLOTinit", reads=["si"])
            LOAD("sync", H2.ap()[T:T + 128, :], zrow[:], "H2zero", reads=["zrow"])
            sc_ds = [p.dsem(f"scat{k}") for k in range(4)]
            sckeys = []
            n = 0
            for i in range(NT):
                for k in range(2):
                    SCATTER(sc_ds[n % 4], SLOT.ap(), Si[:, k, i:i + 1], tokid[:, i, :], ["Si", "tokid", "SLOTinit"], [f"SLOTs{n}"])
                    sckeys.append(f"SLOTs{n}")
                    n += 1
            stk_r = Ring([sbt(cd, f"stk{k}", [128, 16], I32) for k in range(4)], "stk", p)
            Hs_r = Ring([sbt(cd, f"Hs{k}", [128, D], BF16) for k in range(4)], "Hs", p)
            Wa_r = Ring([sbt(cd, f"Wa{k}", [128, 6144], BF16) for k in range(5)], "Wa", p)
            yt_r = Ring([sbt(cd, f"yt{k}", [128, D], F32) for k in range(2)], "yt", p)
            bank8_i = [0]

            def nbank8():
                k = bank8_i[0] % 8
                bank8_i[0] += 1
                return banks[k], f"bank{k}"

            nslot_t = NSLOT_T
            yskeys = []
            dq = {}

            def d_loads(j):
                stk, stkk, stkd = stk_r.next()
                DMA("sync", stkd, stk[:], SLOT.ap()[j * 128:(j + 1) * 128, :], sckeys + ["SLOTinit"], [stkk])
                Hs, Hsk, Hsd = Hs_r.next()
                GATHER(Hsd, Hs[:], H2.ap(), stk[:, 0:1], [stkk, "H2zero"], [Hsk])
                Wa, Wak, Wad = Wa_r.next()
                GATHER(Wad, Wa[:], WEB.ap(), IDXW[:, j:j + 1], ["IDXW"], [Wak])
                dq[j] = (Hs, Hsk, Wa[:, 0:2048], Wak, Wa[:, 2048:4096], Wak, Wa[:, 4096:6144], Wak)

            HsT_r = Ring([sbt(cd, f"HsT{k}", [128, 8, 128], BF16) for k in range(2)], "HsT")
            he_r = Ring([sbt(cd, f"he{k}", [128, 256], BF16) for k in range(2)], "he")
            heT_r = Ring([sbt(cd, f"heT{k}", [128, 256], BF16) for k in range(2)], "heT")
            sgs_r = Ring([sbt(cd, f"sgs{k}", [128, 256], F32) for k in range(2)], "sgs")
            DS = {}

            def sA(j):
                Hs, Hsk = dq[j][0], dq[j][1]
                bk, bkk = nbank8()
                bkb = bk[:].bitcast(BF16)
                for c in range(8):
                    TR(bkb[:, c * 128:(c + 1) * 128], Hs[:, c:D:8], ident_b[:], [Hsk, "ident_b"], [bkk], inc=(c == 7))
                HsT_, HsTk = HsT_r.next()
                COPY("scalar", flat(HsT_[:]), bkb[:, 0:1024], [bkk], [HsTk])
                DS[j] = {"HsT": (HsT_, HsTk)}

            def sB(j):
                _, _, Wg, Wgk, Wu, Wuk_, _, _ = dq[j]
                HsT_, HsTk = DS[j]["HsT"]
                bk, bkk = nbank8()
                for c in range(8):
                    MM(bk[:, 0:256], HsT_[:, c, :], Wg[:, c * 256:(c + 1) * 256], c == 0, c == 7, [Wgk, HsTk], [bkk], inc=False)
                for c in range(8):
                    MM(bk[:, 256:512], HsT_[:, c, :], Wu[:, c * 256:(c + 1) * 256], c == 0, c == 7, [Wuk_, HsTk], [bkk], inc=(c == 7))
                sgs_, sgsk = sgs_r.next()
                ACT(sgs_[:], bk[:, 0:256], AF.Silu, [bkk], [sgsk])
                he_, hek = he_r.next()
                TT("vector", he_[:], sgs_[:], bk[:, 256:512], ALU.mult, [sgsk, bkk], [hek])
                DS[j]["he"] = (he_, hek)

            def sC(j):
                he_, hek = DS[j]["he"]
                bk2, bk2k = nbank8()
                bk2b = bk2[:].bitcast(BF16)
                for c2 in range(2):
                    TR(bk2b[:, c2 * 128:(c2 + 1) * 128], he_[:, c2:256:2], ident_b[:], [hek, "ident_b"], [bk2k], inc=(c2 == 1))
                heT_, heTk = heT_r.next()
                COPY("vector", heT_[:], bk2b[:, 0:256], [bk2k], [heTk])
                DS[j]["heT"] = (heT_, heTk)

            def sD(j):
                Wd, Wdk = dq[j][6], dq[j][7]
                heT_, heTk = DS[j]["heT"]
                yt, ytk, ytd = yt_r.next()
                for half in range(2):
                    bk, bkk = nbank8()
                    for c2 in range(2):
                        MM(bk[:, :], heT_[:, c2 * 128:(c2 + 1) * 128], Wd[:, c2 * 1024 + half * 512:c2 * 1024 + (half + 1) * 512], c2 == 0, c2 == 1, [heTk, Wdk], [bkk], inc=(c2 == 1))
                    COPY("scalar" if half == 0 else "vector", yt[:, half * 512:(half + 1) * 512], bk[:, :], [bkk], [ytk])
                DMA("sync", ytd, YS.ap()[j * 128:(j + 1) * 128, :], yt[:], [ytk], [f"YS{j}"])
                yskeys.append(f"YS{j}")
                dq.pop(j)
                DS.pop(j)

            d_loads(0)
            d_loads(1)
            for j in range(nslot_t + 2):
                if j + 2 < nslot_t:
                    d_loads(j + 2)
                if 0 <= j - 2 < nslot_t:
                    sC(j - 2)
                if j < nslot_t:
                    sA(j)
                if 0 <= j - 1 < nslot_t:
                    sB(j - 1)
                if 0 <= j - 2 < nslot_t:
                    sD(j - 2)
            x1_r = Ring([sbt(cd, f"ex{k}", [128, D], F32) for k in range(3)], "ex", p)
            y1_r = Ring([sbt(cd, f"ey1{k}", [128, D], F32) for k in range(3)], "ey1", p)
            y2_r = Ring([sbt(cd, f"ey2{k}", [128, D], F32) for k in range(3)], "ey2", p)
            ot_r = Ring([sbt(cd, f"ot{k}", [128, D], F32) for k in range(2)], "ot", p)
            es_r = Ring([sbt(cd, f"es{k}", [128, 4], F32) for k in range(2)], "es")
            junke = sbt(cd, "junke", [128, D], BF16)
            eq = {}

            def e_loads(i):
                ex, exk, exd = x1_r.next()
                DMA("sync", exd, ex[:], X1.ap()[i * 128:(i + 1) * 128, :], [], [exk])
                y1, y1k, y1d = y1_r.next()
                GATHER(y1d, y1[:], YS.ap(), Si[:, 0, i:i + 1], ["Si"] + yskeys, [y1k])
                y2, y2k, y2d = y2_r.next()
                GATHER(y2d, y2[:], YS.ap(), Si[:, 1, i:i + 1], ["Si"] + yskeys, [y2k])
                eq[i] = (ex, exk, y1, y1k, y2, y2k)

            e_loads(0)
            e_loads(1)
            for i in range(NT):
                if i + 2 < NT:
                    e_loads(i + 2)
                ex, exk, y1, y1k, y2, y2k = eq.pop(i)
                STT(ex[:], y1[:], W1all[:, i:i + 1], ex[:], ALU.mult, ALU.add, [y1k, exk, "W1all"], [exk])
                STT(ex[:], y2[:], W2all[:, i:i + 1], ex[:], ALU.mult, ALU.add, [y2k, exk, "W2all"], [exk])
                es, esk = es_r.next()
                ACT(junke[:], ex[:], AF.Square, [exk], ["junke", esk + "a"], accum_out=es[:, 0:1])
                rstd_from_ss(es[:, 2:3], es[:, 0:1], 1.0 / D, es[:, 1:2], esk + "c", esk + "a", esk + "b")
                ot, otk, otd = ot_r.next()
                STT(ot[:], ex[:], es[:, 2:3], gfB[:], ALU.mult, ALU.mult, [exk, esk + "c", "gfB"], [otk])
                DMA("sync", otd, out_d.ap()[i * 128:(i + 1) * 128, :], ot[:], [otk], [])
            p.barrier()
            p.recycle()
            p.emit()
    return nc


def make_in_maps(inputs):
    consts = host_consts()
    shared = {}
    f = lambda a: np.ascontiguousarray(np.asarray(a, dtype=np.float32))
    shared["w_in"] = f(inputs["w_in"][0])
    shared["kv_norm_g"] = f(inputs["kv_norm_g"]).reshape(1, 128)
    shared["w_uk"] = f(inputs["w_uk"][0]).reshape(128, 512)
    shared["w_uv"] = f(inputs["w_uv"][0]).reshape(128, 512)
    shared["rel_bias"] = f(inputs["rel_bias"])
    shared["ln_v_g"] = f(inputs["ln_v_g"]).reshape(1, 512)
    shared["ln_v_b"] = f(inputs["ln_v_b"]).reshape(1, 512)
    shared["w_spatial"] = f(inputs["w_spatial"][0])
    shared["b_spatial"] = f(inputs["b_spatial"][0]).reshape(1, 512)
    shared["w_proj_a"] = f(inputs["w_proj_a"][0])
    shared["w_proj_b"] = f(inputs["w_proj_b"][0])
    shared["w_out"] = f(inputs["w_out"][0])
    shared["norm1_g"] = f(f(inputs["norm1_g"][0]).reshape(8, 128).T)
    shared["norm2_g"] = f(inputs["norm2_g"]).reshape(1, D)
    shared["router_group_w"] = f(inputs["router_group_w"][0])
    shared["router_group_b"] = f(inputs["router_group_b"]).reshape(1, 4)
    shared["router_expert_w"] = f(inputs["router_expert_w"][0])
    shared["router_expert_b"] = f(inputs["router_expert_b"]).reshape(1, 32)
    shared["w_gate"] = f(inputs["w_gate"][0]).reshape(32 * 128, 2048)
    shared["w_up"] = f(inputs["w_up"][0]).reshape(32 * 128, 2048)
    shared["w_down"] = f(inputs["w_down"][0]).reshape(32 * 128, 2048)
    shared["final_norm_g"] = f(inputs["final_norm_g"]).reshape(1, D)
    shared.update(consts)
    x = f(inputs["x"])
    return [dict(shared, x=x[b]) for b in range(8)]


def kernel(**inputs):
    nc = build_program("E")
    in_maps = make_in_maps(inputs)
    res = run_bass_kernel_spmd(nc, in_maps, core_ids=list(range(8)))
    return np.stack([r["out"] for r in res.results], axis=0).astype(np.float32)
```

```python
import os
from contextlib import ExitStack
import numpy as np
import concourse.bass as bass
import concourse.mybir as mybir
from concourse.bass_utils import run_bass_kernel_spmd

F32 = mybir.dt.float32
BF16 = mybir.dt.bfloat16
I32 = mybir.dt.int32
AF = mybir.ActivationFunctionType
ALU = mybir.AluOpType
AX = mybir.AxisListType

ENGS = ("tensor", "vector", "scalar", "gpsimd", "sync")

T = 4096
D = 1024
NT = 32
P = 128
NCOLS = 4296
C_Q, C_CKV, C_QI, C_KI, C_WI, C_U, C_V, C_GA, C_GB = 0, 512, 640, 1152, 1216, 1224, 1736, 2248, 3272
NSLOT_T = 96
NSLOT = NSLOT_T * 128
EPS = 1e-6
NIT = int(os.environ.get("K_NIT", "14"))
BIG = 1.0e4


class DSem:
    def __init__(self, prog, name):
        self.sem = prog.ctx.enter_context(prog.nc.semaphore(name))
        self.count = 0
        self.id = name


class Prog:
    def __init__(self, nc, ctx):
        self.nc = nc
        self.ctx = ctx
        self.ops = {e: [] for e in ENGS}
        self.cnt = {e: 0 for e in ENGS}
        self.esem = {e: ctx.enter_context(nc.semaphore("es_" + e)) for e in ENGS}
        self.seen = {e: {} for e in ENGS}
        self.lastw = {}
        self.readers = {}
        self.dsems = []
        self.free = []
        self.inuse = []
        self.pending_noinc = {e: False for e in ENGS}

    def dsem(self, name=None):
        if self.free:
            d = self.free.pop()
        else:
            d = DSem(self, f"ds{len(self.dsems)}")
            self.dsems.append(d)
        self.inuse.append(d)
        return d

    def recycle(self):
        self.free.extend(self.inuse)
        self.inuse = []

    def _need(self, eng, ev, waits):
        sem, val, sid, src = ev
        if self.seen[eng].get(sid, 0) >= val:
            return
        if sid in waits:
            val = max(val, waits[sid][1])
        waits[sid] = (sem, val)

    def _collect(self, eng, reads, writes):
        waits = {}
        for r in reads:
            ev = self.lastw.get(r)
            if ev is not None:
                if ev[3] == eng and eng == "tensor":
                    continue
                self._need(eng, ev, waits)
        for w in writes:
            ev = self.lastw.get(w)
            if ev is not None and ev[3] != eng:
                self._need(eng, ev, waits)
            for ev in self.readers.get(w, ()):
                if ev[3] == eng:
                    continue
                self._need(eng, ev, waits)
        for sid, (sem, val) in waits.items():
            self.seen[eng][sid] = val
        return list(waits.values())

    def _record(self, ev, reads, writes):
        for r in reads:
            self.readers.setdefault(r, []).append(ev)
        for w in writes:
            self.lastw[w] = ev
            self.readers[w] = []

    def op(self, eng, fn, reads=(), writes=(), inc=True):
        waits = self._collect(eng, reads, writes)
        if inc:
            self.cnt[eng] += 1
            val = self.cnt[eng]
            self.pending_noinc[eng] = False
        else:
            val = self.cnt[eng] + 1
            self.pending_noinc[eng] = True
        ev = (self.esem[eng], val, "es_" + eng, eng)
        self._record(ev, reads, writes)
        self.ops[eng].append((waits, fn, (self.esem[eng], 1) if inc else None))
        return ev

    def dma(self, queue, ds, fn, reads=(), writes=()):
        waits = self._collect(queue, reads, writes)
        ds.count += 16
        ev = (ds.sem, ds.count, ds.id, "dma")
        self._record(ev, reads, writes)
        self.ops[queue].append((waits, fn, (ds.sem, 16)))
        return ev

    def barrier(self):
        for e in ENGS:
            waits = {}
            for e2 in ENGS:
                if e2 != e and self.cnt[e2] > 0:
                    self._need(e, (self.esem[e2], self.cnt[e2], "es_" + e2, e2), waits)
            for d in self.dsems:
                if d.count > 0:
                    self._need(e, (d.sem, d.count, d.id, "dma"), waits)
            for sid, (sem, val) in waits.items():
                self.seen[e][sid] = val
            if waits:
                self.ops[e].append((list(waits.values()), None, None))

    def emit(self):
        nc = self.nc
        for e in ENGS:
            assert not self.pending_noinc[e], f"engine {e} ends with non-inc op"
            assert self.cnt[e] < 60000, (e, self.cnt[e])
        with nc.Block() as block:
            for e in ENGS:
                ops = self.ops[e]

                def body(h, ops=ops):
                    for waits, fn, inc in ops:
                        for sem, val in waits:
                            h.wait_ge(sem, val)
                        if fn is None:
                            continue
                        ins = fn(h)
                        if inc is not None:
                            ins.then_inc(inc[0], inc[1])

                getattr(block, e)(body)
        self.ops = {e: [] for e in ENGS}


class Ring:
    def __init__(self, tiles, name, prog=None):
        self.tiles = tiles
        self.name = name
        self.i = 0
        self.ds = [prog.dsem(f"{name}_d{k}") for k in range(len(tiles))] if prog else None

    def next(self):
        k = self.i % len(self.tiles)
        self.i += 1
        if self.ds:
            return self.tiles[k], f"{self.name}{k}", self.ds[k]
        return self.tiles[k], f"{self.name}{k}"


def bc_mid(ap, n):
    a = [list(x) for x in ap.ap]
    return bass.AP(ap.tensor, ap.offset, [a[0], [0, n]] + a[1:])


def bc_last(ap, n):
    a = [list(x) for x in ap.ap]
    return bass.AP(ap.tensor, ap.offset, a + [[0, n]])


def t5_bucket_np(n):
    n = np.asarray(n, dtype=np.int32)
    nf = np.maximum(n, 1).astype(np.float32)
    large = 16 + (np.log(nf / np.float32(16)) / np.float32(np.log(128 / 16)) * np.float32(16)).astype(np.int32)
    large = np.minimum(large, 31)
    return np.where(n < 16, n, large)


def host_consts():
    c = {}
    c["c_ident"] = np.eye(128, dtype=np.float32)
    k = np.arange(128)
    c["c_tri"] = (k[:, None] <= k[None, :]).astype(np.float32)
    c["c_cneg"] = np.where(k[None, :] <= k[:, None], 0.0, -1.0e30).astype(np.float32)
    oh = np.zeros((33, 383), np.float32)
    for npr in range(383):
        n = npr - 127
        if n < 0:
            oh[32, npr] = 1.0
        else:
            oh[int(t5_bucket_np(n)), npr] = 1.0
    c["c_oh33"] = oh
    mc = np.zeros((32, 33), np.float32)
    for m in range(32):
        mc[m, m] += 1.0
        mc[31, m] -= 1.0
    c["c_mc"] = mc
    addc = np.zeros((33, 1), np.float32)
    addc[32, 0] = -30000.0
    c["c_addc"] = addc
    c["c_tokid"] = (np.arange(32)[None, :, None] * 128 + np.arange(128)[:, None, None] + np.zeros((1, 1, 16))).astype(np.int32)
    c["c_pcol"] = np.arange(128, dtype=np.float32)[:, None].copy()
    c["c_j128"] = (np.zeros((128, 1)) + np.arange(NSLOT_T)[None, :] * 128.0).astype(np.float32)
    c["c_pw"] = (np.zeros((128, 1)) + (0.5 ** np.arange(32))[None, :]).astype(np.float32)
    return c


CONST_SHAPES = {"c_ident": ([128, 128], F32), "c_tri": ([128, 128], F32), "c_cneg": ([128, 128], F32),
                "c_oh33": ([33, 383], F32), "c_mc": ([32, 33], F32), "c_addc": ([33, 1], F32),
                "c_tokid": ([128, 32, 16], I32), "c_pcol": ([128, 1], F32), "c_j128": ([128, NSLOT_T], F32), "c_pw": ([128, 32], F32)}

IN_SHAPES = {
    "x": [T, D], "w_in": [D, NCOLS], "kv_norm_g": [1, 128], "w_uk": [128, 512], "w_uv": [128, 512],
    "rel_bias": [32, 8], "ln_v_g": [1, 512], "ln_v_b": [1, 512], "w_spatial": [4, 128, 128], "b_spatial": [1, 512],
    "w_proj_a": [512, D], "w_proj_b": [512, D], "w_out": [D, D], "norm1_g": [128, 8], "norm2_g": [1, D],
    "router_group_w": [D, 4], "router_group_b": [1, 4], "router_expert_w": [D, 32], "router_expert_b": [1, 32],
    "w_gate": [32 * 128, 2048], "w_up": [32 * 128, 2048], "w_down": [32 * 128, 2048], "final_norm_g": [1, D],
}


def build_program(stage="E", debug=False):
    nc = bass.Bass("TRN2", target_bir_lowering=False)
    I = {k: nc.dram_tensor(k, s, F32, kind="ExternalInput") for k, s in IN_SHAPES.items()}
    for k, (s, dt) in CONST_SHAPES.items():
        I[k] = nc.dram_tensor(k, s, dt, kind="ExternalInput")
    out_d = nc.dram_tensor("out", [T, D], F32, kind="ExternalOutput")
    skind = "ExternalOutput" if debug else "Internal"
    QL = nc.dram_tensor("s_ql", [NT, 128, 1024], BF16, kind=skind)
    QI = nc.dram_tensor("s_qi", [NT, 128, 512], BF16, kind=skind)
    GM = nc.dram_tensor("s_gm", [NT, 128, 512], BF16, kind=skind)
    SG = nc.dram_tensor("s_sg", [NT, 128, 2048], BF16, kind=skind)
    X1 = nc.dram_tensor("s_x1", [T, D], F32, kind=skind)
    H2 = nc.dram_tensor("s_h2", [T + 128, D], BF16, kind=skind)
    SLOT = nc.dram_tensor("s_slot", [NSLOT, 16], I32, kind=skind)
    YS = nc.dram_tensor("s_ys", [NSLOT, D], F32, kind=skind)
    EBS = nc.dram_tensor("s_ebs", [8, 128, 383], F32, kind=skind)
    WEB = nc.dram_tensor("s_b_w", [32 * 128, 6144], BF16, kind="Internal")
    DBG = nc.dram_tensor("s_dbg", [NT, 128, 1024], F32, kind=skind) if debug else None

    with ExitStack() as ctx:
        ctx.enter_context(nc.allow_low_precision(reason="bf16 matmul operands / bf16 intermediates by design"))
        p = Prog(nc, ctx)

        def sbt(c, name, shape, dt):
            return c.enter_context(nc.sbuf_tensor(name, shape, dt))

        def pst(c, name, shape, dt=F32):
            return c.enter_context(nc.psum_tensor(name, shape, dt))

        def MM(out, lhsT, rhs, start, stop, reads, writes, inc=True, skip=False):
            if skip:
                return p.op("tensor", lambda e: e.matmul(out, lhsT, rhs, start=start, stop=stop, skip_group_check=True), reads, writes, inc)
            return p.op("tensor", lambda e: e.matmul(out, lhsT, rhs, start=start, stop=stop), reads, writes, inc)

        def TR(out, in_, ident, reads, writes, inc=True):
            return p.op("tensor", lambda e: e.transpose(out, in_, ident), reads, writes, inc)

        def ACT(out, in_, func, reads, writes, **kw):
            return p.op("scalar", lambda e: e.activation(out=out, in_=in_, func=func, **kw), reads, writes)

        def TS(eng, out, in0, s1, s2, op0, op1, reads, writes, accum_out=None):
            if op1 is None:
                return p.op(eng, lambda e: e.tensor_scalar(out=out, in0=in0, scalar1=s1, scalar2=None, op0=op0), reads, writes)
            if accum_out is not None:
                return p.op(eng, lambda e: e.tensor_scalar(out=out, in0=in0, scalar1=s1, scalar2=s2, op0=op0, op1=op1, accum_out=accum_out), reads, writes)
            return p.op(eng, lambda e: e.tensor_scalar(out=out, in0=in0, scalar1=s1, scalar2=s2, op0=op0, op1=op1), reads, writes)

        def TT(eng, out, in0, in1, op, reads, writes):
            return p.op(eng, lambda e: e.tensor_tensor(out=out, in0=in0, in1=in1, op=op), reads, writes)

        def STT(out, in0, scalar, in1, op0, op1, reads, writes):
            return p.op("vector", lambda e: e.scalar_tensor_tensor(out=out, in0=in0, scalar=scalar, in1=in1, op0=op0, op1=op1), reads, writes)

        def COPY(eng, out, in_, reads, writes):
            if eng == "scalar":
                return ACT(out, in_, AF.Copy, reads, writes)
            return p.op(eng, lambda e: e.tensor_copy(out=out, in_=in_), reads, writes)

        def RED(out, in_, op, reads, writes, negate=False):
            return p.op("vector", lambda e: e.tensor_reduce(out=out, in_=in_, axis=AX.X, op=op, negate=negate), reads, writes)

        def RECIP(out, in_, reads, writes):
            return p.op("vector", lambda e: e.reciprocal(out=out, in_=in_), reads, writes)

        def MEMSET(eng, ap, val, writes):
            return p.op(eng, lambda e: e.memset(ap, val), (), writes)

        def DMA(queue, ds, out, in_, reads, writes):
            return p.dma(queue, ds, lambda e: e.dma_start(out=out, in_=in_), reads, writes)

        def LOAD(queue, out, in_, key, reads=()):
            return DMA(queue, p.dsem(), out, in_, list(reads), [key])

        def GATHER(ds, out, in_, idx, reads, writes, bound=None):
            if bound is not None:
                return p.dma("gpsimd", ds, lambda e: e.indirect_dma_start(out=out, out_offset=None, in_=in_, in_offset=bass.IndirectOffsetOnAxis(ap=idx, axis=0), bounds_check=bound, oob_is_err=False), reads, writes)
            return p.dma("gpsimd", ds, lambda e: e.indirect_dma_start(out=out, out_offset=None, in_=in_, in_offset=bass.IndirectOffsetOnAxis(ap=idx, axis=0)), reads, writes)

        def SCATTER(ds, out, idx, in_, reads, writes):
            return p.dma("gpsimd", ds, lambda e: e.indirect_dma_start(out=out, out_offset=bass.IndirectOffsetOnAxis(ap=idx, axis=0), in_=in_, in_offset=None), reads, writes)

        def rstd_from_ss(rstd, ss, scale, tmp, rk, sk, tk, eps=EPS):
            TS("gpsimd", tmp, ss, scale, eps, ALU.mult, ALU.add, [sk], [tk])
            TT("gpsimd", rstd, tmp, mhalf[:, 0:1], ALU.pow, [tk, "mhalf"], [rk])

        ident_f = sbt(ctx, "ident_f", [128, 128], F32)
        ident_b = sbt(ctx, "ident_b", [128, 128], BF16)
        tri_b = sbt(ctx, "tri_b", [128, 128], BF16)
        tri_f = sbt(ctx, "tri_f", [128, 128], F32)
        ones_b = sbt(ctx, "ones_b", [128, 128], BF16)
        zeros_b = sbt(ctx, "zeros_b", [128, 128], BF16)
        mhalf = sbt(ctx, "mhalf", [128, 1], F32)
        cneg = sbt(ctx, "cneg", [128, 128], F32)
        kiT2 = sbt(ctx, "kiT2", [128, T], BF16)
        ckvT = sbt(ctx, "ckvT", [128, T], BF16)
        Vaug = sbt(ctx, "Vaug", [128, NT, 130], BF16)
        WIall = sbt(ctx, "WIall", [128, NT, 8], F32)
        LG = sbt(ctx, "LG", [128, NT, 36], F32)
        MASKall = sbt(ctx, "MASKall", [128, NT, 32], BF16)
        OH1all = sbt(ctx, "OH1all", [128, NT, 32], BF16)
        OH2all = sbt(ctx, "OH2all", [128, NT, 32], BF16)
        W1all = sbt(ctx, "W1all", [128, NT], F32)
        W2all = sbt(ctx, "W2all", [128, NT], F32)
        banks = [pst(ctx, f"bank{k}", [128, 512]) for k in range(8)]
        dconst = p.dsem("dconst")

        with ExitStack() as c0:
            oh33 = sbt(c0, "oh33", [33, 383], F32)
            mc = sbt(c0, "mc", [32, 33], F32)
            addc = sbt(c0, "addc", [33, 1], F32)
            rb = sbt(c0, "rb", [32, 8], F32)
            rbrel = sbt(c0, "rbrel", [33, 8], F32)
            rbB = sbt(c0, "rbB", [33, 8, 128], F32)
            ebrow = sbt(c0, "ebrow", [128, 8, 383], F32)
            LOAD("sync", ident_f[:], I["c_ident"].ap(), "ident_f")
            LOAD("sync", tri_f[:], I["c_tri"].ap(), "tri_f")
            LOAD("sync", cneg[:], I["c_cneg"].ap(), "cneg")
            LOAD("sync", oh33[:], I["c_oh33"].ap(), "oh33")
            LOAD("sync", mc[:], I["c_mc"].ap(), "mc")
            LOAD("sync", addc[:], I["c_addc"].ap(), "addc")
            LOAD("sync", rb[:], I["rel_bias"].ap(), "rb")
            COPY("vector", ident_b[:], ident_f[:], ["ident_f"], ["ident_b"])
            COPY("vector", tri_b[:], tri_f[:], ["tri_f"], ["tri_b"])
            MEMSET("vector", ones_b[:], 1.0, ["ones_b"])
            MEMSET("vector", zeros_b[:], 0.0, ["zeros_b"])
            MEMSET("gpsimd", mhalf[:], -0.5, ["mhalf"])
            MEMSET("vector", Vaug[:, :, 128:129], 1.0, ["Vaug_ones"])
            MM(banks[0][0:33, 0:8], mc[:], rb[:], True, True, ["mc", "rb"], ["bank0"])
            TS("vector", rbrel[:], banks[0][0:33, 0:8], addc[:, 0:1], None, ALU.add, None, ["bank0", "addc"], ["rbrel"])
            COPY("vector", rbB[:], bc_last(rbrel[:], 128), ["rbrel"], ["rbB"])
            for h in range(8):
                bk = banks[1 + (h % 4)]
                bkey = f"bank{1 + (h % 4)}"
                MM(bk[:, 0:383], rbB[:, h, :], oh33[:], True, True, ["rbB", "oh33"], [bkey])
                ACT(ebrow[:, h, :], bk[:, 0:383], AF.Exp, [bkey], [f"ebrow{h}"])
            LOAD("sync", EBS.ap().rearrange("h p n -> p h n"), ebrow[:], "EBS", reads=[f"ebrow{h}" for h in range(8)])
            p.barrier()
            p.recycle()
            p.emit()

        with ExitStack() as ca:
            Win = sbt(ca, "Win", [128, 8, NCOLS], BF16)
            Wki2 = sbt(ca, "Wki2", [128, 8, 128], BF16)
            g1 = sbt(ca, "g1", [128, 8], F32)
            wuk_n = sbt(ca, "wuk_n", [128, 512], BF16)
            WukT = sbt(ca, "WukT", [128, 8, 128], BF16)
            ws_n = sbt(ca, "ws_n", [128, 4, 128], BF16)
            WsT = sbt(ca, "WsT", [128, 4, 128], BF16)
            bsB = sbt(ca, "bsB", [128, 512], F32)
            lngB = sbt(ca, "lngB", [128, 512], F32)
            lnbB = sbt(ca, "lnbB", [128, 512], F32)
            kvgB = sbt(ca, "kvgB", [128, 128], F32)
            dwa = p.dsem("dwa")
            LOAD("sync", g1[:], I["norm1_g"].ap(), "g1")
            for c in range(8):
                LOAD("gpsimd", Win[:, c, :], I["w_in"].ap()[c * 128:(c + 1) * 128, :], f"Win{c}")
            for c in range(8):
                TS("vector", Win[:, c, :], Win[:, c, :], g1[:, c:c + 1], None, ALU.mult, None, [f"Win{c}", "g1"], [f"Win{c}"])
            for c in range(8):
                COPY("vector", Wki2[:, c, 0:64], Win[:, c, C_KI:C_KI + 64], [f"Win{c}"], ["Wki2"])
                COPY("vector", Wki2[:, c, 64:128], Win[:, c, C_KI:C_KI + 64], [f"Win{c}"], ["Wki2"])
            LOAD("gpsimd", wuk_n[:], I["w_uk"].ap(), "wuk_n")
            LOAD("gpsimd", ws_n[:], I["w_spatial"].ap().rearrange("g i j -> i g j"), "ws_n")
            LOAD("sync", bsB[:], I["b_spatial"].ap()[0:1, :].partition_broadcast(128) if False else bass.AP(I["b_spatial"], 0, [[0, 128], [1, 512]]), "bsB")
            LOAD("sync", lngB[:], bass.AP(I["ln_v_g"], 0, [[0, 128], [1, 512]]), "lngB")
            LOAD("sync", lnbB[:], bass.AP(I["ln_v_b"], 0, [[0, 128], [1, 512]]), "lnbB")
            LOAD("sync", kvgB[:], bass.AP(I["kv_norm_g"], 0, [[0, 128], [1, 128]]), "kvgB")
            MEMSET("vector", WukT[:], 0.0, ["WukT"])
            for k in range(4):
                bk = banks[k][:].bitcast(BF16)
                TR(bk[:, 0:128], wuk_n[:, k * 128:(k + 1) * 128], ident_b[:], ["wuk_n", "ident_b"], [f"bank{k}"])
                TS("vector", WukT[0:64, 2 * k, :], bk[0:64, 0:128], 0.125, None, ALU.mult, None, [f"bank{k}"], ["WukT"])
                TS("vector", WukT[64:128, 2 * k + 1, :], bk[64:128, 0:128], 0.125, None, ALU.mult, None, [f"bank{k}"], ["WukT"])
            for g in range(4):
                bk = banks[4 + g][:].bitcast(BF16)
                TR(bk[:, 0:128], ws_n[:, g, :], ident_b[:], ["ws_n", "ident_b"], [f"bank{4 + g}"])
                TT("vector", WsT[:, g, :], bk[:, 0:128], tri_b[:], ALU.mult, [f"bank{4 + g}", "tri_b"], ["WsT"])

            xin_r = Ring([sbt(ca, f"xin{k}", [128, D], F32) for k in range(4)], "xin", p)
            junk_a = sbt(ca, "junk_a", [128, D], BF16)
            ss_r = Ring([sbt(ca, f"ss{k}", [128, 4], F32) for k in range(2)], "ss")
            xs_r = Ring([sbt(ca, f"xs{k}", [128, D], BF16) for k in range(2)], "xs")
            hT_r = Ring([sbt(ca, f"hT{k}", [128, 8, 256], BF16) for k in range(2)], "hT")
            qT_r = Ring([sbt(ca, f"qT{k}", [128, 4, 128], BF16) for k in range(2)], "qT")
            ql_r = Ring([sbt(ca, f"qlt{k}", [128, 8, 128], BF16) for k in range(2)], "qlt", p)
            qi_r = Ring([sbt(ca, f"qit{k}", [128, 4, 128], BF16) for k in range(2)], "qit", p)
            gup_r = Ring([sbt(ca, f"gup{k}", [128, 4, 2, 128], BF16) for k in range(2)], "gup")
            t1_r = Ring([sbt(ca, f"ta{k}", [128, 512], F32) for k in range(3)], "ta")
            t2_r = Ring([sbt(ca, f"tb{k}", [128, 512], F32) for k in range(3)], "tb")
            gv_r = Ring([sbt(ca, f"gv{k}", [128, 512], F32) for k in range(2)], "gv")
            vn_r = Ring([sbt(ca, f"vn{k}", [128, 512], BF16) for k in range(2)], "vn")
            st_r = Ring([sbt(ca, f"st{k}", [128, 16], F32) for k in range(2)], "st")
            gm_r = Ring([sbt(ca, f"gmt{k}", [128, 512], BF16) for k in range(2)], "gmt", p)
            sg_r = Ring([sbt(ca, f"sgt{k}", [128, 2, 16, 128], BF16) for k in range(2)], "sgt", p)
            bank_i = [0]

            def nbank():
                k = bank_i[0] % 8
                bank_i[0] += 1
                return banks[k], f"bank{k}"

            def gelu_chain(ps, pk, outap, outk):
                ta, tak = t1_r.next()
                tb, tbk = t2_r.next()
                ACT(ta[:], ps, AF.Square, [pk], [tak], scale=float(np.sqrt(0.044715)))
                STT(tb[:], ta[:], 1.0, ps, ALU.add, ALU.mult, [tak, pk], [tbk])
                ACT(ta[:], tb[:], AF.Tanh, [tbk], [tak], scale=0.7978845608028654)
                STT(outap, ta[:], 1.0, ps, ALU.add, ALU.mult, [tak, pk], [outk])

            ntiles_a = NT if stage != "A1" else 2
            xin_q = {}

            def load_x(i):
                xin, xk, xd = xin_r.next()
                DMA("sync", xd, xin[:], I["x"].ap()[i * 128:(i + 1) * 128, :], [], [xk])
                xin_q[i] = (xin, xk)

            HT = {}
            XS = {}
            hstate = {}

            def fe(i):
                xin, xk = xin_q.pop(i)
                ss, ssk = ss_r.next()
                ACT(junk_a[:], xin[:], AF.Square, [xk], ["junk_a", ssk + "a"], accum_out=ss[:, 0:1])
                rstd_from_ss(ss[:, 2:3], ss[:, 0:1], 1.0 / D, ss[:, 1:2], ssk + "c", ssk + "a", ssk + "b")
                xs, xsk = xs_r.next()
                TS("vector", xs[:], xin[:], ss[:, 2:3], None, ALU.mult, None, [xk, ssk + "c"], [xsk])
                XS[i] = (xs, xsk)

            def fe_b(i):
                xs, xsk = XS.pop(i)
                bk, bkk = nbank()
                bkb = bk[:].bitcast(BF16)
                for c in range(8):
                    TR(bkb[:, c * 128:(c + 1) * 128], xs[:, c * 128:(c + 1) * 128], ident_b[:], [xsk, "ident_b"], [bkk], inc=(c == 7))
                par = i % 2
                if par == 0:
                    hstate["p"] = hT_r.next()
                hTp, hpk = hstate["p"]
                hT = hTp[:, :, par * 128:(par + 1) * 128]
                hk = hpk + f"_{par}"
                COPY("scalar", hT, bkb[:, 0:1024].rearrange("p (c t) -> p c t", t=128), [bkk], [hk])
                HT[i] = (hTp, hpk, hT, hk, par)

            def pview(bk, par):
                return bk[:, :].rearrange("p (g q t) -> p g q t", g=2, q=2)[:, :, par, :]

            def fm_pair(col0, ngroups, hTp, hkeys, wt=None):
                outb = []
                for bb in range((ngroups + 1) // 2):
                    bk, bkk = nbank()
                    ng = min(2, ngroups - 2 * bb)
                    for g in range(ng):
                        gg = 2 * bb + g
                        for c in range(8):
                            lhsT = Win[:, c, col0 + gg * 128: col0 + (gg + 1) * 128] if wt is None else wt[:, c, :]
                            MM(bk[:, g * 256:(g + 1) * 256], lhsT, hTp[:, c, :], c == 0, c == 7,
                               [f"Win{c}"] + hkeys + (["Wki2"] if wt is not None else []), [bkk], inc=(c == 7 and g == ng - 1))
                    outb.append((bk, bkk))
                return outb

            assert ntiles_a % 2 == 0
            for ii in range(min(4, ntiles_a)):
                load_x(ii)
            fe(0)
            fe(1)
            fe_b(0)
            fe_b(1)
            for m in range(ntiles_a // 2):
                i0 = 2 * m
                for ii in (i0 + 4, i0 + 5):
                    if ii < ntiles_a:
                        load_x(ii)
                hTp, hpk = HT[i0][0], HT[i0][1]
                hkeys = [hpk + "_0", hpk + "_1"]
                qb = fm_pair(C_Q, 4, hTp, hkeys)
                qTs = []
                for par in range(2):
                    qT, qk = qT_r.next()
                    for bb, (bk, bkk) in enumerate(qb):
                        COPY("scalar", qT[:, 2 * bb:2 * bb + 2, :], pview(bk, par), [bkk], [qk])
                    qTs.append((qT, qk))
                for par in range(2):
                    i = i0 + par
                    qT, qk = qTs[par]
                    qlt, qlk, qld = ql_r.next()
                    for half in range(2):
                        bk, bkk = nbank()
                        for hh in range(4):
                            h = half * 4 + hh
                            MM(bk[:, hh * 128:(hh + 1) * 128], WukT[:, h, :], qT[:, h // 2, :], True, True, ["WukT", qk], [bkk], inc=(hh == 3))
                        COPY("scalar", qlt[:, half * 4:(half + 1) * 4, :].rearrange("p c t -> p (c t)"), bk[:, :], [bkk], [qlk])
                    DMA("sync", qld, QL.ap()[i], qlt[:].rearrange("p c t -> p (c t)"), [qlk], [])
                for ii in (i0 + 2, i0 + 3):
                    if ii < ntiles_a:
                        fe(ii)
                qib = fm_pair(C_QI, 4, hTp, hkeys)
                for par in range(2):
                    i = i0 + par
                    qit, qik, qid = qi_r.next()
                    for bb, (bk, bkk) in enumerate(qib):
                        COPY("vector", qit[:, 2 * bb:2 * bb + 2, :], pview(bk, par), [bkk], [qik])
                    DMA("sync", qid, QI.ap()[i], qit[:].rearrange("p c t -> p (c t)"), [qik], [])
                (bk, bkk), = fm_pair(0, 1, hTp, hkeys, wt=Wki2)
                COPY("scalar", kiT2[:, i0 * 128:(i0 + 2) * 128], bk[:, 0:256], [bkk], [f"ki{i0}", f"ki{i0 + 1}"])
                ub = fm_pair(C_U, 4, hTp, hkeys)
                gup, gupk = gup_r.next()
                for bb, (bk, bkk) in enumerate(ub):
                    gelu_chain(bk[:, :], bkk, gup[:, 2 * bb:2 * bb + 2, :, :].rearrange("p g q t -> p (g q t)"), gupk)
                if i0 + 2 < ntiles_a:
                    fe_b(i0 + 2)
                TP = {}

                def tile_a(par):
                    i = i0 + par
                    _, _, hT, hk, _ = HT.pop(i)
                    bk, bkk = nbank()
                    for c in range(8):
                        MM(bk[:, :], hT[:, c, :], Win[:, c, C_V:C_V + 512], c == 0, c == 7, [hk, f"Win{c}"], [bkk], inc=(c == 7))
                    gv, gvk = gv_r.next()
                    gelu_chain(bk[:, :], bkk, gv[:], gvk)
                    st, stk = st_r.next()
                    p.op("vector", lambda e, o=st[:, 0:6], a=gv[:]: e.bn_stats(out=o, in_=a), [gvk], [stk + "a"])
                    p.op("vector", lambda e, o=st[:, 6:8], a=st[:, 0:6]: e.bn_aggr(out=o, in_=a), [stk + "a"], [stk + "b"])
                    rstd_from_ss(st[:, 9:10], st[:, 7:8], 1.0, st[:, 8:9], stk + "d", stk + "b", stk + "c", eps=4.0 * EPS)
                    TS("vector", gv[:], gv[:], st[:, 6:7], st[:, 9:10], ALU.subtract, ALU.mult, [gvk, stk + "b", stk + "d"], [gvk])
                    TT("vector", gv[:], gv[:], lngB[:], ALU.mult, [gvk, "lngB"], [gvk])
                    vn, vnk = vn_r.next()
                    TT("vector", vn[:], gv[:], lnbB[:], ALU.add, [gvk, "lnbB"], [vnk])
                    bk, bkk = nbank()
                    for c in range(8):
                        MM(bk[:, 0:128], hT[:, c, :], Win[:, c, C_CKV:C_CKV + 128], c == 0, c == 7, [hk, f"Win{c}"], [bkk], inc=False)
                    for c in range(8):
                        MM(bk[:, 128:136], hT[:, c, :], Win[:, c, C_WI:C_WI + 8], c == 0, c == 7, [hk, f"Win{c}"], [bkk], inc=(c == 7))
                    ACT(junk_a[:, 0:128], bk[:, 0:128], AF.Square, [bkk], ["junk_a", stk + "e"], accum_out=st[:, 10:11])
                    rstd_from_ss(st[:, 12:13], st[:, 10:11], 1.0 / 128, st[:, 11:12], stk + "g", stk + "e", stk + "f")
                    STT(Vaug[:, i, 0:128], bk[:, 0:128], st[:, 12:13], kvgB[:], ALU.mult, ALU.mult, [bkk, stk + "g", "kvgB"], [f"Vaug{i}"])
                    COPY("vector", WIall[:, i, :], bk[:, 128:136], [bkk], ["WIall"])
                    TP[par] = (vn, vnk)

                def tile_b(par):
                    i = i0 + par
                    vn, vnk = TP[par]
                    bk2, bk2k = nbank()
                    bk2b = bk2[:].bitcast(BF16)
                    TR(bk2b[:, 0:128], Vaug[:, i, 0:128], ident_b[:], [f"Vaug{i}", "ident_b"], [bk2k])
                    COPY("scalar", ckvT[:, i * 128:(i + 1) * 128], bk2b[:, 0:128], [bk2k], [f"ckvT{i}"])
                    bk, bkk = nbank()
                    for g in range(4):
                        MM(bk[:, g * 128:(g + 1) * 128], vn[:, g * 128:(g + 1) * 128], WsT[:, g, :], True, True, [vnk, "WsT"], [bkk], inc=(g == 3))
                    ta, tak = t1_r.next()
                    TT("vector", ta[:], bk[:, :], bsB[:], ALU.add, [bkk, "bsB"], [tak])
                    gmt, gmk, gmd = gm_r.next()
                    STT(gmt[:].rearrange("p (g t) -> p g t", t=128), gup[:, :, par, :], 0.5, ta[:].rearrange("p (g t) -> p g t", t=128),
                        ALU.mult, ALU.mult, [tak, gupk], [gmk])
                    DMA("sync", gmd, GM.ap()[i], gmt[:], [gmk], [])

                tile_a(0)
                tile_a(1)
                if i0 + 3 < ntiles_a:
                    fe_b(i0 + 3)
                sgp, sgk, sgd = sg_r.next()
                for b8 in range(8):
                    bk, bkk = nbank()
                    for g in range(2):
                        gg = b8 * 2 + g
                        for c in range(8):
                            MM(bk[:, g * 256:(g + 1) * 256], Win[:, c, C_GA + gg * 128:C_GA + (gg + 1) * 128], hTp[:, c, :], c == 0, c == 7,
                               [f"Win{c}"] + hkeys, [bkk], inc=(c == 7 and g == 1))
                    ta, tak = t1_r.next()
                    ACT(ta[:], bk[:, :], AF.Tanh, [bkk], [tak], scale=0.5)
                    TS("vector", sgp[:, :, b8 * 2:b8 * 2 + 2, :].rearrange("p par g t -> p g par t"),
                       ta[:].rearrange("p (g par t) -> p g par t", g=2, par=2), 0.5, 0.5, ALU.mult, ALU.add, [tak], [sgk])
                    if b8 == 3:
                        tile_b(0)
                    if b8 == 6:
                        tile_b(1)
                for pp in range(2):
                    DMA("sync", sgd, SG.ap()[i0 + pp], sgp[:, pp, :, :].rearrange("p c t -> p (c t)"), [sgk], [])
            if debug:
                ddbg = p.dsem("ddbg")
                p.barrier()
                DMA("gpsimd", ddbg, DBG.ap()[0], kiT2[:, 0:1024], [], [])
                DMA("gpsimd", ddbg, DBG.ap()[1], ckvT[:, 0:1024], [], [])
                DMA("gpsimd", ddbg, DBG.ap()[2][:, 0:260], Vaug[:, 0:2, :].rearrange("p a b -> p (a b)"), [], [])
                DMA("gpsimd", ddbg, DBG.ap()[3][:, 0:16], WIall[:, 0:2, :].rearrange("p a b -> p (a b)"), [], [])
            p.barrier()
            p.recycle()
            p.emit()

        if stage in ("A", "A1"):
            return nc

        with ExitStack() as cb:
            Wuv = sbt(cb, "Wuv", [128, 8, 128], BF16)
            Wpa = sbt(cb, "Wpa", [128, 4, D], BF16)
            Wpb = sbt(cb, "Wpb", [128, 4, D], BF16)
            Wout = sbt(cb, "Wout", [128, 8, D], BF16)
            g2B = sbt(cb, "g2B", [128, D], F32)
            Wr = sbt(cb, "Wr", [128, 8, 36], F32)
            rbias = sbt(cb, "rbias", [128, 36], F32)
            MEMSET("vector", Wuv[:], 0.0, ["Wuv"])
            BD = sbt(cb, "BD", [128, 8, 128], F32)
            BO = sbt(cb, "BO", [128, 8, 128], F32)
            for h in range(8):
                LOAD("sync", BD[:, h, :], bass.AP(EBS, h * 128 * 383 + 127, [[382, 128], [1, 128]]), f"BD{h}", reads=["EBS"])
                LOAD("sync", BO[:, h, :], bass.AP(EBS, h * 128 * 383 + 255, [[382, 128], [1, 128]]), f"BO{h}", reads=["EBS"])
            wuv_v = I["w_uv"].ap().rearrange("r (h d) -> r h d", d=64)
            LOAD("gpsimd", Wuv[:, 0::2, 0:64], wuv_v[:, 0::2, :], "Wuv")
            LOAD("gpsimd", Wuv[:, 1::2, 64:128], wuv_v[:, 1::2, :], "Wuv")
            LOAD("gpsimd", Wpa[:], I["w_proj_a"].ap().rearrange("(k p) n -> p k n", p=128), "Wpa")
            LOAD("gpsimd", Wpb[:], I["w_proj_b"].ap().rearrange("(k p) n -> p k n", p=128), "Wpb")
            LOAD("gpsimd", Wout[:], I["w_out"].ap().rearrange("(c p) n -> p c n", p=128), "Wout")
            LOAD("sync", g2B[:], bass.AP(I["norm2_g"], 0, [[0, 128], [1, D]]), "g2B")
            LOAD("sync", Wr[:, :, 0:4], I["router_group_w"].ap().rearrange("(c p) n -> p c n", p=128), "Wr")
            LOAD("sync", Wr[:, :, 4:36], I["router_expert_w"].ap().rearrange("(c p) n -> p c n", p=128), "Wr")
            LOAD("sync", rbias[:, 0:4], bass.AP(I["router_group_b"], 0, [[0, 128], [1, 4]]), "rbias")
            LOAD("sync", rbias[:, 4:36], bass.AP(I["router_expert_b"], 0, [[0, 128], [1, 32]]), "rbias")

            bql_r = Ring([sbt(cb, f"bql{k}", [128, 8, 128], BF16) for k in range(3)], "bql", p)
            bqi_r = Ring([sbt(cb, f"bqi{k}", [128, 8, 128], BF16) for k in range(2)], "bqi", p)
            for k in range(2):
                MEMSET("gpsimd", bqi_r.tiles[k][:], 0.0, [f"bqi{k}"])
            idx2 = [sbt(cb, f"idx{k}", [128, T], F32) for k in range(2)]
            mask = sbt(cb, "mask", [128, T], BF16)
            maskT = sbt(cb, "maskT", [128, NT, 128], BF16)
            rl_r = Ring([sbt(cb, f"rl{k}", [128, 512], BF16) for k in range(4)], "rl")
            Dg_r = Ring([sbt(cb, f"Dg{k}", [128, 8, 128], BF16) for k in range(2)], "Dg")
            aw_r = Ring([sbt(cb, f"aw{k}", [128, 16], F32) for k in range(2)], "aw")
            sc_r = Ring([sbt(cb, f"sc{k}", [128, 64], F32) for k in range(4)], "sc")
            pw = sbt(cb, "pw", [128, 32], F32)
            LOAD("sync", pw[:], I["c_pw"].ap(), "pw")
            MnD = sbt(cb, "MnD", [128, 8, 128], BF16)
            MnO = sbt(cb, "MnO", [128, 8, 128], BF16)
            P_r = Ring([sbt(cb, f"Pt{k}", [128, 8, 128], BF16) for k in range(4)], "Pt")
            OL = sbt(cb, "OL", [128, 8, 128], BF16)
            rden = sbt(cb, "rden", [128, 8], F32)
            OLT = sbt(cb, "OLT", [128, 8, 128], BF16)
            oaT = sbt(cb, "oaT", [128, 4, 128], BF16)
            bgm_r = Ring([sbt(cb, f"bgm{k}", [128, 512], BF16) for k in range(2)], "bgm", p)
            bsg_r = Ring([sbt(cb, f"bsg{k}", [128, 16, 128], BF16) for k in range(2)], "bsg", p)
            xr_r = Ring([sbt(cb, f"xr{k}", [128, D], F32) for k in range(2)], "xr", p)
            x1_ds = [p.dsem(f"x1d{k}") for k in range(2)]
            t12 = sbt(cb, "t12", [128, 512], F32)
            mT = sbt(cb, "mT", [128, 8, 128], BF16)
            junkb = sbt(cb, "junkb", [128, D], BF16)
            h2f = sbt(cb, "h2f", [128, D], F32)
            h2b_r = Ring([sbt(cb, f"h2b{k}", [128, D], BF16) for k in range(2)], "h2b", p)
            h2T = sbt(cb, "h2T", [128, 8, 128], F32)
            bank5_i = [0]

            def nbank5():
                k = bank5_i[0] % 3
                bank5_i[0] += 1
                return banks[k], f"bank{k}"

            def flat(ap):
                return ap.rearrange("p a b -> p (a b)")

            ntiles_b = NT if stage not in ("B1",) else 3
            TS_ = {}

            def P1_load(i):
                t = TS_[i] = {}
                qlt, qlk, qld = bql_r.next()
                DMA("sync", qld, flat(qlt[:]), QL.ap()[i], [], [qlk])
                qip, qik, qid = bqi_r.next()
                qi_v = QI.ap()[i].rearrange("p (k t) -> p k t", t=128)
                DMA("sync", qid, qip[0:64, 0::2, :], qi_v[0:64, :, :], [], [qik])
                DMA("sync", qid, qip[64:128, 1::2, :], qi_v[64:128, :, :], [], [qik])
                sc, sck = sc_r.next()
                t.update(qlt=qlt, qlk=qlk, qip=qip, qik=qik, sc=sc, sck=sck, idx=idx2[i % 2], idxk=f"idx{i % 2}")

            def out_loads(i):
                t = TS_[i]
                gmt, gmk, gmd = bgm_r.next()
                DMA("sync", gmd, gmt[:], GM.ap()[i], [], [gmk])
                sgt, sgk, sgd = bsg_r.next()
                DMA("sync", sgd, flat(sgt[:]), SG.ap()[i], [], [sgk])
                xres, xk, xd = xr_r.next()
                DMA("sync", xd, xres[:], I["x"].ap()[i * 128:(i + 1) * 128, :], [], [xk])
                t.update(gmt=gmt, gmk=gmk, sgt=sgt, sgk=sgk, xres=xres, xk=xk)

            def idx_units(i):
                S = (i + 1) * 128
                t = TS_[i]
                qip, qik, idx, idxk = t["qip"], t["qik"], t["idx"], t["idxk"]
                Dg, Dgk = Dg_r.next()
                TT("vector", Dg[:], bc_mid(ident_b[:], 8), bc_last(WIall[:, i, :], 128), ALU.mult, ["ident_b", "WIall"], [Dgk])
                units = []
                accb, acck = banks[4], "bank4"
                nch = (S + 511) // 512
                st = {"q": []}
                LAGD = 2
                for ch in range(nch):
                    w = min(512, S - ch * 512)
                    kkeys = [f"ki{j}" for j in range(ch * 4, ch * 4 + w // 128)]

                    def diag(pend, w=w, last=False):
                        ph, prl, prlk = pend
                        MM(accb[:, 0:w], Dg[:, ph, :], prl[:, 0:w], ph == 0, last, [Dgk, prlk], [acck])

                    for h in range(8):
                        def unit(ch=ch, h=h, w=w, kkeys=kkeys, diag=diag):
                            bk, bkk = nbank5()
                            MM(bk[:, 0:w], qip[:, h, :], kiT2[:, ch * 512: ch * 512 + w], True, True, [qik] + kkeys, [bkk])
                            rl, rlk = rl_r.next()
                            ACT(rl[:, 0:w], bk[:, 0:w], AF.Relu, [bkk], [rlk])
                            st["q"].append((h, rl, rlk))
                            if len(st["q"]) > LAGD:
                                diag(st["q"].pop(0))
                        units.append(unit)

                    def fin(ch=ch, w=w, diag=diag):
                        while len(st["q"]) > 1:
                            diag(st["q"].pop(0))
                        diag(st["q"].pop(0), last=True)
                        COPY("scalar", idx[:, ch * 512: ch * 512 + w], accb[:, 0:w], [acck], [idxk])
                    units.append(fin)
                return units

            def P1_fin(i):
                S = (i + 1) * 128
                t = TS_[i]
                idx, idxk, sc, sck = t["idx"], t["idxk"], t["sc"], t["sck"]
                dg = idx[:, i * 128:(i + 1) * 128]
                TT("vector", dg, dg, cneg[:], ALU.add, [idxk, "cneg"], [idxk])
                lo, hi, wid, cnd, cnt, uu = [sc[:, q:q + 1] for q in range(6)]
                Hx = sc[:, 16:16 + NIT + 1]
                if i >= 2:
                    RED(lo, idx[:, 0:256], ALU.min, [idxk], [sck])
                    RED(hi, idx[:, 0:S], ALU.max, [idxk, sck], [sck])
                    TT("vector", wid, hi, lo, ALU.subtract, [sck], [sck])
                    TS("vector", Hx, pw[:, 0:NIT + 1], wid, None, ALU.mult, None, ["pw", sck], [sck])
                    TT("vector", cnd, lo, Hx[:, 1:2], ALU.add, [sck], [sck])
                else:
                    MEMSET("vector", lo, -1.0e29, [sck])

            def bis(i, k):
                S = (i + 1) * 128
                sc, sck, idx, idxk = TS_[i]["sc"], TS_[i]["sck"], TS_[i]["idx"], TS_[i]["idxk"]
                lo, hi, wid, cnd, cnt, uu = [sc[:, q:q + 1] for q in range(6)]
                Hx = sc[:, 16:16 + NIT + 1]
                TS("vector", mask[:, 0:S], idx[:, 0:S], cnd, None, ALU.is_ge, ALU.add, [idxk, sck], ["mask", sck], accum_out=cnt)
                if k < NIT - 1:
                    TS("vector", uu, cnt, 255.5, Hx[:, k + 1:k + 2], ALU.is_ge, ALU.mult, [sck], [sck])
                    STT(cnd, cnd, Hx[:, k + 2:k + 3], uu, ALU.subtract, ALU.add, [sck], [sck])
                else:
                    TS("vector", uu, cnt, 255.5, Hx[:, NIT:NIT + 1], ALU.is_ge, ALU.mult, [sck], [sck])
                    STT(lo, cnd, Hx[:, NIT:NIT + 1], uu, ALU.subtract, ALU.add, [sck], [sck])

            def P1c(i):
                S = (i + 1) * 128
                sc, sck, idx, idxk = TS_[i]["sc"], TS_[i]["sck"], TS_[i]["idx"], TS_[i]["idxk"]
                TS("vector", mask[:, 0:S], idx[:, 0:S], sc[:, 0:1], None, ALU.is_ge, None, [idxk, sck], ["mask"])

            def P2(i):
                for j0 in range(0, i + 1, 8):
                    nb = min(8, i + 1 - j0)
                    bk, bkk = nbank5()
                    bkb = bk[:].bitcast(BF16)
                    for jj in range(nb):
                        TR(bkb[:, jj * 128:(jj + 1) * 128], mask[:, (j0 + jj) * 128:(j0 + jj + 1) * 128], ident_b[:], ["mask", "ident_b"], [bkk], inc=(jj == nb - 1))
                    COPY("scalar", flat(maskT[:, j0:j0 + nb, :]), bkb[:, 0:nb * 128], [bkk], [f"mT{j}" for j in range(j0, j0 + nb)])
                TT("gpsimd", MnD[:], BD[:], bc_mid(maskT[:, i, :], 8), ALU.mult, [f"BD{h}" for h in range(8)] + [f"mT{i}"], ["MnD"])
                if i >= 1:
                    TT("gpsimd", MnO[:], BO[:], bc_mid(maskT[:, i - 1, :], 8), ALU.mult, [f"BO{h}" for h in range(8)] + [f"mT{i - 1}"], ["MnO"])

            def blk_pro(i):
                for b in range(3):
                    MM(banks[5 + b][:, :], zeros_b[:], ckvT[:, 0:512], True, True, ["zeros_b"] + [f"ckvT{j}" for j in range(4)], [f"bank{5 + b}"])

            PQ = {}

            def blk_j(i, j):
                blk_score(i, j)
                if j >= 3:
                    blk_pv(i, j - 3)

            def blk_score(i, j):
                qlt, qlk = TS_[i]["qlt"], TS_[i]["qlk"]
                Pt, Pk = P_r.next()
                PQ[(i, j)] = (Pt, Pk)
                for g in range(2):
                    bk, bkk = nbank5()
                    MM(bk[:, :], ckvT[:, j * 128:(j + 1) * 128], flat(qlt[:, g * 4:(g + 1) * 4, :]), True, True, [f"ckvT{j}", qlk], [bkk])
                    ACT(flat(Pt[:, g * 4:(g + 1) * 4, :]), bk[:, :], AF.Exp, [bkk], [Pk])
                if j == i:
                    TT("gpsimd", Pt[:], Pt[:], MnD[:], ALU.mult, [Pk, "MnD"], [Pk])
                elif j == i - 1:
                    TT("gpsimd", Pt[:], Pt[:], MnO[:], ALU.mult, [Pk, "MnO"], [Pk])
                else:
                    TT("gpsimd", Pt[:], Pt[:], bc_mid(maskT[:, j, :], 8), ALU.mult, [Pk, f"mT{j}"], [Pk])

            def blk_pv(i, j):
                Pt, Pk = PQ.pop((i, j))
                for h in range(8):
                    b = 5 + h // 3
                    MM(banks[b][:, (h % 3) * 130:(h % 3) * 130 + 129], Pt[:, h, :], Vaug[:, j, 0:129], False, False,
                       [Pk, f"Vaug{j}", "Vaug_ones"], [f"bank{b}"], inc=(h == 7), skip=True)

            def blk_epi(i):
                for jj in range(max(0, i - 2), i + 1):
                    blk_pv(i, jj)
                for b in range(3):
                    nb = 3 if b < 2 else 2
                    RECIP(rden[:, 3 * b:3 * b + nb], banks[5 + b][:, 128:128 + 130 * (nb - 1) + 1:130], [f"bank{5 + b}"], ["rden"])
                for h in range(8):
                    b = 5 + h // 3
                    ACT(OL[:, h, :], banks[b][:, (h % 3) * 130:(h % 3) * 130 + 128], AF.Copy, [f"bank{b}", "rden"], ["OL"], scale=rden[:, h:h + 1])
                bk, bkk = nbank5()
                bkb = bk[:].bitcast(BF16)
                for h in range(8):
                    TR(bkb[:, h * 128:(h + 1) * 128], OL[:, h, :], ident_b[:], ["OL", "ident_b"], [bkk], inc=(h == 7))
                COPY("scalar", flat(OLT[:]), bkb[:, 0:1024], [bkk], ["OLT"])
                bk, bkk = nbank5()
                for k in range(4):
                    for hh in range(2):
                        MM(bk[:, k * 128:(k + 1) * 128], Wuv[:, 2 * k + hh, :], OLT[:, 2 * k + hh, :], hh == 0, hh == 1, ["Wuv", "OLT"], [bkk], inc=(k == 3 and hh == 1))
                COPY("scalar", flat(oaT[:]), bk[:, :], [bkk], ["oaT"])

            def out_stages(i):
                t = TS_[i]
                gmt, gmk, sgt, sgk, xres, xk, sc, sck = t["gmt"], t["gmk"], t["sgt"], t["sgk"], t["xres"], t["xk"], t["sc"], t["sck"]
                hb = {}

                B3, B3k = banks[3], "bank3"

                def yq(q):
                    for mm in range(2):
                        m = 2 * q + mm
                        for kc in range(4):
                            MM(B3[:, mm * 128:(mm + 1) * 128], Wpa[:, kc, m * 128:(m + 1) * 128], oaT[:, kc, :], kc == 0, kc == 3, ["Wpa", "oaT"], [B3k], inc=False)
                    for mm in range(2):
                        m = 2 * q + mm
                        for kc in range(4):
                            MM(B3[:, 256 + mm * 128:256 + (mm + 1) * 128], Wpb[:, kc, m * 128:(m + 1) * 128], gmt[:, kc * 128:(kc + 1) * 128], kc == 0, kc == 3, ["Wpb", gmk], [B3k], inc=(kc == 3 and mm == 1))

                def gq(q):
                    sgv = sgt[:].rearrange("p (a g) t -> p a g t", a=2)[:, :, 2 * q:2 * q + 2, :]
                    TT("vector", t12[:].rearrange("p (a g t) -> p a g t", a=2, g=2), B3[:, :].rearrange("p (a g t) -> p a g t", a=2, g=2), sgv,
                       ALU.mult, [B3k, sgk], ["t12"])
                    TT("gpsimd", flat(mT[:, 2 * q:2 * q + 2, :]), t12[:, 0:256], t12[:, 256:512], ALU.add, ["t12"], ["mT"])

                def wout(half):
                    for c in range(8):
                        MM(B3[:, :], mT[:, c, :], Wout[:, c, half * 512:(half + 1) * 512], c == 0, c == 7, ["mT", "Wout"], [B3k], inc=(c == 7))

                def xadd(half):
                    xs_ = xres[:, half * 512:(half + 1) * 512]
                    TT("vector", xs_, B3[:, :], xs_, ALU.add, [B3k, xk], [xk])

                def norm2():
                    DMA("sync", x1_ds[i % 2], X1.ap()[i * 128:(i + 1) * 128, :], xres[:], [xk], [])
                    ACT(junkb[:], xres[:], AF.Square, [xk], ["junkb", sck + "n"], accum_out=sc[:, 40:41])
                    rstd_from_ss(sc[:, 42:43], sc[:, 40:41], 1.0 / D, sc[:, 41:42], sck + "n3", sck + "n", sck + "n2")
                    STT(h2f[:], xres[:], sc[:, 42:43], g2B[:], ALU.mult, ALU.mult, [xk, sck + "n3", "g2B"], ["h2f"])
                    h2b, h2bk, h2bd = h2b_r.next()
                    COPY("scalar", h2b[:], h2f[:], ["h2f"], [h2bk])
                    DMA("sync", h2bd, H2.ap()[i * 128:(i + 1) * 128, :], h2b[:], [h2bk], [])

                def trh(half):
                    for cc in range(4):
                        c = half * 4 + cc
                        TR(B3[:, cc * 128:(cc + 1) * 128], h2f[:, c * 128:(c + 1) * 128], ident_f[:], ["h2f", "ident_f"], [B3k], inc=(cc == 3))
                    COPY("scalar", flat(h2T[:, half * 4:(half + 1) * 4, :]), B3[:, :], [B3k], ["h2T"])

                def rmm():
                    for c in range(8):
                        MM(B3[:, 0:36], h2T[:, c, :], Wr[:, c, :], c == 0, c == 7, ["h2T", "Wr"], [B3k], inc=(c == 7))
                    hb[20] = (B3, B3k)

                stages = [
                    lambda: yq(0),
                    lambda: (gq(0), yq(1)),
                    lambda: (gq(1), yq(2)),
                    lambda: (gq(2), yq(3)),
                    lambda: gq(3),
                    lambda: wout(0),
                    lambda: (xadd(0), wout(1)),
                    lambda: (xadd(1), norm2()),
                    lambda: trh(0),
                    lambda: trh(1),
                    lambda: rmm(),
                ]

                def s7():
                    bk, bkk = hb[20]
                    TT("vector", LG[:, i, :], bk[:, 0:36], rbias[:], ALU.add, [bkk, "rbias"], ["LG"])

                return stages + [s7]

            def run_units(us):
                for u in us:
                    u()

            for i0 in range(min(2, ntiles_b)):
                P1_load(i0)
                run_units(idx_units(i0))
                P1_fin(i0)
            P1c(0)
            P2(0)
            web_ds = {wn: p.dsem() for wn in ("w_gate", "w_up", "w_down")}
            for it in range(ntiles_b):
                for wq, wn in enumerate(("w_gate", "w_up", "w_down")):
                    DMA("gpsimd", web_ds[wn], WEB.ap()[it * 128:(it + 1) * 128, wq * 2048:(wq + 1) * 2048], I[wn].ap()[it * 128:(it + 1) * 128, :], [], [])
                if it >= 1:
                    out_loads(it - 1)
                U = []
                if it + 2 < ntiles_b:
                    P1_load(it + 2)
                    U = idx_units(it + 2)
                nxt = it + 1 < ntiles_b
                blk_pro(it)
                nb = it + 1
                nk = NIT if (nxt and it + 1 >= 2) else 0
                ost = out_stages(it - 1) if it >= 1 else []
                NS = 12
                Tn = max(nb, nk, NS)
                nu = len(U)
                for tck in range(Tn):
                    for k in range((tck * nk) // Tn, ((tck + 1) * nk) // Tn):
                        bis(it + 1, k)
                    for j in range((tck * nb) // Tn, ((tck + 1) * nb) // Tn):
                        blk_j(it, j)
                    for q in range((tck * nu) // Tn, ((tck + 1) * nu) // Tn):
                        U[q]()
                    for si, st_fn in enumerate(ost):
                        if (si * Tn) // NS == tck:
                            st_fn()
                if nxt:
                    P1c(it + 1)
                    P2(it + 1)
                blk_epi(it)
                if it + 2 < ntiles_b:
                    P1_fin(it + 2)
            out_loads(ntiles_b - 1)
            for st_fn in out_stages(ntiles_b - 1):
                st_fn()
            if debug and stage in ("B", "B1"):
                ddbg = p.dsem("ddbg2")
                p.barrier()
                DMA("gpsimd", ddbg, DBG.ap()[6][:, 0:NT], W1all[:], [], [])
                DMA("gpsimd", ddbg, DBG.ap()[7][:, 0:NT], W2all[:], [], [])
                DMA("gpsimd", ddbg, DBG.ap()[8][:, 0:1024], flat(OH1all[:]), [], [])
                DMA("gpsimd", ddbg, DBG.ap()[9][:, 0:1024], flat(OH2all[:]), [], [])
            p.barrier()
            p.recycle()
            p.emit()
        if stage in ("B", "B1"):
            return nc

        with ExitStack() as cr:
            gl = LG[:, :, 0:4]
            el4 = LG[:, :, 4:36].rearrange("p i (g j) -> p i g j", j=8)
            r_gmax = sbt(cr, "r_gmax", [128, NT], F32)
            r_gsh = sbt(cr, "r_gsh", [128, NT, 4], F32)
            r_ge = sbt(cr, "r_ge", [128, NT, 4], F32)
            r_gsum = sbt(cr, "r_gsum", [128, NT], F32)
            r_gw = sbt(cr, "r_gw", [128, NT], F32)
            r_goh = sbt(cr, "r_goh", [128, NT, 4], F32)
            r_gpen = sbt(cr, "r_gpen", [128, NT, 4], F32)
            r_elm = sbt(cr, "r_elm", [128, NT, 32], F32)
            r_elm2 = sbt(cr, "r_elm2", [128, NT, 32], F32)
            r_m1 = sbt(cr, "r_m1", [128, NT], F32)
            r_m2 = sbt(cr, "r_m2", [128, NT], F32)
            r_dd = sbt(cr, "r_dd", [128, NT], F32)
            r_ee = sbt(cr, "r_ee", [128, NT], F32)
            r_s1 = sbt(cr, "r_s1", [128, NT], F32)
            RED(r_gmax[:], gl, ALU.max, ["LG"], ["r_gmax"])
            TT("vector", r_gsh[:], gl, bc_last(r_gmax[:], 4), ALU.subtract, ["LG", "r_gmax"], ["r_gsh"])
            ACT(r_ge[:], r_gsh[:], AF.Exp, ["r_gsh"], ["r_ge"])
            RED(r_gsum[:], r_ge[:], ALU.add, ["r_ge"], ["r_gsum"])
            RECIP(r_gw[:], r_gsum[:], ["r_gsum"], ["r_gw"])
            TS("vector", r_goh[:], r_gsh[:], 0.0, None, ALU.is_ge, None, ["r_gsh"], ["r_goh"])
            TS("vector", r_gpen[:], r_goh[:], 1.0, BIG, ALU.subtract, ALU.mult, ["r_goh"], ["r_gpen"])
            TT("vector", r_elm[:].rearrange("p i (g j) -> p i g j", j=8), el4, bc_last(r_gpen[:], 8), ALU.add, ["LG", "r_gpen"], ["r_elm"])
            RED(r_m1[:], r_elm[:], ALU.max, ["r_elm"], ["r_m1"])
            TT("vector", OH1all[:], r_elm[:], bc_last(r_m1[:], 32), ALU.is_ge, ["r_elm", "r_m1"], ["OH1all"])
            STT(r_elm2[:], OH1all[:], -BIG, r_elm[:], ALU.mult, ALU.add, ["OH1all", "r_elm"], ["r_elm2"])
            RED(r_m2[:], r_elm2[:], ALU.max, ["r_elm2"], ["r_m2"])
            TT("vector", OH2all[:], r_elm2[:], bc_last(r_m2[:], 32), ALU.is_ge, ["r_elm2", "r_m2"], ["OH2all"])
            TT("vector", r_dd[:], r_m2[:], r_m1[:], ALU.subtract, ["r_m1", "r_m2"], ["r_dd"])
            ACT(r_ee[:], r_dd[:], AF.Exp, ["r_dd"], ["r_ee"])
            TS("vector", r_ee[:], r_ee[:], 1.0, None, ALU.add, None, ["r_ee"], ["r_ee"])
            RECIP(r_s1[:], r_ee[:], ["r_ee"], ["r_s1"])
            TT("vector", W1all[:], r_gw[:], r_s1[:], ALU.mult, ["r_gw", "r_s1"], ["W1all"])
            TT("vector", W2all[:], r_gw[:], W1all[:], ALU.subtract, ["r_gw", "W1all"], ["W2all"])
            TT("vector", MASKall[:], OH1all[:], OH2all[:], ALU.add, ["OH1all", "OH2all"], ["MASKall"])
            p.barrier()
            p.recycle()
            p.emit()

        with ExitStack() as cd:
            tokid = sbt(cd, "tokid", [128, NT, 16], I32)
            pcol = sbt(cd, "pcol", [128, 1], F32)
            j128 = sbt(cd, "j128", [128, NSLOT_T], F32)
            gfB = sbt(cd, "gfB", [128, D], F32)
            LOAD("sync", tokid[:], I["c_tokid"].ap(), "tokid")
            LOAD("sync", pcol[:], I["c_pcol"].ap(), "pcol")
            LOAD("sync", j128[:], I["c_j128"].ap(), "j128")
            LOAD("sync", gfB[:], bass.AP(I["final_norm_g"], 0, [[0, 128], [1, D]]), "gfB")
            for i in range(NT):
                bk = banks[i // 16]
                col = (i % 16) * 32
                MM(bk[:, col:col + 32], tri_b[:], MASKall[:, i, :], True, i == 0, ["tri_b", "MASKall"], [f"bank{i // 16}"], inc=(i == 0), skip=True)
                for i2 in range(i):
                    MM(bk[:, col:col + 32], ones_b[:], MASKall[:, i2, :], False, i2 == i - 1, ["ones_b", "MASKall"], [f"bank{i // 16}"], inc=(i2 == i - 1), skip=True)
            for i in range(NT):
                MM(banks[2][:, 0:32], ones_b[:], MASKall[:, i, :], i == 0, i == NT - 1, ["ones_b", "MASKall"], ["bank2"], inc=(i == NT - 1), skip=True)
            ci = sbt(cd, "ci", [128, 32], I32)
            padf = sbt(cd, "padf", [128, 32], F32)
            pa = sbt(cd, "pa", [128, 32], F32)
            pb = sbt(cd, "pb", [128, 32], F32)
            bm1 = sbt(cd, "bm1", [128, 32], F32)
            TS("vector", ci[:], banks[2][:, 0:32], 127.0, None, ALU.add, None, ["bank2"], ["ci"])
            p.op("vector", lambda e: e.tensor_scalar(out=ci[:], in0=ci[:], scalar1=7, scalar2=7, op0=ALU.logical_shift_right, op1=ALU.logical_shift_left), ["ci"], ["ci"])
            COPY("vector", padf[:], ci[:], ["ci"], ["padf"])
            COPY("vector", pa[:], padf[:], ["padf"], ["pa"])
            src, srck, dst, dstk = pa, "pa", pb, "pb"
            for sft in (1, 2, 4, 8, 16):
                COPY("vector", dst[:, 0:sft], src[:, 0:sft], [srck], [dstk])
                TT("vector", dst[:, sft:32], src[:, sft:32], src[:, 0:32 - sft], ALU.add, [srck], [dstk])
                src, srck, dst, dstk = dst, dstk, src, srck
            incl, inclk = src, srck
            TT("vector", bm1[:], incl[:], padf[:], ALU.subtract, [inclk, "padf"], ["bm1"])
            TS("vector", bm1[:], bm1[:], -1.0, None, ALU.add, None, ["bm1"], ["bm1"])
            tmpAll = sbt(cd, "tmpAll", [128, NT, 32], F32)
            prod = sbt(cd, "prod", [128, NT, 32], F32)
            Sf = sbt(cd, "Sf", [128, 2, NT], F32)
            Si = sbt(cd, "Si", [128, 2, NT], I32)
            for b in range(2):
                TT("vector", tmpAll[:, b * 16:(b + 1) * 16, :], banks[b][:, :].rearrange("p (a b) -> p a b", b=32), bc_mid(bm1[:], 16), ALU.add, [f"bank{b}", "bm1"], ["tmpAll"])
            for k, OH in enumerate((OH1all, OH2all)):
                TT("vector", prod[:], tmpAll[:], OH[:], ALU.mult, ["tmpAll", "OH1all", "OH2all"], ["prod"])
                RED(Sf[:, k, :], prod[:], ALU.add, ["prod"], ["Sf"])
            COPY("vector", Si[:], Sf[:], ["Sf"], ["Si"])
            cmp = sbt(cd, "cmp", [128, NSLOT_T, 32], F32)
            texp = sbt(cd, "texp", [128, NSLOT_T], F32)
            IDXW = sbt(cd, "IDXW", [128, NSLOT_T], I32)
            TT("vector", cmp[:], bc_mid(incl[:], NSLOT_T), bc_last(j128[:], 32), ALU.is_le, [inclk, "j128"], ["cmp"])
            RED(texp[:], cmp[:], ALU.add, ["cmp"], ["texp"])
            TS("vector", texp[:], texp[:], 31.0, 128.0, ALU.min, ALU.mult, ["texp"], ["texp"])
            TS("vector", texp[:], texp[:], pcol[:, 0:1], None, ALU.add, None, ["texp", "pcol"], ["texp"])
            COPY("vector", IDXW[:], texp[:], ["texp"], ["IDXW"])
            si = sbt(cd, "si", [128, 16], I32)
            zrow = sbt(cd, "zrow", [128, D], BF16)
            MEMSET("gpsimd", si[:], T, ["si"])
            MEMSET("gpsimd", zrow[:], 0.0, ["zrow"])
            LOAD("sync", SLOT.ap().rearrange("(j p) c -> p j c", p=128), bc_mid(si[:], NSLOT_T), "SLOTinit", reads=["si"])
            LOAD("sync", H2.ap()[T:T + 128, :], zrow[:], "H2zero", reads=["zrow"])
            sc_ds = [p.dsem(f"scat{k}") for k in range(4)]
            sckeys = []
            n = 0
            for i in range(NT):
                for k in range(2):
                    SCATTER(sc_ds[n % 4], SLOT.ap(), Si[:, k, i:i + 1], tokid[:, i, :], ["Si", "tokid", "SLOTinit"], [f"SLOTs{n}"])
                    sckeys.append(f"SLOTs{n}")
                    n += 1
            stk_r = Ring([sbt(cd, f"stk{k}", [128, 16], I32) for k in range(4)], "stk", p)
            Hs_r = Ring([sbt(cd, f"Hs{k}", [128, D], BF16) for k in range(4)], "Hs", p)
            Wa_r = Ring([sbt(cd, f"Wa{k}", [128, 6144], BF16) for k in range(5)], "Wa", p)
            yt_r = Ring([sbt(cd, f"yt{k}", [128, D], F32) for k in range(2)], "yt", p)
            bank8_i = [0]

            def nbank8():
                k = bank8_i[0] % 8
                bank8_i[0] += 1
                return banks[k], f"bank{k}"

            nslot_t = NSLOT_T
            yskeys = []
            dq = {}

            def d_loads(j):
                stk, stkk, stkd = stk_r.next()
                DMA("sync", stkd, stk[:], SLOT.ap()[j * 128:(j + 1) * 128, :], sckeys + ["SLOTinit"], [stkk])
                Hs, Hsk, Hsd = Hs_r.next()
                GATHER(Hsd, Hs[:], H2.ap(), stk[:, 0:1], [stkk, "H2zero"], [Hsk])
                Wa, Wak, Wad = Wa_r.next()
                GATHER(Wad, Wa[:], WEB.ap(), IDXW[:, j:j + 1], ["IDXW"], [Wak])
                dq[j] = (Hs, Hsk, Wa[:, 0:2048], Wak, Wa[:, 2048:4096], Wak, Wa[:, 4096:6144], Wak)

            HsT_r = Ring([sbt(cd, f"HsT{k}", [128, 8, 128], BF16) for k in range(2)], "HsT")
            he_r = Ring([sbt(cd, f"he{k}", [128, 256], BF16) for k in range(2)], "he")
            heT_r = Ring([sbt(cd, f"heT{k}", [128, 256], BF16) for k in range(2)], "heT")
            sgs_r = Ring([sbt(cd, f"sgs{k}", [128, 256], F32) for k in range(2)], "sgs")
            DS = {}

            def sA(j):
                Hs, Hsk = dq[j][0], dq[j][1]
                bk, bkk = nbank8()
                bkb = bk[:].bitcast(BF16)
                for c in range(8):
                    TR(bkb[:, c * 128:(c + 1) * 128], Hs[:, c:D:8], ident_b[:], [Hsk, "ident_b"], [bkk], inc=(c == 7))
                HsT_, HsTk = HsT_r.next()
                COPY("scalar", flat(HsT_[:]), bkb[:, 0:1024], [bkk], [HsTk])
                DS[j] = {"HsT": (HsT_, HsTk)}

            def sB(j):
                _, _, Wg, Wgk, Wu, Wuk_, _, _ = dq[j]
                HsT_, HsTk = DS[j]["HsT"]
                bk, bkk = nbank8()
                for c in range(8):
                    MM(bk[:, 0:256], HsT_[:, c, :], Wg[:, c * 256:(c + 1) * 256], c == 0, c == 7, [Wgk, HsTk], [bkk], inc=False)
                for c in range(8):
                    MM(bk[:, 256:512], HsT_[:, c, :], Wu[:, c * 256:(c + 1) * 256], c == 0, c == 7, [Wuk_, HsTk], [bkk], inc=(c == 7))
                sgs_, sgsk = sgs_r.next()
                ACT(sgs_[:], bk[:, 0:256], AF.Silu, [bkk], [sgsk])
                he_, hek = he_r.next()
                TT("vector", he_[:], sgs_[:], bk[:, 256:512], ALU.mult, [sgsk, bkk], [hek])
                DS[j]["he"] = (he_, hek)

            def sC(j):
                he_, hek = DS[j]["he"]
                bk2, bk2k = nbank8()
                bk2b = bk2[:].bitcast(BF16)
                for c2 in range(2):
                    TR(bk2b[:, c2 * 128:(c2 + 1) * 128], he_[:, c2:256:2], ident_b[:], [hek, "ident_b"], [bk2k], inc=(c2 == 1))
                heT_, heTk = heT_r.next()
                COPY("vector", heT_[:], bk2b[:, 0:256], [bk2k], [heTk])
                DS[j]["heT"] = (heT_, heTk)

            def sD(j):
                Wd, Wdk = dq[j][6], dq[j][7]
                heT_, heTk = DS[j]["heT"]
                yt, ytk, ytd = yt_r.next()
                for half in range(2):
                    bk, bkk = nbank8()
                    for c2 in range(2):
                        MM(bk[:, :], heT_[:, c2 * 128:(c2 + 1) * 128], Wd[:, c2 * 1024 + half * 512:c2 * 1024 + (half + 1) * 512], c2 == 0, c2 == 1, [heTk, Wdk], [bkk], inc=(c2 == 1))
                    COPY("scalar" if half == 0 else "vector", yt[:, half * 512:(half + 1) * 512], bk[:, :], [bkk], [ytk])
                DMA("sync", ytd, YS.ap()[j * 128:(j + 1) * 128, :], yt[:], [ytk], [f"YS{j}"])
                yskeys.append(f"YS{j}")
                dq.pop(j)
                DS.pop(j)

            d_loads(0)
            d_loads(1)
            for j in range(nslot_t + 2):
                if j + 2 < nslot_t:
                    d_loads(j + 2)
                if 0 <= j - 2 < nslot_t:
                    sC(j - 2)
                if j < nslot_t:
                    sA(j)
                if 0 <= j - 1 < nslot_t:
                    sB(j - 1)
                if 0 <= j - 2 < nslot_t:
                    sD(j - 2)
            x1_r = Ring([sbt(cd, f"ex{k}", [128, D], F32) for k in range(3)], "ex", p)
            y1_r = Ring([sbt(cd, f"ey1{k}", [128, D], F32) for k in range(3)], "ey1", p)
            y2_r = Ring([sbt(cd, f"ey2{k}", [128, D], F32) for k in range(3)], "ey2", p)
            ot_r = Ring([sbt(cd, f"ot{k}", [128, D], F32) for k in range(2)], "ot", p)
            es_r = Ring([sbt(cd, f"es{k}", [128, 4], F32) for k in range(2)], "es")
            junke = sbt(cd, "junke", [128, D], BF16)
            eq = {}

            def e_loads(i):
                ex, exk, exd = x1_r.next()
                DMA("sync", exd, ex[:], X1.ap()[i * 128:(i + 1) * 128, :], [], [exk])
                y1, y1k, y1d = y1_r.next()
                GATHER(y1d, y1[:], YS.ap(), Si[:, 0, i:i + 1], ["Si"] + yskeys, [y1k])
                y2, y2k, y2d = y2_r.next()
                GATHER(y2d, y2[:], YS.ap(), Si[:, 1, i:i + 1], ["Si"] + yskeys, [y2k])
                eq[i] = (ex, exk, y1, y1k, y2, y2k)

            e_loads(0)
            e_loads(1)
            for i in range(NT):
                if i + 2 < NT:
                    e_loads(i + 2)
                ex, exk, y1, y1k, y2, y2k = eq.pop(i)
                STT(ex[:], y1[:], W1all[:, i:i + 1], ex[:], ALU.mult, ALU.add, [y1k, exk, "W1all"], [exk])
                STT(ex[:], y2[:], W2all[:, i:i + 1], ex[:], ALU.mult, ALU.add, [y2k, exk, "W2all"], [exk])
                es, esk = es_r.next()
                ACT(junke[:], ex[:], AF.Square, [exk], ["junke", esk + "a"], accum_out=es[:, 0:1])
                rstd_from_ss(es[:, 2:3], es[:, 0:1], 1.0 / D, es[:, 1:2], esk + "c", esk + "a", esk + "b")
                ot, otk, otd = ot_r.next()
                STT(ot[:], ex[:], es[:, 2:3], gfB[:], ALU.mult, ALU.mult, [exk, esk + "c", "gfB"], [otk])
                DMA("sync", otd, out_d.ap()[i * 128:(i + 1) * 128, :], ot[:], [otk], [])
            p.barrier()
            p.recycle()
            p.emit()
    return nc


def make_in_maps(inputs):
    consts = host_consts()
    shared = {}
    f = lambda a: np.ascontiguousarray(np.asarray(a, dtype=np.float32))
    shared["w_in"] = f(inputs["w_in"][0])
    shared["kv_norm_g"] = f(inputs["kv_norm_g"]).reshape(1, 128)
    shared["w_uk"] = f(inputs["w_uk"][0]).reshape(128, 512)
    shared["w_uv"] = f(inputs["w_uv"][0]).reshape(128, 512)
    shared["rel_bias"] = f(inputs["rel_bias"])
    shared["ln_v_g"] = f(inputs["ln_v_g"]).reshape(1, 512)
    shared["ln_v_b"] = f(inputs["ln_v_b"]).reshape(1, 512)
    shared["w_spatial"] = f(inputs["w_spatial"][0])
    shared["b_spatial"] = f(inputs["b_spatial"][0]).reshape(1, 512)
    shared["w_proj_a"] = f(inputs["w_proj_a"][0])
    shared["w_proj_b"] = f(inputs["w_proj_b"][0])
    shared["w_out"] = f(inputs["w_out"][0])
    shared["norm1_g"] = f(f(inputs["norm1_g"][0]).reshape(8, 128).T)
    shared["norm2_g"] = f(inputs["norm2_g"]).reshape(1, D)
    shared["router_group_w"] = f(inputs["router_group_w"][0])
    shared["router_group_b"] = f(inputs["router_group_b"]).reshape(1, 4)
    shared["router_expert_w"] = f(inputs["router_expert_w"][0])
    shared["router_expert_b"] = f(inputs["router_expert_b"]).reshape(1, 32)
    shared["w_gate"] = f(inputs["w_gate"][0]).reshape(32 * 128, 2048)
    shared["w_up"] = f(inputs["w_up"][0]).reshape(32 * 128, 2048)
    shared["w_down"] = f(inputs["w_down"][0]).reshape(32 * 128, 2048)
    shared["final_norm_g"] = f(inputs["final_norm_g"]).reshape(1, D)
    shared.update(consts)
    x = f(inputs["x"])
    return [dict(shared, x=x[b]) for b in range(8)]


def kernel(**inputs):
    nc = build_program("E")
    in_maps = make_in_maps(inputs)
    res = run_bass_kernel_spmd(nc, in_maps, core_ids=list(range(8)))
    return np.stack([r["out"] for r in res.results], axis=0).astype(np.float32)
```

```python
import os
from contextlib import ExitStack
import numpy as np
import concourse.bass as bass
import concourse.mybir as mybir
from concourse.bass_utils import run_bass_kernel_spmd

F32 = mybir.dt.float32
BF16 = mybir.dt.bfloat16
I32 = mybir.dt.int32
AF = mybir.ActivationFunctionType
ALU = mybir.AluOpType
AX = mybir.AxisListType

ENGS = ("tensor", "vector", "scalar", "gpsimd", "sync")

T = 4096
D = 1024
NT = 32
P = 128
NCOLS = 4296
C_Q, C_CKV, C_QI, C_KI, C_WI, C_U, C_V, C_GA, C_GB = 0, 512, 640, 1152, 1216, 1224, 1736, 2248, 3272
NSLOT_T = 96
NSLOT = NSLOT_T * 128
EPS = 1e-6
NIT = int(os.environ.get("K_NIT", "14"))
BIG = 1.0e4


class DSem:
    def __init__(self, prog, name):
        self.sem = prog.ctx.enter_context(prog.nc.semaphore(name))
        self.count = 0
        self.id = name


class Prog:
    def __init__(self, nc, ctx):
        self.nc = nc
        self.ctx = ctx
        self.ops = {e: [] for e in ENGS}
        self.cnt = {e: 0 for e in ENGS}
        self.esem = {e: ctx.enter_context(nc.semaphore("es_" + e)) for e in ENGS}
        self.seen = {e: {} for e in ENGS}
        self.lastw = {}
        self.readers = {}
        self.dsems = []
        self.free = []
        self.inuse = []
        self.pending_noinc = {e: False for e in ENGS}

    def dsem(self, name=None):
        if self.free:
            d = self.free.pop()
        else:
            d = DSem(self, f"ds{len(self.dsems)}")
            self.dsems.append(d)
        self.inuse.append(d)
        return d

    def recycle(self):
        self.free.extend(self.inuse)
        self.inuse = []

    def _need(self, eng, ev, waits):
        sem, val, sid, src = ev
        if self.seen[eng].get(sid, 0) >= val:
            return
        if sid in waits:
            val = max(val, waits[sid][1])
        waits[sid] = (sem, val)

    def _collect(self, eng, reads, writes):
        waits = {}
        for r in reads:
            ev = self.lastw.get(r)
            if ev is not None:
                if ev[3] == eng and eng == "tensor":
                    continue
                self._need(eng, ev, waits)
        for w in writes:
            ev = self.lastw.get(w)
            if ev is not None and ev[3] != eng:
                self._need(eng, ev, waits)
            for ev in self.readers.get(w, ()):
                if ev[3] == eng:
                    continue
                self._need(eng, ev, waits)
        for sid, (sem, val) in waits.items():
            self.seen[eng][sid] = val
        return list(waits.values())

    def _record(self, ev, reads, writes):
        for r in reads:
            self.readers.setdefault(r, []).append(ev)
        for w in writes:
            self.lastw[w] = ev
            self.readers[w] = []

    def op(self, eng, fn, reads=(), writes=(), inc=True):
        waits = self._collect(eng, reads, writes)
        if inc:
            self.cnt[eng] += 1
            val = self.cnt[eng]
            self.pending_noinc[eng] = False
        else:
            val = self.cnt[eng] + 1
            self.pending_noinc[eng] = True
        ev = (self.esem[eng], val, "es_" + eng, eng)
        self._record(ev, reads, writes)
        self.ops[eng].append((waits, fn, (self.esem[eng], 1) if inc else None))
        return ev

    def dma(self, queue, ds, fn, reads=(), writes=()):
        waits = self._collect(queue, reads, writes)
        ds.count += 16
        ev = (ds.sem, ds.count, ds.id, "dma")
        self._record(ev, reads, writes)
        self.ops[queue].append((waits, fn, (ds.sem, 16)))
        return ev

    def barrier(self):
        for e in ENGS:
            waits = {}
            for e2 in ENGS:
                if e2 != e and self.cnt[e2] > 0:
                    self._need(e, (self.esem[e2], self.cnt[e2], "es_" + e2, e2), waits)
            for d in self.dsems:
                if d.count > 0:
                    self._need(e, (d.sem, d.count, d.id, "dma"), waits)
            for sid, (sem, val) in waits.items():
                self.seen[e][sid] = val
            if waits:
                self.ops[e].append((list(waits.values()), None, None))

    def emit(self):
        nc = self.nc
        for e in ENGS:
            assert not self.pending_noinc[e], f"engine {e} ends with non-inc op"
            assert self.cnt[e] < 60000, (e, self.cnt[e])
        with nc.Block() as block:
            for e in ENGS:
                ops = self.ops[e]

                def body(h, ops=ops):
                    for waits, fn, inc in ops:
                        for sem, val in waits:
                            h.wait_ge(sem, val)
                        if fn is None:
                            continue
                        ins = fn(h)
                        if inc is not None:
                            ins.then_inc(inc[0], inc[1])

                getattr(block, e)(body)
        self.ops = {e: [] for e in ENGS}


class Ring:
    def __init__(self, tiles, name, prog=None):
        self.tiles = tiles
        self.name = name
        self.i = 0
        self.ds = [prog.dsem(f"{name}_d{k}") for k in range(len(tiles))] if prog else None

    def next(self):
        k = self.i % len(self.tiles)
        self.i += 1
        if self.ds:
            return self.tiles[k], f"{self.name}{k}", self.ds[k]
        return self.tiles[k], f"{self.name}{k}"


def bc_mid(ap, n):
    a = [list(x) for x in ap.ap]
    return bass.AP(ap.tensor, ap.offset, [a[0], [0, n]] + a[1:])


def bc_last(ap, n):
    a = [list(x) for x in ap.ap]
    return bass.AP(ap.tensor, ap.offset, a + [[0, n]])


def t5_bucket_np(n):
    n = np.asarray(n, dtype=np.int32)
    nf = np.maximum(n, 1).astype(np.float32)
    large = 16 + (np.log(nf / np.float32(16)) / np.float32(np.log(128 / 16)) * np.float32(16)).astype(np.int32)
    large = np.minimum(large, 31)
    return np.where(n < 16, n, large)


def host_consts():
    c = {}
    c["c_ident"] = np.eye(128, dtype=np.float32)
    k = np.arange(128)
    c["c_tri"] = (k[:, None] <= k[None, :]).astype(np.float32)
    c["c_cneg"] = np.where(k[None, :] <= k[:, None], 0.0, -1.0e30).astype(np.float32)
    oh = np.zeros((33, 383), np.float32)
    for npr in range(383):
        n = npr - 127
        if n < 0:
            oh[32, npr] = 1.0
        else:
            oh[int(t5_bucket_np(n)), npr] = 1.0
    c["c_oh33"] = oh
    mc = np.zeros((32, 33), np.float32)
    for m in range(32):
        mc[m, m] += 1.0
        mc[31, m] -= 1.0
    c["c_mc"] = mc
    addc = np.zeros((33, 1), np.float32)
    addc[32, 0] = -30000.0
    c["c_addc"] = addc
    c["c_tokid"] = (np.arange(32)[None, :, None] * 128 + np.arange(128)[:, None, None] + np.zeros((1, 1, 16))).astype(np.int32)
    c["c_pcol"] = np.arange(128, dtype=np.float32)[:, None].copy()
    c["c_j128"] = (np.zeros((128, 1)) + np.arange(NSLOT_T)[None, :] * 128.0).astype(np.float32)
    c["c_pw"] = (np.zeros((128, 1)) + (0.5 ** np.arange(32))[None, :]).astype(np.float32)
    return c


CONST_SHAPES = {"c_ident": ([128, 128], F32), "c_tri": ([128, 128], F32), "c_cneg": ([128, 128], F32),
                "c_oh33": ([33, 383], F32), "c_mc": ([32, 33], F32), "c_addc": ([33, 1], F32),
                "c_tokid": ([128, 32, 16], I32), "c_pcol": ([128, 1], F32), "c_j128": ([128, NSLOT_T], F32), "c_pw": ([128, 32], F32)}

IN_SHAPES = {
    "x": [T, D], "w_in": [D, NCOLS], "kv_norm_g": [1, 128], "w_uk": [128, 512], "w_uv": [128, 512],
    "rel_bias": [32, 8], "ln_v_g": [1, 512], "ln_v_b": [1, 512], "w_spatial": [4, 128, 128], "b_spatial": [1, 512],
    "w_proj_a": [512, D], "w_proj_b": [512, D], "w_out": [D, D], "norm1_g": [128, 8], "norm2_g": [1, D],
    "router_group_w": [D, 4], "router_group_b": [1, 4], "router_expert_w": [D, 32], "router_expert_b": [1, 32],
    "w_gate": [32 * 128, 2048], "w_up": [32 * 128, 2048], "w_down": [32 * 128, 2048], "final_norm_g": [1, D],
}


def build_program(stage="E", debug=False):
    nc = bass.Bass("TRN2", target_bir_lowering=False)
    I = {k: nc.dram_tensor(k, s, F32, kind="ExternalInput") for k, s in IN_SHAPES.items()}
    for k, (s, dt) in CONST_SHAPES.items():
        I[k] = nc.dram_tensor(k, s, dt, kind="ExternalInput")
    out_d = nc.dram_tensor("out", [T, D], F32, kind="ExternalOutput")
    skind = "ExternalOutput" if debug else "Internal"
    QL = nc.dram_tensor("s_ql", [NT, 128, 1024], BF16, kind=skind)
    QI = nc.dram_tensor("s_qi", [NT, 128, 512], BF16, kind=skind)
    GM = nc.dram_tensor("s_gm", [NT, 128, 512], BF16, kind=skind)
    SG = nc.dram_tensor("s_sg", [NT, 128, 2048], BF16, kind=skind)
    X1 = nc.dram_tensor("s_x1", [T, D], F32, kind=skind)
    H2 = nc.dram_tensor("s_h2", [T + 128, D], BF16, kind=skind)
    SLOT = nc.dram_tensor("s_slot", [NSLOT, 16], I32, kind=skind)
    YS = nc.dram_tensor("s_ys", [NSLOT, D], F32, kind=skind)
    EBS = nc.dram_tensor("s_ebs", [8, 128, 383], F32, kind=skind)
    WEB = nc.dram_tensor("s_b_w", [32 * 128, 6144], BF16, kind="Internal")
    DBG = nc.dram_tensor("s_dbg", [NT, 128, 1024], F32, kind=skind) if debug else None

    with ExitStack() as ctx:
        ctx.enter_context(nc.allow_low_precision(reason="bf16 matmul operands / bf16 intermediates by design"))
        p = Prog(nc, ctx)

        def sbt(c, name, shape, dt):
            return c.enter_context(nc.sbuf_tensor(name, shape, dt))

        def pst(c, name, shape, dt=F32):
            return c.enter_context(nc.psum_tensor(name, shape, dt))

        def MM(out, lhsT, rhs, start, stop, reads, writes, inc=True, skip=False):
            if skip:
                return p.op("tensor", lambda e: e.matmul(out, lhsT, rhs, start=start, stop=stop, skip_group_check=True), reads, writes, inc)
            return p.op("tensor", lambda e: e.matmul(out, lhsT, rhs, start=start, stop=stop), reads, writes, inc)

        def TR(out, in_, ident, reads, writes, inc=True):
            return p.op("tensor", lambda e: e.transpose(out, in_, ident), reads, writes, inc)

        def ACT(out, in_, func, reads, writes, **kw):
            return p.op("scalar", lambda e: e.activation(out=out, in_=in_, func=func, **kw), reads, writes)

        def TS(eng, out, in0, s1, s2, op0, op1, reads, writes, accum_out=None):
            if op1 is None:
                return p.op(eng, lambda e: e.tensor_scalar(out=out, in0=in0, scalar1=s1, scalar2=None, op0=op0), reads, writes)
            if accum_out is not None:
                return p.op(eng, lambda e: e.tensor_scalar(out=out, in0=in0, scalar1=s1, scalar2=s2, op0=op0, op1=op1, accum_out=accum_out), reads, writes)
            return p.op(eng, lambda e: e.tensor_scalar(out=out, in0=in0, scalar1=s1, scalar2=s2, op0=op0, op1=op1), reads, writes)

        def TT(eng, out, in0, in1, op, reads, writes):
            return p.op(eng, lambda e: e.tensor_tensor(out=out, in0=in0, in1=in1, op=op), reads, writes)

        def STT(out, in0, scalar, in1, op0, op1, reads, writes):
            return p.op("vector", lambda e: e.scalar_tensor_tensor(out=out, in0=in0, scalar=scalar, in1=in1, op0=op0, op1=op1), reads, writes)

        def COPY(eng, out, in_, reads, writes):
            if eng == "scalar":
                return ACT(out, in_, AF.Copy, reads, writes)
            return p.op(eng, lambda e: e.tensor_copy(out=out, in_=in_), reads, writes)

        def RED(out, in_, op, reads, writes, negate=False):
            return p.op("vector", lambda e: e.tensor_reduce(out=out, in_=in_, axis=AX.X, op=op, negate=negate), reads, writes)

        def RECIP(out, in_, reads, writes):
            return p.op("vector", lambda e: e.reciprocal(out=out, in_=in_), reads, writes)

        def MEMSET(eng, ap, val, writes):
            return p.op(eng, lambda e: e.memset(ap, val), (), writes)

        def DMA(queue, ds, out, in_, reads, writes):
            return p.dma(queue, ds, lambda e: e.dma_start(out=out, in_=in_), reads, writes)

        def LOAD(queue, out, in_, key, reads=()):
            return DMA(queue, p.dsem(), out, in_, list(reads), [key])

        def GATHER(ds, out, in_, idx, reads, writes, bound=None):
            if bound is not None:
                return p.dma("gpsimd", ds, lambda e: e.indirect_dma_start(out=out, out_offset=None, in_=in_, in_offset=bass.IndirectOffsetOnAxis(ap=idx, axis=0), bounds_check=bound, oob_is_err=False), reads, writes)
            return p.dma("gpsimd", ds, lambda e: e.indirect_dma_start(out=out, out_offset=None, in_=in_, in_offset=bass.IndirectOffsetOnAxis(ap=idx, axis=0)), reads, writes)

        def SCATTER(ds, out, idx, in_, reads, writes):
            return p.dma("gpsimd", ds, lambda e: e.indirect_dma_start(out=out, out_offset=bass.IndirectOffsetOnAxis(ap=idx, axis=0), in_=in_, in_offset=None), reads, writes)

        def rstd_from_ss(rstd, ss, scale, tmp, rk, sk, tk, eps=EPS):
            TS("gpsimd", tmp, ss, scale, eps, ALU.mult, ALU.add, [sk], [tk])
            TT("gpsimd", rstd, tmp, mhalf[:, 0:1], ALU.pow, [tk, "mhalf"], [rk])

        ident_f = sbt(ctx, "ident_f", [128, 128], F32)
        ident_b = sbt(ctx, "ident_b", [128, 128], BF16)
        tri_b = sbt(ctx, "tri_b", [128, 128], BF16)
        tri_f = sbt(ctx, "tri_f", [128, 128], F32)
        ones_b = sbt(ctx, "ones_b", [128, 128], BF16)
        zeros_b = sbt(ctx, "zeros_b", [128, 128], BF16)
        mhalf = sbt(ctx, "mhalf", [128, 1], F32)
        cneg = sbt(ctx, "cneg", [128, 128], F32)
        kiT2 = sbt(ctx, "kiT2", [128, T], BF16)
        ckvT = sbt(ctx, "ckvT", [128, T], BF16)
        Vaug = sbt(ctx, "Vaug", [128, NT, 130], BF16)
        WIall = sbt(ctx, "WIall", [128, NT, 8], F32)
        LG = sbt(ctx, "LG", [128, NT, 36], F32)
        MASKall = sbt(ctx, "MASKall", [128, NT, 32], BF16)
        OH1all = sbt(ctx, "OH1all", [128, NT, 32], BF16)
        OH2all = sbt(ctx, "OH2all", [128, NT, 32], BF16)
        W1all = sbt(ctx, "W1all", [128, NT], F32)
        W2all = sbt(ctx, "W2all", [128, NT], F32)
        banks = [pst(ctx, f"bank{k}", [128, 512]) for k in range(8)]
        dconst = p.dsem("dconst")

        with ExitStack() as c0:
            oh33 = sbt(c0, "oh33", [33, 383], F32)
            mc = sbt(c0, "mc", [32, 33], F32)
            addc = sbt(c0, "addc", [33, 1], F32)
            rb = sbt(c0, "rb", [32, 8], F32)
            rbrel = sbt(c0, "rbrel", [33, 8], F32)
            rbB = sbt(c0, "rbB", [33, 8, 128], F32)
            ebrow = sbt(c0, "ebrow", [128, 8, 383], F32)
            LOAD("sync", ident_f[:], I["c_ident"].ap(), "ident_f")
            LOAD("sync", tri_f[:], I["c_tri"].ap(), "tri_f")
            LOAD("sync", cneg[:], I["c_cneg"].ap(), "cneg")
            LOAD("sync", oh33[:], I["c_oh33"].ap(), "oh33")
            LOAD("sync", mc[:], I["c_mc"].ap(), "mc")
            LOAD("sync", addc[:], I["c_addc"].ap(), "addc")
            LOAD("sync", rb[:], I["rel_bias"].ap(), "rb")
            COPY("vector", ident_b[:], ident_f[:], ["ident_f"], ["ident_b"])
            COPY("vector", tri_b[:], tri_f[:], ["tri_f"], ["tri_b"])
            MEMSET("vector", ones_b[:], 1.0, ["ones_b"])
            MEMSET("vector", zeros_b[:], 0.0, ["zeros_b"])
            MEMSET("gpsimd", mhalf[:], -0.5, ["mhalf"])
            MEMSET("vector", Vaug[:, :, 128:129], 1.0, ["Vaug_ones"])
            MM(banks[0][0:33, 0:8], mc[:], rb[:], True, True, ["mc", "rb"], ["bank0"])
            TS("vector", rbrel[:], banks[0][0:33, 0:8], addc[:, 0:1], None, ALU.add, None, ["bank0", "addc"], ["rbrel"])
            COPY("vector", rbB[:], bc_last(rbrel[:], 128), ["rbrel"], ["rbB"])
            for h in range(8):
                bk = banks[1 + (h % 4)]
                bkey = f"bank{1 + (h % 4)}"
                MM(bk[:, 0:383], rbB[:, h, :], oh33[:], True, True, ["rbB", "oh33"], [bkey])
                ACT(ebrow[:, h, :], bk[:, 0:383], AF.Exp, [bkey], [f"ebrow{h}"])
            LOAD("sync", EBS.ap().rearrange("h p n -> p h n"), ebrow[:], "EBS", reads=[f"ebrow{h}" for h in range(8)])
            p.barrier()
            p.recycle()
            p.emit()

        with ExitStack() as ca:
            Win = sbt(ca, "Win", [128, 8, NCOLS], BF16)
            Wki2 = sbt(ca, "Wki2", [128, 8, 128], BF16)
            g1 = sbt(ca, "g1", [128, 8], F32)
            wuk_n = sbt(ca, "wuk_n", [128, 512], BF16)
            WukT = sbt(ca, "WukT", [128, 8, 128], BF16)
            ws_n = sbt(ca, "ws_n", [128, 4, 128], BF16)
            WsT = sbt(ca, "WsT", [128, 4, 128], BF16)
            bsB = sbt(ca, "bsB", [128, 512], F32)
            lngB = sbt(ca, "lngB", [128, 512], F32)
            lnbB = sbt(ca, "lnbB", [128, 512], F32)
            kvgB = sbt(ca, "kvgB", [128, 128], F32)
            dwa = p.dsem("dwa")
            LOAD("sync", g1[:], I["norm1_g"].ap(), "g1")
            for c in range(8):
                LOAD("gpsimd", Win[:, c, :], I["w_in"].ap()[c * 128:(c + 1) * 128, :], f"Win{c}")
            for c in range(8):
                TS("vector", Win[:, c, :], Win[:, c, :], g1[:, c:c + 1], None, ALU.mult, None, [f"Win{c}", "g1"], [f"Win{c}"])
            for c in range(8):
                COPY("vector", Wki2[:, c, 0:64], Win[:, c, C_KI:C_KI + 64], [f"Win{c}"], ["Wki2"])
                COPY("vector", Wki2[:, c, 64:128], Win[:, c, C_KI:C_KI + 64], [f"Win{c}"], ["Wki2"])
            LOAD("gpsimd", wuk_n[:], I["w_uk"].ap(), "wuk_n")
            LOAD("gpsimd", ws_n[:], I["w_spatial"].ap().rearrange("g i j -> i g j"), "ws_n")
            LOAD("sync", bsB[:], I["b_spatial"].ap()[0:1, :].partition_broadcast(128) if False else bass.AP(I["b_spatial"], 0, [[0, 128], [1, 512]]), "bsB")
            LOAD("sync", lngB[:], bass.AP(I["ln_v_g"], 0, [[0, 128], [1, 512]]), "lngB")
            LOAD("sync", lnbB[:], bass.AP(I["ln_v_b"], 0, [[0, 128], [1, 512]]), "lnbB")
            LOAD("sync", kvgB[:], bass.AP(I["kv_norm_g"], 0, [[0, 128], [1, 128]]), "kvgB")
            MEMSET("vector", WukT[:], 0.0, ["WukT"])
            for k in range(4):
                bk = banks[k][:].bitcast(BF16)
                TR(bk[:, 0:128], wuk_n[:, k * 128:(k + 1) * 128], ident_b[:], ["wuk_n", "ident_b"], [f"bank{k}"])
                TS("vector", WukT[0:64, 2 * k, :], bk[0:64, 0:128], 0.125, None, ALU.mult, None, [f"bank{k}"], ["WukT"])
                TS("vector", WukT[64:128, 2 * k + 1, :], bk[64:128, 0:128], 0.125, None, ALU.mult, None, [f"bank{k}"], ["WukT"])
            for g in range(4):
                bk = banks[4 + g][:].bitcast(BF16)
                TR(bk[:, 0:128], ws_n[:, g, :], ident_b[:], ["ws_n", "ident_b"], [f"bank{4 + g}"])
                TT("vector", WsT[:, g, :], bk[:, 0:128], tri_b[:], ALU.mult, [f"bank{4 + g}", "tri_b"], ["WsT"])

            xin_r = Ring([sbt(ca, f"xin{k}", [128, D], F32) for k in range(4)], "xin", p)
            junk_a = sbt(ca, "junk_a", [128, D], BF16)
            ss_r = Ring([sbt(ca, f"ss{k}", [128, 4], F32) for k in range(2)], "ss")
            xs_r = Ring([sbt(ca, f"xs{k}", [128, D], BF16) for k in range(2)], "xs")
            hT_r = Ring([sbt(ca, f"hT{k}", [128, 8, 256], BF16) for k in range(2)], "hT")
            qT_r = Ring([sbt(ca, f"qT{k}", [128, 4, 128], BF16) for k in range(2)], "qT")
            ql_r = Ring([sbt(ca, f"qlt{k}", [128, 8, 128], BF16) for k in range(2)], "qlt", p)
            qi_r = Ring([sbt(ca, f"qit{k}", [128, 4, 128], BF16) for k in range(2)], "qit", p)
            gup_r = Ring([sbt(ca, f"gup{k}", [128, 4, 2, 128], BF16) for k in range(2)], "gup")
            t1_r = Ring([sbt(ca, f"ta{k}", [128, 512], F32) for k in range(3)], "ta")
            t2_r = Ring([sbt(ca, f"tb{k}", [128, 512], F32) for k in range(3)], "tb")
            gv_r = Ring([sbt(ca, f"gv{k}", [128, 512], F32) for k in range(2)], "gv")
            vn_r = Ring([sbt(ca, f"vn{k}", [128, 512], BF16) for k in range(2)], "vn")
            st_r = Ring([sbt(ca, f"st{k}", [128, 16], F32) for k in range(2)], "st")
            gm_r = Ring([sbt(ca, f"gmt{k}", [128, 512], BF16) for k in range(2)], "gmt", p)
            sg_r = Ring([sbt(ca, f"sgt{k}", [128, 2, 16, 128], BF16) for k in range(2)], "sgt", p)
            bank_i = [0]

            def nbank():
                k = bank_i[0] % 8
                bank_i[0] += 1
                return banks[k], f"bank{k}"

            def gelu_chain(ps, pk, outap, outk):
                ta, tak = t1_r.next()
                tb, tbk = t2_r.next()
                ACT(ta[:], ps, AF.Square, [pk], [tak], scale=float(np.sqrt(0.044715)))
                STT(tb[:], ta[:], 1.0, ps, ALU.add, ALU.mult, [tak, pk], [tbk])
                ACT(ta[:], tb[:], AF.Tanh, [tbk], [tak], scale=0.7978845608028654)
                STT(outap, ta[:], 1.0, ps, ALU.add, ALU.mult, [tak, pk], [outk])

            ntiles_a = NT if stage != "A1" else 2
            xin_q = {}

            def load_x(i):
                xin, xk, xd = xin_r.next()
                DMA("sync", xd, xin[:], I["x"].ap()[i * 128:(i + 1) * 128, :], [], [xk])
                xin_q[i] = (xin, xk)

            HT = {}
            XS = {}
            hstate = {}

            def fe(i):
                xin, xk = xin_q.pop(i)
                ss, ssk = ss_r.next()
                ACT(junk_a[:], xin[:], AF.Square, [xk], ["junk_a", ssk + "a"], accum_out=ss[:, 0:1])
                rstd_from_ss(ss[:, 2:3], ss[:, 0:1], 1.0 / D, ss[:, 1:2], ssk + "c", ssk + "a", ssk + "b")
                xs, xsk = xs_r.next()
                TS("vector", xs[:], xin[:], ss[:, 2:3], None, ALU.mult, None, [xk, ssk + "c"], [xsk])
                XS[i] = (xs, xsk)

            def fe_b(i):
                xs, xsk = XS.pop(i)
                bk, bkk = nbank()
                bkb = bk[:].bitcast(BF16)
                for c in range(8):
                    TR(bkb[:, c * 128:(c + 1) * 128], xs[:, c * 128:(c + 1) * 128], ident_b[:], [xsk, "ident_b"], [bkk], inc=(c == 7))
                par = i % 2
                if par == 0:
                    hstate["p"] = hT_r.next()
                hTp, hpk = hstate["p"]
                hT = hTp[:, :, par * 128:(par + 1) * 128]
                hk = hpk + f"_{par}"
                COPY("scalar", hT, bkb[:, 0:1024].rearrange("p (c t) -> p c t", t=128), [bkk], [hk])
                HT[i] = (hTp, hpk, hT, hk, par)

            def pview(bk, par):
                return bk[:, :].rearrange("p (g q t) -> p g q t", g=2, q=2)[:, :, par, :]

            def fm_pair(col0, ngroups, hTp, hkeys, wt=None):
                outb = []
                for bb in range((ngroups + 1) // 2):
                    bk, bkk = nbank()
                    ng = min(2, ngroups - 2 * bb)
                    for g in range(ng):
                        gg = 2 * bb + g
                        for c in range(8):
                            lhsT = Win[:, c, col0 + gg * 128: col0 + (gg + 1) * 128] if wt is None else wt[:, c, :]
                            MM(bk[:, g * 256:(g + 1) * 256], lhsT, hTp[:, c, :], c == 0, c == 7,
                               [f"Win{c}"] + hkeys + (["Wki2"] if wt is not None else []), [bkk], inc=(c == 7 and g == ng - 1))
                    outb.append((bk, bkk))
                return outb

            assert ntiles_a % 2 == 0
            for ii in range(min(4, ntiles_a)):
                load_x(ii)
            fe(0)
            fe(1)
            fe_b(0)
            fe_b(1)
            for m in range(ntiles_a // 2):
                i0 = 2 * m
                for ii in (i0 + 4, i0 + 5):
                    if ii < ntiles_a:
                        load_x(ii)
                hTp, hpk = HT[i0][0], HT[i0][1]
                hkeys = [hpk + "_0", hpk + "_1"]
                qb = fm_pair(C_Q, 4, hTp, hkeys)
                qTs = []
                for par in range(2):
                    qT, qk = qT_r.next()
                    for bb, (bk, bkk) in enumerate(qb):
                        COPY("scalar", qT[:, 2 * bb:2 * bb + 2, :], pview(bk, par), [bkk], [qk])
                    qTs.append((qT, qk))
                for par in range(2):
                    i = i0 + par
                    qT, qk = qTs[par]
                    qlt, qlk, qld = ql_r.next()
                    for half in range(2):
                        bk, bkk = nbank()
                        for hh in range(4):
                            h = half * 4 + hh
                            MM(bk[:, hh * 128:(hh + 1) * 128], WukT[:, h, :], qT[:, h // 2, :], True, True, ["WukT", qk], [bkk], inc=(hh == 3))
                        COPY("scalar", qlt[:, half * 4:(half + 1) * 4, :].rearrange("p c t -> p (c t)"), bk[:, :], [bkk], [qlk])
                    DMA("sync", qld, QL.ap()[i], qlt[:].rearrange("p c t -> p (c t)"), [qlk], [])
                for ii in (i0 + 2, i0 + 3):
                    if ii < ntiles_a:
                        fe(ii)
                qib = fm_pair(C_QI, 4, hTp, hkeys)
                for par in range(2):
                    i = i0 + par
                    qit, qik, qid = qi_r.next()
                    for bb, (bk, bkk) in enumerate(qib):
                        COPY("vector", qit[:, 2 * bb:2 * bb + 2, :], pview(bk, par), [bkk], [qik])
                    DMA("sync", qid, QI.ap()[i], qit[:].rearrange("p c t -> p (c t)"), [qik], [])
                (bk, bkk), = fm_pair(0, 1, hTp, hkeys, wt=Wki2)
                COPY("scalar", kiT2[:, i0 * 128:(i0 + 2) * 128], bk[:, 0:256], [bkk], [f"ki{i0}", f"ki{i0 + 1}"])
                ub = fm_pair(C_U, 4, hTp, hkeys)
                gup, gupk = gup_r.next()
                for bb, (bk, bkk) in enumerate(ub):
                    gelu_chain(bk[:, :], bkk, gup[:, 2 * bb:2 * bb + 2, :, :].rearrange("p g q t -> p (g q t)"), gupk)
                if i0 + 2 < ntiles_a:
                    fe_b(i0 + 2)
                TP = {}

                def tile_a(par):
                    i = i0 + par
                    _, _, hT, hk, _ = HT.pop(i)
                    bk, bkk = nbank()
                    for c in range(8):
                        MM(bk[:, :], hT[:, c, :], Win[:, c, C_V:C_V + 512], c == 0, c == 7, [hk, f"Win{c}"], [bkk], inc=(c == 7))
                    gv, gvk = gv_r.next()
                    gelu_chain(bk[:, :], bkk, gv[:], gvk)
                    st, stk = st_r.next()
                    p.op("vector", lambda e, o=st[:, 0:6], a=gv[:]: e.bn_stats(out=o, in_=a), [gvk], [stk + "a"])
                    p.op("vector", lambda e, o=st[:, 6:8], a=st[:, 0:6]: e.bn_aggr(out=o, in_=a), [stk + "a"], [stk + "b"])
                    rstd_from_ss(st[:, 9:10], st[:, 7:8], 1.0, st[:, 8:9], stk + "d", stk + "b", stk + "c", eps=4.0 * EPS)
                    TS("vector", gv[:], gv[:], st[:, 6:7], st[:, 9:10], ALU.subtract, ALU.mult, [gvk, stk + "b", stk + "d"], [gvk])
                    TT("vector", gv[:], gv[:], lngB[:], ALU.mult, [gvk, "lngB"], [gvk])
                    vn, vnk = vn_r.next()
                    TT("vector", vn[:], gv[:], lnbB[:], ALU.add, [gvk, "lnbB"], [vnk])
                    bk, bkk = nbank()
                    for c in range(8):
                        MM(bk[:, 0:128], hT[:, c, :], Win[:, c, C_CKV:C_CKV + 128], c == 0, c == 7, [hk, f"Win{c}"], [bkk], inc=False)
                    for c in range(8):
                        MM(bk[:, 128:136], hT[:, c, :], Win[:, c, C_WI:C_WI + 8], c == 0, c == 7, [hk, f"Win{c}"], [bkk], inc=(c == 7))
                    ACT(junk_a[:, 0:128], bk[:, 0:128], AF.Square, [bkk], ["junk_a", stk + "e"], accum_out=st[:, 10:11])
                    rstd_from_ss(st[:, 12:13], st[:, 10:11], 1.0 / 128, st[:, 11:12], stk + "g", stk + "e", stk + "f")
                    STT(Vaug[:, i, 0:128], bk[:, 0:128], st[:, 12:13], kvgB[:], ALU.mult, ALU.mult, [bkk, stk + "g", "kvgB"], [f"Vaug{i}"])
                    COPY("vector", WIall[:, i, :], bk[:, 128:136], [bkk], ["WIall"])
                    TP[par] = (vn, vnk)

                def tile_b(par):
                    i = i0 + par
                    vn, vnk = TP[par]
                    bk2, bk2k = nbank()
                    bk2b = bk2[:].bitcast(BF16)
                    TR(bk2b[:, 0:128], Vaug[:, i, 0:128], ident_b[:], [f"Vaug{i}", "ident_b"], [bk2k])
                    COPY("scalar", ckvT[:, i * 128:(i + 1) * 128], bk2b[:, 0:128], [bk2k], [f"ckvT{i}"])
                    bk, bkk = nbank()
                    for g in range(4):
                        MM(bk[:, g * 128:(g + 1) * 128], vn[:, g * 128:(g + 1) * 128], WsT[:, g, :], True, True, [vnk, "WsT"], [bkk], inc=(g == 3))
                    ta, tak = t1_r.next()
                    TT("vector", ta[:], bk[:, :], bsB[:], ALU.add, [bkk, "bsB"], [tak])
                    gmt, gmk, gmd = gm_r.next()
                    STT(gmt[:].rearrange("p (g t) -> p g t", t=128), gup[:, :, par, :], 0.5, ta[:].rearrange("p (g t) -> p g t", t=128),
                        ALU.mult, ALU.mult, [tak, gupk], [gmk])
                    DMA("sync", gmd, GM.ap()[i], gmt[:], [gmk], [])

                tile_a(0)
                tile_a(1)
                if i0 + 3 < ntiles_a:
                    fe_b(i0 + 3)
                sgp, sgk, sgd = sg_r.next()
                for b8 in range(8):
                    bk, bkk = nbank()
                    for g in range(2):
                        gg = b8 * 2 + g
                        for c in range(8):
                            MM(bk[:, g * 256:(g + 1) * 256], Win[:, c, C_GA + gg * 128:C_GA + (gg + 1) * 128], hTp[:, c, :], c == 0, c == 7,
                               [f"Win{c}"] + hkeys, [bkk], inc=(c == 7 and g == 1))
                    ta, tak = t1_r.next()
                    ACT(ta[:], bk[:, :], AF.Tanh, [bkk], [tak], scale=0.5)
                    TS("vector", sgp[:, :, b8 * 2:b8 * 2 + 2, :].rearrange("p par g t -> p g par t"),
                       ta[:].rearrange("p (g par t) -> p g par t", g=2, par=2), 0.5, 0.5, ALU.mult, ALU.add, [tak], [sgk])
                    if b8 == 3:
                        tile_b(0)
                    if b8 == 6:
                        tile_b(1)
                for pp in range(2):
                    DMA("sync", sgd, SG.ap()[i0 + pp], sgp[:, pp, :, :].rearrange("p c t -> p (c t)"), [sgk], [])
            if debug:
                ddbg = p.dsem("ddbg")
                p.barrier()
                DMA("gpsimd", ddbg, DBG.ap()[0], kiT2[:, 0:1024], [], [])
                DMA("gpsimd", ddbg, DBG.ap()[1], ckvT[:, 0:1024], [], [])
                DMA("gpsimd", ddbg, DBG.ap()[2][:, 0:260], Vaug[:, 0:2, :].rearrange("p a b -> p (a b)"), [], [])
                DMA("gpsimd", ddbg, DBG.ap()[3][:, 0:16], WIall[:, 0:2, :].rearrange("p a b -> p (a b)"), [], [])
            p.barrier()
            p.recycle()
            p.emit()

        if stage in ("A", "A1"):
            return nc

        with ExitStack() as cb:
            Wuv = sbt(cb, "Wuv", [128, 8, 128], BF16)
            Wpa = sbt(cb, "Wpa", [128, 4, D], BF16)
            Wpb = sbt(cb, "Wpb", [128, 4, D], BF16)
            Wout = sbt(cb, "Wout", [128, 8, D], BF16)
            g2B = sbt(cb, "g2B", [128, D], F32)
            Wr = sbt(cb, "Wr", [128, 8, 36], F32)
            rbias = sbt(cb, "rbias", [128, 36], F32)
            MEMSET("vector", Wuv[:], 0.0, ["Wuv"])
            BD = sbt(cb, "BD", [128, 8, 128], F32)
            BO = sbt(cb, "BO", [128, 8, 128], F32)
            for h in range(8):
                LOAD("sync", BD[:, h, :], bass.AP(EBS, h * 128 * 383 + 127, [[382, 128], [1, 128]]), f"BD{h}", reads=["EBS"])
                LOAD("sync", BO[:, h, :], bass.AP(EBS, h * 128 * 383 + 255, [[382, 128], [1, 128]]), f"BO{h}", reads=["EBS"])
            wuv_v = I["w_uv"].ap().rearrange("r (h d) -> r h d", d=64)
            LOAD("gpsimd", Wuv[:, 0::2, 0:64], wuv_v[:, 0::2, :], "Wuv")
            LOAD("gpsimd", Wuv[:, 1::2, 64:128], wuv_v[:, 1::2, :], "Wuv")
            LOAD("gpsimd", Wpa[:], I["w_proj_a"].ap().rearrange("(k p) n -> p k n", p=128), "Wpa")
            LOAD("gpsimd", Wpb[:], I["w_proj_b"].ap().rearrange("(k p) n -> p k n", p=128), "Wpb")
            LOAD("gpsimd", Wout[:], I["w_out"].ap().rearrange("(c p) n -> p c n", p=128), "Wout")
            LOAD("sync", g2B[:], bass.AP(I["norm2_g"], 0, [[0, 128], [1, D]]), "g2B")
            LOAD("sync", Wr[:, :, 0:4], I["router_group_w"].ap().rearrange("(c p) n -> p c n", p=128), "Wr")
            LOAD("sync", Wr[:, :, 4:36], I["router_expert_w"].ap().rearrange("(c p) n -> p c n", p=128), "Wr")
            LOAD("sync", rbias[:, 0:4], bass.AP(I["router_group_b"], 0, [[0, 128], [1, 4]]), "rbias")
            LOAD("sync", rbias[:, 4:36], bass.AP(I["router_expert_b"], 0, [[0, 128], [1, 32]]), "rbias")

            bql_r = Ring([sbt(cb, f"bql{k}", [128, 8, 128], BF16) for k in range(3)], "bql", p)
            bqi_r = Ring([sbt(cb, f"bqi{k}", [128, 8, 128], BF16) for k in range(2)], "bqi", p)
            for k in range(2):
                MEMSET("gpsimd", bqi_r.tiles[k][:], 0.0, [f"bqi{k}"])
            idx2 = [sbt(cb, f"idx{k}", [128, T], F32) for k in range(2)]
            mask = sbt(cb, "mask", [128, T], BF16)
            maskT = sbt(cb, "maskT", [128, NT, 128], BF16)
            rl_r = Ring([sbt(cb, f"rl{k}", [128, 512], BF16) for k in range(4)], "rl")
            Dg_r = Ring([sbt(cb, f"Dg{k}", [128, 8, 128], BF16) for k in range(2)], "Dg")
            aw_r = Ring([sbt(cb, f"aw{k}", [128, 16], F32) for k in range(2)], "aw")
            sc_r = Ring([sbt(cb, f"sc{k}", [128, 64], F32) for k in range(4)], "sc")
            pw = sbt(cb, "pw", [128, 32], F32)
            LOAD("sync", pw[:], I["c_pw"].ap(), "pw")
            MnD = sbt(cb, "MnD", [128, 8, 128], BF16)
            MnO = sbt(cb, "MnO", [128, 8, 128], BF16)
            P_r = Ring([sbt(cb, f"Pt{k}", [128, 8, 128], BF16) for k in range(4)], "Pt")
            OL = sbt(cb, "OL", [128, 8, 128], BF16)
            rden = sbt(cb, "rden", [128, 8], F32)
            OLT = sbt(cb, "OLT", [128, 8, 128], BF16)
            oaT = sbt(cb, "oaT", [128, 4, 128], BF16)
            bgm_r = Ring([sbt(cb, f"bgm{k}", [128, 512], BF16) for k in range(2)], "bgm", p)
            bsg_r = Ring([sbt(cb, f"bsg{k}", [128, 16, 128], BF16) for k in range(2)], "bsg", p)
            xr_r = Ring([sbt(cb, f"xr{k}", [128, D], F32) for k in range(2)], "xr", p)
            x1_ds = [p.dsem(f"x1d{k}") for k in range(2)]
            t12 = sbt(cb, "t12", [128, 512], F32)
            mT = sbt(cb, "mT", [128, 8, 128], BF16)
            junkb = sbt(cb, "junkb", [128, D], BF16)
            h2f = sbt(cb, "h2f", [128, D], F32)
            h2b_r = Ring([sbt(cb, f"h2b{k}", [128, D], BF16) for k in range(2)], "h2b", p)
            h2T = sbt(cb, "h2T", [128, 8, 128], F32)
            bank5_i = [0]

            def nbank5():
                k = bank5_i[0] % 3
                bank5_i[0] += 1
                return banks[k], f"bank{k}"

            def flat(ap):
                return ap.rearrange("p a b -> p (a b)")

            ntiles_b = NT if stage not in ("B1",) else 3
            TS_ = {}

            def P1_load(i):
                t = TS_[i] = {}
                qlt, qlk, qld = bql_r.next()
                DMA("sync", qld, flat(qlt[:]), QL.ap()[i], [], [qlk])
                qip, qik, qid = bqi_r.next()
                qi_v = QI.ap()[i].rearrange("p (k t) -> p k t", t=128)
                DMA("sync", qid, qip[0:64, 0::2, :], qi_v[0:64, :, :], [], [qik])
                DMA("sync", qid, qip[64:128, 1::2, :], qi_v[64:128, :, :], [], [qik])
                sc, sck = sc_r.next()
                t.update(qlt=qlt, qlk=qlk, qip=qip, qik=qik, sc=sc, sck=sck, idx=idx2[i % 2], idxk=f"idx{i % 2}")

            def out_loads(i):
                t = TS_[i]
                gmt, gmk, gmd = bgm_r.next()
                DMA("sync", gmd, gmt[:], GM.ap()[i], [], [gmk])
                sgt, sgk, sgd = bsg_r.next()
                DMA("sync", sgd, flat(sgt[:]), SG.ap()[i], [], [sgk])
                xres, xk, xd = xr_r.next()
                DMA("sync", xd, xres[:], I["x"].ap()[i * 128:(i + 1) * 128, :], [], [xk])
                t.update(gmt=gmt, gmk=gmk, sgt=sgt, sgk=sgk, xres=xres, xk=xk)

            def idx_units(i):
                S = (i + 1) * 128
                t = TS_[i]
                qip, qik, idx, idxk = t["qip"], t["qik"], t["idx"], t["idxk"]
                Dg, Dgk = Dg_r.next()
                TT("vector", Dg[:], bc_mid(ident_b[:], 8), bc_last(WIall[:, i, :], 128), ALU.mult, ["ident_b", "WIall"], [Dgk])
                units = []
                accb, acck = banks[4], "bank4"
                nch = (S + 511) // 512
                st = {"q": []}
                LAGD = 2
                for ch in range(nch):
                    w = min(512, S - ch * 512)
                    kkeys = [f"ki{j}" for j in range(ch * 4, ch * 4 + w // 128)]

                    def diag(pend, w=w, last=False):
                        ph, prl, prlk = pend
                        MM(accb[:, 0:w], Dg[:, ph, :], prl[:, 0:w], ph == 0, last, [Dgk, prlk], [acck])

                    for h in range(8):
                        def unit(ch=ch, h=h, w=w, kkeys=kkeys, diag=diag):
                            bk, bkk = nbank5()
                            MM(bk[:, 0:w], qip[:, h, :], kiT2[:, ch * 512: ch * 512 + w], True, True, [qik] + kkeys, [bkk])
                            rl, rlk = rl_r.next()
                            ACT(rl[:, 0:w], bk[:, 0:w], AF.Relu, [bkk], [rlk])
                            st["q"].append((h, rl, rlk))
                            if len(st["q"]) > LAGD:
                                diag(st["q"].pop(0))
                        units.append(unit)

                    def fin(ch=ch, w=w, diag=diag):
                        while len(st["q"]) > 1:
                            diag(st["q"].pop(0))
                        diag(st["q"].pop(0), last=True)
                        COPY("scalar", idx[:, ch * 512: ch * 512 + w], accb[:, 0:w], [acck], [idxk])
                    units.append(fin)
                return units

            def P1_fin(i):
                S = (i + 1) * 128
                t = TS_[i]
                idx, idxk, sc, sck = t["idx"], t["idxk"], t["sc"], t["sck"]
                dg = idx[:, i * 128:(i + 1) * 128]
                TT("vector", dg, dg, cneg[:], ALU.add, [idxk, "cneg"], [idxk])
                lo, hi, wid, cnd, cnt, uu = [sc[:, q:q + 1] for q in range(6)]
                Hx = sc[:, 16:16 + NIT + 1]
                if i >= 2:
                    RED(lo, idx[:, 0:256], ALU.min, [idxk], [sck])
                    RED(hi, idx[:, 0:S], ALU.max, [idxk, sck], [sck])
                    TT("vector", wid, hi, lo, ALU.subtract, [sck], [sck])
                    TS("vector", Hx, pw[:, 0:NIT + 1], wid, None, ALU.mult, None, ["pw", sck], [sck])
                    TT("vector", cnd, lo, Hx[:, 1:2], ALU.add, [sck], [sck])
                else:
                    MEMSET("vector", lo, -1.0e29, [sck])

            def bis(i, k):
                S = (i + 1) * 128
                sc, sck, idx, idxk = TS_[i]["sc"], TS_[i]["sck"], TS_[i]["idx"], TS_[i]["idxk"]
                lo, hi, wid, cnd, cnt, uu = [sc[:, q:q + 1] for q in range(6)]
                Hx = sc[:, 16:16 + NIT + 1]
                TS("vector", mask[:, 0:S], idx[:, 0:S], cnd, None, ALU.is_ge, ALU.add, [idxk, sck], ["mask", sck], accum_out=cnt)
                if k < NIT - 1:
                    TS("vector", uu, cnt, 255.5, Hx[:, k + 1:k + 2], ALU.is_ge, ALU.mult, [sck], [sck])
                    STT(cnd, cnd, Hx[:, k + 2:k + 3], uu, ALU.subtract, ALU.add, [sck], [sck])
                else:
                    TS("vector", uu, cnt, 255.5, Hx[:, NIT:NIT + 1], ALU.is_ge, ALU.mult, [sck], [sck])
                    STT(lo, cnd, Hx[:, NIT:NIT + 1], uu, ALU.subtract, ALU.add, [sck], [sck])

            def P1c(i):
                S = (i + 1) * 128
                sc, sck, idx, idxk = TS_[i]["sc"], TS_[i]["sck"], TS_[i]["idx"], TS_[i]["idxk"]
                TS("vector", mask[:, 0:S], idx[:, 0:S], sc[:, 0:1], None, ALU.is_ge, None, [idxk, sck], ["mask"])

            def P2(i):
                for j0 in range(0, i + 1, 8):
                    nb = min(8, i + 1 - j0)
                    bk, bkk = nbank5()
                    bkb = bk[:].bitcast(BF16)
                    for jj in range(nb):
                        TR(bkb[:, jj * 128:(jj + 1) * 128], mask[:, (j0 + jj) * 128:(j0 + jj + 1) * 128], ident_b[:], ["mask", "ident_b"], [bkk], inc=(jj == nb - 1))
                    COPY("scalar", flat(maskT[:, j0:j0 + nb, :]), bkb[:, 0:nb * 128], [bkk], [f"mT{j}" for j in range(j0, j0 + nb)])
                TT("gpsimd", MnD[:], BD[:], bc_mid(maskT[:, i, :], 8), ALU.mult, [f"BD{h}" for h in range(8)] + [f"mT{i}"], ["MnD"])
                if i >= 1:
                    TT("gpsimd", MnO[:], BO[:], bc_mid(maskT[:, i - 1, :], 8), ALU.mult, [f"BO{h}" for h in range(8)] + [f"mT{i - 1}"], ["MnO"])

            def blk_pro(i):
                for b in range(3):
                    MM(banks[5 + b][:, :], zeros_b[:], ckvT[:, 0:512], True, True, ["zeros_b"] + [f"ckvT{j}" for j in range(4)], [f"bank{5 + b}"])

            PQ = {}

            def blk_j(i, j):
                blk_score(i, j)
                if j >= 3:
                    blk_pv(i, j - 3)

            def blk_score(i, j):
                qlt, qlk = TS_[i]["qlt"], TS_[i]["qlk"]
                Pt, Pk = P_r.next()
                PQ[(i, j)] = (Pt, Pk)
                for g in range(2):
                    bk, bkk = nbank5()
                    MM(bk[:, :], ckvT[:, j * 128:(j + 1) * 128], flat(qlt[:, g * 4:(g + 1) * 4, :]), True, True, [f"ckvT{j}", qlk], [bkk])
                    ACT(flat(Pt[:, g * 4:(g + 1) * 4, :]), bk[:, :], AF.Exp, [bkk], [Pk])
                if j == i:
                    TT("gpsimd", Pt[:], Pt[:], MnD[:], ALU.mult, [Pk, "MnD"], [Pk])
                elif j == i - 1:
                    TT("gpsimd", Pt[:], Pt[:], MnO[:], ALU.mult, [Pk, "MnO"], [Pk])
                else:
                    TT("gpsimd", Pt[:], Pt[:], bc_mid(maskT[:, j, :], 8), ALU.mult, [Pk, f"mT{j}"], [Pk])

            def blk_pv(i, j):
                Pt, Pk = PQ.pop((i, j))
                for h in range(8):
                    b = 5 + h // 3
                    MM(banks[b][:, (h % 3) * 130:(h % 3) * 130 + 129], Pt[:, h, :], Vaug[:, j, 0:129], False, False,
                       [Pk, f"Vaug{j}", "Vaug_ones"], [f"bank{b}"], inc=(h == 7), skip=True)

            def blk_epi(i):
                for jj in range(max(0, i - 2), i + 1):
                    blk_pv(i, jj)
                for b in range(3):
                    nb = 3 if b < 2 else 2
                    RECIP(rden[:, 3 * b:3 * b + nb], banks[5 + b][:, 128:128 + 130 * (nb - 1) + 1:130], [f"bank{5 + b}"], ["rden"])
                for h in range(8):
                    b = 5 + h // 3
                    ACT(OL[:, h, :], banks[b][:, (h % 3) * 130:(h % 3) * 130 + 128], AF.Copy, [f"bank{b}", "rden"], ["OL"], scale=rden[:, h:h + 1])
                bk, bkk = nbank5()
                bkb = bk[:].bitcast(BF16)
                for h in range(8):
                    TR(bkb[:, h * 128:(h + 1) * 128], OL[:, h, :], ident_b[:], ["OL", "ident_b"], [bkk], inc=(h == 7))
                COPY("scalar", flat(OLT[:]), bkb[:, 0:1024], [bkk], ["OLT"])
                bk, bkk = nbank5()
                for k in range(4):
                    for hh in range(2):
                        MM(bk[:, k * 128:(k + 1) * 128], Wuv[:, 2 * k + hh, :], OLT[:, 2 * k + hh, :], hh == 0, hh == 1, ["Wuv", "OLT"], [bkk], inc=(k == 3 and hh == 1))
                COPY("scalar", flat(oaT[:]), bk[:, :], [bkk], ["oaT"])

            def out_stages(i):
                t = TS_[i]
                gmt, gmk, sgt, sgk, xres, xk, sc, sck = t["gmt"], t["gmk"], t["sgt"], t["sgk"], t["xres"], t["xk"], t["sc"], t["sck"]
                hb = {}

                B3, B3k = banks[3], "bank3"

                def yq(q):
                    for mm in range(2):
                        m = 2 * q + mm
                        for kc in range(4):
                            MM(B3[:, mm * 128:(mm + 1) * 128], Wpa[:, kc, m * 128:(m + 1) * 128], oaT[:, kc, :], kc == 0, kc == 3, ["Wpa", "oaT"], [B3k], inc=False)
                    for mm in range(2):
                        m = 2 * q + mm
                        for kc in range(4):
                            MM(B3[:, 256 + mm * 128:256 + (mm + 1) * 128], Wpb[:, kc, m * 128:(m + 1) * 128], gmt[:, kc * 128:(kc + 1) * 128], kc == 0, kc == 3, ["Wpb", gmk], [B3k], inc=(kc == 3 and mm == 1))

                def gq(q):
                    sgv = sgt[:].rearrange("p (a g) t -> p a g t", a=2)[:, :, 2 * q:2 * q + 2, :]
                    TT("vector", t12[:].rearrange("p (a g t) -> p a g t", a=2, g=2), B3[:, :].rearrange("p (a g t) -> p a g t", a=2, g=2), sgv,
                       ALU.mult, [B3k, sgk], ["t12"])
                    TT("gpsimd", flat(mT[:, 2 * q:2 * q + 2, :]), t12[:, 0:256], t12[:, 256:512], ALU.add, ["t12"], ["mT"])

                def wout(half):
                    for c in range(8):
                        MM(B3[:, :], mT[:, c, :], Wout[:, c, half * 512:(half + 1) * 512], c == 0, c == 7, ["mT", "Wout"], [B3k], inc=(c == 7))

                def xadd(half):
                    xs_ = xres[:, half * 512:(half + 1) * 512]
                    TT("vector", xs_, B3[:, :], xs_, ALU.add, [B3k, xk], [xk])

                def norm2():
                    DMA("sync", x1_ds[i % 2], X1.ap()[i * 128:(i + 1) * 128, :], xres[:], [xk], [])
                    ACT(junkb[:], xres[:], AF.Square, [xk], ["junkb", sck + "n"], accum_out=sc[:, 40:41])
                    rstd_from_ss(sc[:, 42:43], sc[:, 40:41], 1.0 / D, sc[:, 41:42], sck + "n3", sck + "n", sck + "n2")
                    STT(h2f[:], xres[:], sc[:, 42:43], g2B[:], ALU.mult, ALU.mult, [xk, sck + "n3", "g2B"], ["h2f"])
                    h2b, h2bk, h2bd = h2b_r.next()
                    COPY("scalar", h2b[:], h2f[:], ["h2f"], [h2bk])
                    DMA("sync", h2bd, H2.ap()[i * 128:(i + 1) * 128, :], h2b[:], [h2bk], [])

                def trh(half):
                    for cc in range(4):
                        c = half * 4 + cc
                        TR(B3[:, cc * 128:(cc + 1) * 128], h2f[:, c * 128:(c + 1) * 128], ident_f[:], ["h2f", "ident_f"], [B3k], inc=(cc == 3))
                    COPY("scalar", flat(h2T[:, half * 4:(half + 1) * 4, :]), B3[:, :], [B3k], ["h2T"])

                def rmm():
                    for c in range(8):
                        MM(B3[:, 0:36], h2T[:, c, :], Wr[:, c, :], c == 0, c == 7, ["h2T", "Wr"], [B3k], inc=(c == 7))
                    hb[20] = (B3, B3k)

                stages = [
                    lambda: yq(0), lambda: gq(0),
                    lambda: yq(1), lambda: gq(1),
                    lambda: yq(2), lambda: gq(2),
                    lambda: yq(3), lambda: gq(3),
                    lambda: wout(0), lambda: xadd(0),
                    lambda: wout(1), lambda: (xadd(1), norm2()),
                    lambda: trh(0),
                    lambda: trh(1),
                    lambda: rmm(),
                ]

                def s7():
                    bk, bkk = hb[20]
                    TT("vector", LG[:, i, :], bk[:, 0:36], rbias[:], ALU.add, [bkk, "rbias"], ["LG"])

                return stages + [s7]

            def run_units(us):
                for u in us:
                    u()

            for i0 in range(min(2, ntiles_b)):
                P1_load(i0)
                run_units(idx_units(i0))
                P1_fin(i0)
            P1c(0)
            P2(0)
            web_ds = {wn: p.dsem() for wn in ("w_gate", "w_up", "w_down")}
            for it in range(ntiles_b):
                for wq, wn in enumerate(("w_gate", "w_up", "w_down")):
                    DMA("gpsimd", web_ds[wn], WEB.ap()[it * 128:(it + 1) * 128, wq * 2048:(wq + 1) * 2048], I[wn].ap()[it * 128:(it + 1) * 128, :], [], [])
                if it >= 1:
                    out_loads(it - 1)
                U = []
                if it + 2 < ntiles_b:
                    P1_load(it + 2)
                    U = idx_units(it + 2)
                nxt = it + 1 < ntiles_b
                blk_pro(it)
                nb = it + 1
                nk = NIT if (nxt and it + 1 >= 2) else 0
                ost = out_stages(it - 1) if it >= 1 else []
                NS = 16
                Tn = max(nb, nk, NS)
                nu = len(U)
                for tck in range(Tn):
                    for k in range((tck * nk) // Tn, ((tck + 1) * nk) // Tn):
                        bis(it + 1, k)
                    for j in range((tck * nb) // Tn, ((tck + 1) * nb) // Tn):
                        blk_j(it, j)
                    for q in range((tck * nu) // Tn, ((tck + 1) * nu) // Tn):
                        U[q]()
                    for si, st_fn in enumerate(ost):
                        if (si * Tn) // NS == tck:
                            st_fn()
                if nxt:
                    P1c(it + 1)
                    P2(it + 1)
                blk_epi(it)
                if it + 2 < ntiles_b:
                    P1_fin(it + 2)
            out_loads(ntiles_b - 1)
            for st_fn in out_stages(ntiles_b - 1):
                st_fn()
            if debug and stage in ("B", "B1"):
                ddbg = p.dsem("ddbg2")
                p.barrier()
                DMA("gpsimd", ddbg, DBG.ap()[6][:, 0:NT], W1all[:], [], [])
                DMA("gpsimd", ddbg, DBG.ap()[7][:, 0:NT], W2all[:], [], [])
                DMA("gpsimd", ddbg, DBG.ap()[8][:, 0:1024], flat(OH1all[:]), [], [])
                DMA("gpsimd", ddbg, DBG.ap()[9][:, 0:1024], flat(OH2all[:]), [], [])
            p.barrier()
            p.recycle()
            p.emit()
        if stage in ("B", "B1"):
            return nc

        with ExitStack() as cr:
            gl = LG[:, :, 0:4]
            el4 = LG[:, :, 4:36].rearrange("p i (g j) -> p i g j", j=8)
            r_gmax = sbt(cr, "r_gmax", [128, NT], F32)
            r_gsh = sbt(cr, "r_gsh", [128, NT, 4], F32)
            r_ge = sbt(cr, "r_ge", [128, NT, 4], F32)
            r_gsum = sbt(cr, "r_gsum", [128, NT], F32)
            r_gw = sbt(cr, "r_gw", [128, NT], F32)
            r_goh = sbt(cr, "r_goh", [128, NT, 4], F32)
            r_gpen = sbt(cr, "r_gpen", [128, NT, 4], F32)
            r_elm = sbt(cr, "r_elm", [128, NT, 32], F32)
            r_elm2 = sbt(cr, "r_elm2", [128, NT, 32], F32)
            r_m1 = sbt(cr, "r_m1", [128, NT], F32)
            r_m2 = sbt(cr, "r_m2", [128, NT], F32)
            r_dd = sbt(cr, "r_dd", [128, NT], F32)
            r_ee = sbt(cr, "r_ee", [128, NT], F32)
            r_s1 = sbt(cr, "r_s1", [128, NT], F32)
            RED(r_gmax[:], gl, ALU.max, ["LG"], ["r_gmax"])
            TT("vector", r_gsh[:], gl, bc_last(r_gmax[:], 4), ALU.subtract, ["LG", "r_gmax"], ["r_gsh"])
            ACT(r_ge[:], r_gsh[:], AF.Exp, ["r_gsh"], ["r_ge"])
            RED(r_gsum[:], r_ge[:], ALU.add, ["r_ge"], ["r_gsum"])
            RECIP(r_gw[:], r_gsum[:], ["r_gsum"], ["r_gw"])
            TS("vector", r_goh[:], r_gsh[:], 0.0, None, ALU.is_ge, None, ["r_gsh"], ["r_goh"])
            TS("vector", r_gpen[:], r_goh[:], 1.0, BIG, ALU.subtract, ALU.mult, ["r_goh"], ["r_gpen"])
            TT("vector", r_elm[:].rearrange("p i (g j) -> p i g j", j=8), el4, bc_last(r_gpen[:], 8), ALU.add, ["LG", "r_gpen"], ["r_elm"])
            RED(r_m1[:], r_elm[:], ALU.max, ["r_elm"], ["r_m1"])
            TT("vector", OH1all[:], r_elm[:], bc_last(r_m1[:], 32), ALU.is_ge, ["r_elm", "r_m1"], ["OH1all"])
            STT(r_elm2[:], OH1all[:], -BIG, r_elm[:], ALU.mult, ALU.add, ["OH1all", "r_elm"], ["r_elm2"])
            RED(r_m2[:], r_elm2[:], ALU.max, ["r_elm2"], ["r_m2"])
            TT("vector", OH2all[:], r_elm2[:], bc_last(r_m2[:], 32), ALU.is_ge, ["r_elm2", "r_m2"], ["OH2all"])
            TT("vector", r_dd[:], r_m2[:], r_m1[:], ALU.subtract, ["r_m1", "r_m2"], ["r_dd"])
            ACT(r_ee[:], r_dd[:], AF.Exp, ["r_dd"], ["r_ee"])
            TS("vector", r_ee[:], r_ee[:], 1.0, None, ALU.add, None, ["r_ee"], ["r_ee"])
            RECIP(r_s1[:], r_ee[:], ["r_ee"], ["r_s1"])
            TT("vector", W1all[:], r_gw[:], r_s1[:], ALU.mult, ["r_gw", "r_s1"], ["W1all"])
            TT("vector", W2all[:], r_gw[:], W1all[:], ALU.subtract, ["r_gw", "W1all"], ["W2all"])
            TT("vector", MASKall[:], OH1all[:], OH2all[:], ALU.add, ["OH1all", "OH2all"], ["MASKall"])
            p.barrier()
            p.recycle()
            p.emit()

        with ExitStack() as cd:
            tokid = sbt(cd, "tokid", [128, NT, 16], I32)
            pcol = sbt(cd, "pcol", [128, 1], F32)
            j128 = sbt(cd, "j128", [128, NSLOT_T], F32)
            gfB = sbt(cd, "gfB", [128, D], F32)
            LOAD("sync", tokid[:], I["c_tokid"].ap(), "tokid")
            LOAD("sync", pcol[:], I["c_pcol"].ap(), "pcol")
            LOAD("sync", j128[:], I["c_j128"].ap(), "j128")
            LOAD("sync", gfB[:], bass.AP(I["final_norm_g"], 0, [[0, 128], [1, D]]), "gfB")
            for i in range(NT):
                bk = banks[i // 16]
                col = (i % 16) * 32
                MM(bk[:, col:col + 32], tri_b[:], MASKall[:, i, :], True, i == 0, ["tri_b", "MASKall"], [f"bank{i // 16}"], inc=(i == 0), skip=True)
                for i2 in range(i):
                    MM(bk[:, col:col + 32], ones_b[:], MASKall[:, i2, :], False, i2 == i - 1, ["ones_b", "MASKall"], [f"bank{i // 16}"], inc=(i2 == i - 1), skip=True)
            for i in range(NT):
                MM(banks[2][:, 0:32], ones_b[:], MASKall[:, i, :], i == 0, i == NT - 1, ["ones_b", "MASKall"], ["bank2"], inc=(i == NT - 1), skip=True)
            ci = sbt(cd, "ci", [128, 32], I32)
            padf = sbt(cd, "padf", [128, 32], F32)
            pa = sbt(cd, "pa", [128, 32], F32)
            pb = sbt(cd, "pb", [128, 32], F32)
            bm1 = sbt(cd, "bm1", [128, 32], F32)
            TS("vector", ci[:], banks[2][:, 0:32], 127.0, None, ALU.add, None, ["bank2"], ["ci"])
            p.op("vector", lambda e: e.tensor_scalar(out=ci[:], in0=ci[:], scalar1=7, scalar2=7, op0=ALU.logical_shift_right, op1=ALU.logical_shift_left), ["ci"], ["ci"])
            COPY("vector", padf[:], ci[:], ["ci"], ["padf"])
            COPY("vector", pa[:], padf[:], ["padf"], ["pa"])
            src, srck, dst, dstk = pa, "pa", pb, "pb"
            for sft in (1, 2, 4, 8, 16):
                COPY("vector", dst[:, 0:sft], src[:, 0:sft], [srck], [dstk])
                TT("vector", dst[:, sft:32], src[:, sft:32], src[:, 0:32 - sft], ALU.add, [srck], [dstk])
                src, srck, dst, dstk = dst, dstk, src, srck
            incl, inclk = src, srck
            TT("vector", bm1[:], incl[:], padf[:], ALU.subtract, [inclk, "padf"], ["bm1"])
            TS("vector", bm1[:], bm1[:], -1.0, None, ALU.add, None, ["bm1"], ["bm1"])
            tmpAll = sbt(cd, "tmpAll", [128, NT, 32], F32)
            prod = sbt(cd, "prod", [128, NT, 32], F32)
            Sf = sbt(cd, "Sf", [128, 2, NT], F32)
            Si = sbt(cd, "Si", [128, 2, NT], I32)
            for b in range(2):
                TT("vector", tmpAll[:, b * 16:(b + 1) * 16, :], banks[b][:, :].rearrange("p (a b) -> p a b", b=32), bc_mid(bm1[:], 16), ALU.add, [f"bank{b}", "bm1"], ["tmpAll"])
            for k, OH in enumerate((OH1all, OH2all)):
                TT("vector", prod[:], tmpAll[:], OH[:], ALU.mult, ["tmpAll", "OH1all", "OH2all"], ["prod"])
                RED(Sf[:, k, :], prod[:], ALU.add, ["prod"], ["Sf"])
            COPY("vector", Si[:], Sf[:], ["Sf"], ["Si"])
            cmp = sbt(cd, "cmp", [128, NSLOT_T, 32], F32)
            texp = sbt(cd, "texp", [128, NSLOT_T], F32)
            IDXW = sbt(cd, "IDXW", [128, NSLOT_T], I32)
            TT("vector", cmp[:], bc_mid(incl[:], NSLOT_T), bc_last(j128[:], 32), ALU.is_le, [inclk, "j128"], ["cmp"])
            RED(texp[:], cmp[:], ALU.add, ["cmp"], ["texp"])
            TS("vector", texp[:], texp[:], 31.0, 128.0, ALU.min, ALU.mult, ["texp"], ["texp"])
            TS("vector", texp[:], texp[:], pcol[:, 0:1], None, ALU.add, None, ["texp", "pcol"], ["texp"])
            COPY("vector", IDXW[:], texp[:], ["texp"], ["IDXW"])
            si = sbt(cd, "si", [128, 16], I32)
            zrow = sbt(cd, "zrow", [128, D], BF16)
            MEMSET("gpsimd", si[:], T, ["si"])
            MEMSET("gpsimd", zrow[:], 0.0, ["zrow"])
            LOAD("sync", SLOT.ap().rearrange("(j p) c -> p j c", p=128), bc_mid(si[:], NSLOT_T), "SLOTinit", reads=["si"])
            LOAD("sync", H2.ap()[T:T + 128, :], zrow[:], "H2zero", reads=["zrow"])
            sc_ds = [p.dsem(f"scat{k}") for k in range(4)]
            sckeys = []
            n = 0
            for i in range(NT):
                for k in range(2):
                    SCATTER(sc_ds[n % 4], SLOT.ap(), Si[:, k, i:i + 1], tokid[:, i, :], ["Si", "tokid", "SLOTinit"], [f"SLOTs{n}"])
                    sckeys.append(f"SLOTs{n}")
                    n += 1
            stk_r = Ring([sbt(cd, f"stk{k}", [128, 16], I32) for k in range(4)], "stk", p)
            Hs_r = Ring([sbt(cd, f"Hs{k}", [128, D], BF16) for k in range(4)], "Hs", p)
            Wa_r = Ring([sbt(cd, f"Wa{k}", [128, 6144], BF16) for k in range(5)], "Wa", p)
            yt_r = Ring([sbt(cd, f"yt{k}", [128, D], F32) for k in range(2)], "yt", p)
            bank8_i = [0]

            def nbank8():
                k = bank8_i[0] % 8
                bank8_i[0] += 1
                return banks[k], f"bank{k}"

            nslot_t = NSLOT_T
            yskeys = []
            dq = {}

            def d_loads(j):
                stk, stkk, stkd = stk_r.next()
                DMA("sync", stkd, stk[:], SLOT.ap()[j * 128:(j + 1) * 128, :], sckeys + ["SLOTinit"], [stkk])
                Hs, Hsk, Hsd = Hs_r.next()
                GATHER(Hsd, Hs[:], H2.ap(), stk[:, 0:1], [stkk, "H2zero"], [Hsk])
                Wa, Wak, Wad = Wa_r.next()
                GATHER(Wad, Wa[:], WEB.ap(), IDXW[:, j:j + 1], ["IDXW"], [Wak])
                dq[j] = (Hs, Hsk, Wa[:, 0:2048], Wak, Wa[:, 2048:4096], Wak, Wa[:, 4096:6144], Wak)

            HsT_r = Ring([sbt(cd, f"HsT{k}", [128, 8, 128], BF16) for k in range(2)], "HsT")
            he_r = Ring([sbt(cd, f"he{k}", [128, 256], BF16) for k in range(2)], "he")
            heT_r = Ring([sbt(cd, f"heT{k}", [128, 256], BF16) for k in range(2)], "heT")
            sgs_r = Ring([sbt(cd, f"sgs{k}", [128, 256], F32) for k in range(2)], "sgs")
            DS = {}

            def sA(j):
                Hs, Hsk = dq[j][0], dq[j][1]
                bk, bkk = nbank8()
                bkb = bk[:].bitcast(BF16)
                for c in range(8):
                    TR(bkb[:, c * 128:(c + 1) * 128], Hs[:, c:D:8], ident_b[:], [Hsk, "ident_b"], [bkk], inc=(c == 7))
                HsT_, HsTk = HsT_r.next()
                COPY("scalar", flat(HsT_[:]), bkb[:, 0:1024], [bkk], [HsTk])
                DS[j] = {"HsT": (HsT_, HsTk)}

            def sB(j):
                _, _, Wg, Wgk, Wu, Wuk_, _, _ = dq[j]
                HsT_, HsTk = DS[j]["HsT"]
                bk, bkk = nbank8()
                for c in range(8):
                    MM(bk[:, 0:256], HsT_[:, c, :], Wg[:, c * 256:(c + 1) * 256], c == 0, c == 7, [Wgk, HsTk], [bkk], inc=False)
                for c in range(8):
                    MM(bk[:, 256:512], HsT_[:, c, :], Wu[:, c * 256:(c + 1) * 256], c == 0, c == 7, [Wuk_, HsTk], [bkk], inc=(c == 7))
                sgs_, sgsk = sgs_r.next()
                ACT(sgs_[:], bk[:, 0:256], AF.Silu, [bkk], [sgsk])
                he_, hek = he_r.next()
                TT("vector", he_[:], sgs_[:], bk[:, 256:512], ALU.mult, [sgsk, bkk], [hek])
                DS[j]["he"] = (he_, hek)

            def sC(j):
                he_, hek = DS[j]["he"]
                bk2, bk2k = nbank8()
                bk2b = bk2[:].bitcast(BF16)
                for c2 in range(2):
                    TR(bk2b[:, c2 * 128:(c2 + 1) * 128], he_[:, c2:256:2], ident_b[:], [hek, "ident_b"], [bk2k], inc=(c2 == 1))
                heT_, heTk = heT_r.next()
                COPY("vector", heT_[:], bk2b[:, 0:256], [bk2k], [heTk])
                DS[j]["heT"] = (heT_, heTk)

            def sD(j):
                Wd, Wdk = dq[j][6], dq[j][7]
                heT_, heTk = DS[j]["heT"]
                yt, ytk, ytd = yt_r.next()
                for half in range(2):
                    bk, bkk = nbank8()
                    for c2 in range(2):
                        MM(bk[:, :], heT_[:, c2 * 128:(c2 + 1) * 128], Wd[:, c2 * 1024 + half * 512:c2 * 1024 + (half + 1) * 512], c2 == 0, c2 == 1, [heTk, Wdk], [bkk], inc=(c2 == 1))
                    COPY("scalar" if half == 0 else "vector", yt[:, half * 512:(half + 1) * 512], bk[:, :], [bkk], [ytk])
                DMA("sync", ytd, YS.ap()[j * 128:(j + 1) * 128, :], yt[:], [ytk], [f"YS{j}"])
                yskeys.append(f"YS{j}")
                dq.pop(j)
                DS.pop(j)

            d_loads(0)
            d_loads(1)
            for j in range(nslot_t + 2):
                if j + 2 < nslot_t:
                    d_loads(j + 2)
                if 0 <= j - 2 < nslot_t:
                    sC(j - 2)
                if j < nslot_t:
                    sA(j)
                if 0 <= j - 1 < nslot_t:
                    sB(j - 1)
                if 0 <= j - 2 < nslot_t:
                    sD(j - 2)
            x1_r = Ring([sbt(cd, f"ex{k}", [128, D], F32) for k in range(3)], "ex", p)
            y1_r = Ring([sbt(cd, f"ey1{k}", [128, D], F32) for k in range(3)], "ey1", p)
            y2_r = Ring([sbt(cd, f"ey2{k}", [128, D], F32) for k in range(3)], "ey2", p)
            ot_r = Ring([sbt(cd, f"ot{k}", [128, D], F32) for k in range(2)], "ot", p)
            es_r = Ring([sbt(cd, f"es{k}", [128, 4], F32) for k in range(2)], "es")
            junke = sbt(cd, "junke", [128, D], BF16)
            eq = {}

            def e_loads(i):
                ex, exk, exd = x1_r.next()
                DMA("sync", exd, ex[:], X1.ap()[i * 128:(i + 1) * 128, :], [], [exk])
                y1, y1k, y1d = y1_r.next()
                GATHER(y1d, y1[:], YS.ap(), Si[:, 0, i:i + 1], ["Si"] + yskeys, [y1k])
                y2, y2k, y2d = y2_r.next()
                GATHER(y2d, y2[:], YS.ap(), Si[:, 1, i:i + 1], ["Si"] + yskeys, [y2k])
                eq[i] = (ex, exk, y1, y1k, y2, y2k)

            e_loads(0)
            e_loads(1)
            for i in range(NT):
                if i + 2 < NT:
                    e_loads(i + 2)
                ex, exk, y1, y1k, y2, y2k = eq.pop(i)
                STT(ex[:], y1[:], W1all[:, i:i + 1], ex[:], ALU.mult, ALU.add, [y1k, exk, "W1all"], [exk])
                STT(ex[:], y2[:], W2all[:, i:i + 1], ex[:], ALU.mult, ALU.add, [y2k, exk, "W2all"], [exk])
                es, esk = es_r.next()
                ACT(junke[:], ex[:], AF.Square, [exk], ["junke", esk + "a"], accum_out=es[:, 0:1])
                rstd_from_ss(es[:, 2:3], es[:, 0:1], 1.0 / D, es[:, 1:2], esk + "c", esk + "a", esk + "b")
                ot, otk, otd = ot_r.next()
                STT(ot[:], ex[:], es[:, 2:3], gfB[:], ALU.mult, ALU.mult, [exk, esk + "c", "gfB"], [otk])
                DMA("sync", otd, out_d.ap()[i * 128:(i + 1) * 128, :], ot[:], [otk], [])
            p.barrier()
            p.recycle()
            p.emit()
    return nc


def make_in_maps(inputs):
    consts = host_consts()
    shared = {}
    f = lambda a: np.ascontiguousarray(np.asarray(a, dtype=np.float32))
    shared["w_in"] = f(inputs["w_in"][0])
    shared["kv_norm_g"] = f(inputs["kv_norm_g"]).reshape(1, 128)
    shared["w_uk"] = f(inputs["w_uk"][0]).reshape(128, 512)
    shared["w_uv"] = f(inputs["w_uv"][0]).reshape(128, 512)
    shared["rel_bias"] = f(inputs["rel_bias"])
    shared["ln_v_g"] = f(inputs["ln_v_g"]).reshape(1, 512)
    shared["ln_v_b"] = f(inputs["ln_v_b"]).reshape(1, 512)
    shared["w_spatial"] = f(inputs["w_spatial"][0])
    shared["b_spatial"] = f(inputs["b_spatial"][0]).reshape(1, 512)
    shared["w_proj_a"] = f(inputs["w_proj_a"][0])
    shared["w_proj_b"] = f(inputs["w_proj_b"][0])
    shared["w_out"] = f(inputs["w_out"][0])
    shared["norm1_g"] = f(f(inputs["norm1_g"][0]).reshape(8, 128).T)
    shared["norm2_g"] = f(inputs["norm2_g"]).reshape(1, D)
    shared["router_group_w"] = f(inputs["router_group_w"][0])
    shared["router_group_b"] = f(inputs["router_group_b"]).reshape(1, 4)
    shared["router_expert_w"] = f(inputs["router_expert_w"][0])
    shared["router_expert_b"] = f(inputs["router_expert_b"]).reshape(1, 32)
    shared["w_gate"] = f(inputs["w_gate"][0]).reshape(32 * 128, 2048)
    shared["w_up"] = f(inputs["w_up"][0]).reshape(32 * 128, 2048)
    shared["w_down"] = f(inputs["w_down"][0]).reshape(32 * 128, 2048)
    shared["final_norm_g"] = f(inputs["final_norm_g"]).reshape(1, D)
    shared.update(consts)
    x = f(inputs["x"])
    return [dict(shared, x=x[b]) for b in range(8)]


def kernel(**inputs):
    nc = build_program("E")
    in_maps = make_in_maps(inputs)
    res = run_bass_kernel_spmd(nc, in_maps, core_ids=list(range(8)))
    return np.stack([r["out"] for r in res.results], axis=0).astype(np.float32)
```
